# Optimizing a Trainium2 kernel written in Bass

```python
import jax
import jax.numpy as jnp
from jax import lax
import numpy as np

D_MODEL = 1024
BATCH = 4
SEQ = 4096
DEPTH = 2

N_BRANCH = 4
BRANCH_WIDTH = 256
HEAD_DIM = 64
N_HEADS = BRANCH_WIDTH // HEAD_DIM
MLA_Q_RANK = 256
MLA_KV_RANK = 128
MLA_NOPE_DIM = 64
MLA_ROPE_DIM = 32
MLA_V_DIM = BRANCH_WIDTH // N_HEADS
ROPE_BASE = 10000.0
CONV_CH = BRANCH_WIDTH
CONV_WIDTH = 31
FORGET_BIAS_INIT = 3.0
Q_BLOCK = 128
N_GROUPS = 4
EXPERTS_PER_GROUP = 8
N_EXPERTS = N_GROUPS * EXPERTS_PER_GROUP
TOP_K = 2
EXPERT_FF = 256
ROW_BLOCK = 128
NORM_EPS = 1e-5
IN_SPLITS = (MLA_Q_RANK, MLA_KV_RANK, MLA_ROPE_DIM, 3 * BRANCH_WIDTH, 2 * CONV_CH, 3 * BRANCH_WIDTH, N_HEADS, N_BRANCH * D_MODEL)
N_IN = sum(IN_SPLITS)

kernel_name = 'hybrid_mla_stickbreak_conformer_fox_hiermoe'


def _layernorm(x, g, b):
    xf = x.astype(jnp.float32)
    mu = jnp.mean(xf, axis=-1, keepdims=True)
    var = jnp.mean(jnp.square(xf - mu), axis=-1, keepdims=True)
    y = (xf - mu) * lax.rsqrt(var + NORM_EPS) * g.astype(jnp.float32) + b.astype(jnp.float32)
    return y.astype(x.dtype)


def _rmsnorm(x, g):
    xf = x.astype(jnp.float32)
    y = xf * lax.rsqrt(jnp.mean(xf * xf, axis=-1, keepdims=True) + NORM_EPS) * g.astype(jnp.float32)
    return y.astype(x.dtype)


def _rope(x, positions):
    half = x.shape[-1] // 2
    inv = ROPE_BASE ** (-jnp.arange(half, dtype=jnp.float32) / half)
    ang = (positions.astype(jnp.float32)[..., None] * inv)[:, :, None, :]
    cos, sin = jnp.cos(ang), jnp.sin(ang)
    x1 = x[..., :half].astype(jnp.float32)
    x2 = x[..., half:].astype(jnp.float32)
    out = jnp.concatenate([x1 * cos - x2 * sin, x2 * cos + x1 * sin], axis=-1)
    return out.astype(x.dtype)


def _split_cols(z):
    idx = [int(i) for i in np.cumsum(IN_SPLITS)[:-1]]
    return jnp.split(z, idx, axis=-1)


def _split_heads(qkv):
    b, s, _ = qkv.shape
    qkv = qkv.reshape(b, s, 3, N_HEADS, HEAD_DIM).transpose(2, 0, 3, 1, 4)
    return qkv[0], qkv[1], qkv[2]


def _merge_heads(o):
    b, h, s, d = o.shape
    return o.transpose(0, 2, 1, 3).reshape(b, s, h * d)


def _sweep_query_blocks(block_fn, q_side):
    b, h, s = q_side[0].shape[:3]
    nb = s // Q_BLOCK
    blocks = tuple(jnp.moveaxis(a.reshape((b, h, nb, Q_BLOCK) + a.shape[3:]), 2, 0) for a in q_side)
    t_pos = jnp.arange(s, dtype=jnp.int32).reshape(nb, Q_BLOCK)
    out = lax.map(lambda args: block_fn(*args), (t_pos,) + blocks)
    return jnp.moveaxis(out, 0, 2).reshape(b, h, s, out.shape[-1])


def _mla(c_q, c_kv, k_rope_raw, positions, q_norm, kv_norm, w_uq, w_ukv):
    b, s, _ = c_q.shape
    q = (_rmsnorm(c_q, q_norm) @ w_uq).reshape(b, s, N_HEADS, MLA_NOPE_DIM + MLA_ROPE_DIM)
    q_nope = q[..., :MLA_NOPE_DIM].transpose(0, 2, 1, 3)
    q_rope = _rope(q[..., MLA_NOPE_DIM:], positions).transpose(0, 2, 1, 3)
    kv = (_rmsnorm(c_kv, kv_norm) @ w_ukv).reshape(b, s, N_HEADS, MLA_NOPE_DIM + MLA_V_DIM)
    k_nope = kv[..., :MLA_NOPE_DIM].transpose(0, 2, 1, 3)
    v = kv[..., MLA_NOPE_DIM:].transpose(0, 2, 1, 3)
    k_rope = _rope(k_rope_raw[:, :, None, :], positions)[:, :, 0, :]
    scale = (MLA_NOPE_DIM + MLA_ROPE_DIM) ** -0.5
    key_pos = jnp.arange(s, dtype=jnp.int32)

    def block(t_pos, qn, qr):
        logits = jnp.einsum('bhqd,bhkd->bhqk', qn, k_nope) + jnp.einsum('bhqr,bkr->bhqk', qr, k_rope)
        logits = logits.astype(jnp.float32) * scale
        logits = jnp.where(key_pos[None, :] <= t_pos[:, None], logits, -jnp.inf)
        p = jax.nn.softmax(logits, axis=-1).astype(v.dtype)
        return jnp.einsum('bhqk,bhkd->bhqd', p, v)

    return _merge_heads(_sweep_query_blocks(block, (q_nope, q_rope)))


def _stick_breaking(q, k, v):
    s = k.shape[2]
    scale = HEAD_DIM ** -0.5
    key_pos = jnp.arange(s, dtype=jnp.int32)

    def block(t_pos, qb):
        z = jnp.einsum('bhqd,bhkd->bhqk', qb, k).astype(jnp.float32) * scale
        strict = key_pos[None, :] < t_pos[:, None]
        log_stay = jnp.where(strict, jax.nn.log_sigmoid(-z), 0.0)
        after = lax.cumsum(log_stay, axis=3, reverse=True) - log_stay
        a = jnp.where(strict, jnp.exp(jax.nn.log_sigmoid(z) + after), 0.0).astype(v.dtype)
        return jnp.einsum('bhqk,bhkd->bhqd', a, v)

    return _merge_heads(_sweep_query_blocks(block, (q,)))


def _conformer_conv(u, conv_w, conv_b, ln_g, ln_b):
    a, g = jnp.split(u, 2, axis=-1)
    h = a * jax.nn.sigmoid(g)
    hp = jnp.pad(h, ((0, 0), (CONV_WIDTH - 1, 0), (0, 0)))
    y = lax.conv_general_dilated(hp, conv_w[:, None, :], window_strides=(1,), padding='VALID',
                                 dimension_numbers=('NWC', 'WIO', 'NWC'), feature_group_count=CONV_CH)
    y = _layernorm(y + conv_b, ln_g, ln_b)
    return jax.nn.silu(y)


def _forgetting_attention(q, k, v, forget_logit, b_forget):
    s = k.shape[2]
    scale = HEAD_DIM ** -0.5
    log_f = jax.nn.log_sigmoid((forget_logit + b_forget).astype(jnp.float32))
    c = jnp.cumsum(log_f, axis=1).transpose(0, 2, 1)
    key_pos = jnp.arange(s, dtype=jnp.int32)

    def block(t_pos, qb, cb):
        logits = jnp.einsum('bhqd,bhkd->bhqk', qb, k).astype(jnp.float32) * scale
        logits = logits + cb[..., None] - c[:, :, None, :]
        logits = jnp.where(key_pos[None, :] <= t_pos[:, None], logits, -jnp.inf)
        p = jax.nn.softmax(logits, axis=-1).astype(v.dtype)
        return jnp.einsum('bhqk,bhkd->bhqd', p, v)

    return _merge_heads(_sweep_query_blocks(block, (q, c)))


def _token_mixer(x, positions, w_in, b_gate, b_forget, mla_q_norm, mla_kv_norm, mla_w_uq, mla_w_ukv,
                 conv_w, conv_b, conv_ln_g, conv_ln_b, w_branch, w_o):
    b, s, d = x.shape
    z = x @ w_in
    c_q, c_kv, k_rope, sb_qkv, conv_in, fox_qkv, fox_f, gate_logits = _split_cols(z)
    y_a = _mla(c_q, c_kv, k_rope, positions, mla_q_norm, mla_kv_norm, mla_w_uq, mla_w_ukv)
    y_b = _stick_breaking(*_split_heads(sb_qkv))
    y_c = _conformer_conv(conv_in, conv_w, conv_b, conv_ln_g, conv_ln_b)
    y_d = _forgetting_attention(*_split_heads(fox_qkv), fox_f, b_forget)
    ys = jnp.stack([y_a, y_b, y_c, y_d], axis=2)
    gates = jax.nn.sigmoid(gate_logits + b_gate).reshape(b, s, N_BRANCH, d)
    proj = jnp.einsum('bsnc,ncd->bsnd', ys, w_branch)
    merged = jnp.sum(proj * gates, axis=2)
    return merged @ w_o


def _hier_moe(h, w_rg, b_rg, w_re, b_re, w_gate, w_up, w_down):
    b, s, d = h.shape
    xt = h.reshape(-1, d)
    n_tok = xt.shape[0]
    grp_prob = jax.nn.softmax((xt @ w_rg + b_rg).astype(jnp.float32), axis=-1)
    grp_p, grp_idx = lax.top_k(grp_prob, 1)
    exp_logits = (xt @ w_re + b_re).astype(jnp.float32).reshape(n_tok, N_GROUPS, EXPERTS_PER_GROUP)
    in_grp = jnp.take_along_axis(exp_logits, grp_idx[:, :, None], axis=1)[:, 0]
    top_p, top_i = lax.top_k(jax.nn.softmax(in_grp, axis=-1), TOP_K)
    weights = grp_p * top_p / jnp.sum(top_p, axis=-1, keepdims=True)
    expert_id = (grp_idx * EXPERTS_PER_GROUP + top_i).reshape(-1)
    n_assign = expert_id.shape[0]
    order = jnp.argsort(expert_id).astype(jnp.int32)
    sorted_e = expert_id[order]
    counts = jnp.bincount(expert_id, length=N_EXPERTS).astype(jnp.int32)
    padded = (counts + ROW_BLOCK - 1) // ROW_BLOCK * ROW_BLOCK
    pad_end = jnp.cumsum(padded)
    pad_start = pad_end - padded
    start = jnp.cumsum(counts) - counts
    slot = pad_start[sorted_e] + jnp.arange(n_assign, dtype=jnp.int32) - start[sorted_e]
    n_blocks = (n_assign + ROW_BLOCK - 1) // ROW_BLOCK + N_EXPERTS
    slot_token = jnp.full((n_blocks * ROW_BLOCK,), n_tok, dtype=jnp.int32).at[slot].set(order // TOP_K)
    block_start = jnp.arange(n_blocks, dtype=jnp.int32) * ROW_BLOCK
    block_expert = jnp.minimum(jnp.searchsorted(pad_end, block_start, side='right'), N_EXPERTS - 1)
    x_pad = jnp.concatenate([xt, jnp.zeros((1, d), xt.dtype)], axis=0)
    xb = x_pad[slot_token].reshape(n_blocks, ROW_BLOCK, d)

    def expert_block(args):
        xe, e = args
        hid = jax.nn.silu(xe @ w_gate[e]) * (xe @ w_up[e])
        return hid @ w_down[e]

    yb = lax.map(expert_block, (xb, block_expert)).reshape(-1, d)
    y_assign = jnp.zeros((n_assign, d), yb.dtype).at[order].set(yb[slot])
    y = jnp.einsum('tkd,tk->td', y_assign.reshape(n_tok, TOP_K, d), weights.astype(yb.dtype))
    return y.reshape(b, s, d)


def setup_inputs(seed: int = 0) -> dict:
    key = jax.random.key(seed)
    ks = jax.random.split(key, 28)
    L, D = DEPTH, D_MODEL
    beta = (8.0 * DEPTH) ** -0.25

    def nrm(k, shape, scale):
        return jax.random.normal(k, shape, jnp.float32) * scale

    def gain(k, shape):
        return 1.0 + 0.02 * jax.random.normal(k, shape, jnp.float32)

    offset = jax.random.randint(ks[1], (BATCH, 1), 0, 1024, dtype=jnp.int32)
    positions = offset + jnp.arange(SEQ, dtype=jnp.int32)[None, :]
    return {
        'x': nrm(ks[0], (BATCH, SEQ, D), 1.0),
        'positions': positions,
        'w_in': nrm(ks[2], (L, D, N_IN), D ** -0.5),
        'b_gate': nrm(ks[3], (L, N_BRANCH * D), 0.02),
        'b_forget': FORGET_BIAS_INIT + nrm(ks[4], (L, N_HEADS), 0.1),
        'mla_q_norm': gain(ks[5], (L, MLA_Q_RANK)),
        'mla_kv_norm': gain(ks[6], (L, MLA_KV_RANK)),
        'mla_w_uq': nrm(ks[7], (L, MLA_Q_RANK, N_HEADS * (MLA_NOPE_DIM + MLA_ROPE_DIM)), MLA_Q_RANK ** -0.5),
        'mla_w_ukv': nrm(ks[8], (L, MLA_KV_RANK, N_HEADS * (MLA_NOPE_DIM + MLA_V_DIM)), MLA_KV_RANK ** -0.5),
        'conv_w': nrm(ks[9], (L, CONV_WIDTH, CONV_CH), CONV_WIDTH ** -0.5),
        'conv_b': nrm(ks[10], (L, CONV_CH), 0.02),
        'conv_ln_g': gain(ks[11], (L, CONV_CH)),
        'conv_ln_b': nrm(ks[12], (L, CONV_CH), 0.02),
        'w_branch': nrm(ks[13], (L, N_BRANCH, BRANCH_WIDTH, D), BRANCH_WIDTH ** -0.5),
        'w_o': nrm(ks[14], (L, D, D), beta * D ** -0.5),
        'ln1_g': gain(ks[15], (L, D)),
        'ln1_b': nrm(ks[16], (L, D), 0.02),
        'w_router_group': nrm(ks[17], (L, D, N_GROUPS), D ** -0.5),
        'b_router_group': nrm(ks[18], (L, N_GROUPS), 0.01),
        'w_router_expert': nrm(ks[19], (L, D, N_EXPERTS), D ** -0.5),
        'b_router_expert': nrm(ks[20], (L, N_EXPERTS), 0.01),
        'w_exp_gate': nrm(ks[21], (L, N_EXPERTS, D, EXPERT_FF), D ** -0.5),
        'w_exp_up': nrm(ks[22], (L, N_EXPERTS, D, EXPERT_FF), D ** -0.5),
        'w_exp_down': nrm(ks[23], (L, N_EXPERTS, EXPERT_FF, D), beta * EXPERT_FF ** -0.5),
        'ln2_g': gain(ks[24], (L, D)),
        'ln2_b': nrm(ks[25], (L, D), 0.02),
    }


def reference(x, positions, w_in, b_gate, b_forget, mla_q_norm, mla_kv_norm, mla_w_uq, mla_w_ukv,
              conv_w, conv_b, conv_ln_g, conv_ln_b, w_branch, w_o, ln1_g, ln1_b,
              w_router_group, b_router_group, w_router_expert, b_router_expert,
              w_exp_gate, w_exp_up, w_exp_down, ln2_g, ln2_b):
    alpha = (2.0 * DEPTH) ** 0.25
    for l in range(DEPTH):
        mix = _token_mixer(x, positions, w_in[l], b_gate[l], b_forget[l], mla_q_norm[l], mla_kv_norm[l],
                           mla_w_uq[l], mla_w_ukv[l], conv_w[l], conv_b[l], conv_ln_g[l], conv_ln_b[l],
                           w_branch[l], w_o[l])
        x = _layernorm(alpha * x + mix, ln1_g[l], ln1_b[l])
        ffn = _hier_moe(x, w_router_group[l], b_router_group[l], w_router_expert[l], b_router_expert[l],
                        w_exp_gate[l], w_exp_up[l], w_exp_down[l])
        x = _layernorm(alpha * x + ffn, ln2_g[l], ln2_b[l])
    return x
```

```python
import numpy as np
from contextlib import ExitStack
import concourse.bass as bass
import concourse.mybir as mybir
from concourse.bass_utils import run_bass_kernel_spmd

F32 = mybir.dt.float32
BF16 = mybir.dt.bfloat16
I32 = mybir.dt.int32
ALU = mybir.AluOpType
AF = mybir.ActivationFunctionType
AX = mybir.AxisListType

D = 1024
SEQ = 4096
TL = 2048
GS = 512
NSLOT = 4
G_PAR = ((0, 3, 4, 7), (1, 2, 5, 6))
ALPHA = float((2.0 * 2) ** 0.25)
EPS = 1e-5
NEGBIG = -30000.0
DEPTH = 2
N_CORES = 8


class Buf:
    __slots__ = ("name", "w", "r")

    def __init__(self, name=""):
        self.name = name
        self.w = None
        self.r = {}


class Sched:
    ENGS = ("pe", "act", "dve", "pool", "sp")

    def __init__(self, nc):
        self.nc = nc
        self.q = {e: [] for e in self.ENGS}
        self.cnt = {e: 0 for e in self.ENGS}
        self.sems = {}
        self._ctx = []
        for e in self.ENGS:
            cm = nc.semaphore("s_" + e)
            self.sems[e] = cm.__enter__()
            self._ctx.append(cm)
        self.dma_sems = {}
        self.dma_cnt = {}
        self.seen = {e: {} for e in self.ENGS}
        self.n_instr = 0
        self.epoch = 0

    def new_epoch(self):
        self.epoch += 1
        for e in self.ENGS:
            cm = self.nc.semaphore("s_%s_%d" % (e, self.epoch))
            self.sems[e] = cm.__enter__()
            self._ctx.append(cm)
            self.cnt[e] = 0
        for e in self.ENGS:
            self.seen[e] = {k: v for k, v in self.seen[e].items() if not isinstance(k, tuple)}

    def close(self):
        for cm in reversed(self._ctx):
            cm.__exit__(None, None, None)

    def _dma_sem(self, key):
        if key not in self.dma_sems:
            cm = self.nc.semaphore("d_" + str(key))
            self.dma_sems[key] = cm.__enter__()
            self._ctx.append(cm)
            self.dma_cnt[key] = 0
        return self.dma_sems[key]

    def _semh(self, key):
        return self.sems[key[0]] if isinstance(key, tuple) else self.dma_sems[key]

    def _waits(self, eng, reads, writes):
        need = {}
        for b in reads:
            if b.w is not None:
                k, v = b.w
                if v > need.get(k, 0):
                    need[k] = v
        for b in writes:
            if b.w is not None:
                k, v = b.w
                if v > need.get(k, 0):
                    need[k] = v
            for k, v in b.r.items():
                if v > need.get(k, 0):
                    need[k] = v
        out = []
        seen = self.seen[eng]
        for k, v in need.items():
            if isinstance(k, tuple):
                if k[1] < self.epoch:
                    continue
                if k[0] == "pe" and eng == "pe":
                    continue
            if seen.get(k, 0) >= v:
                continue
            seen[k] = v
            out.append((self._semh(k), v))
        return out

    def _mark(self, tok, reads, writes):
        k, v = tok
        for b in reads:
            if v > b.r.get(k, 0):
                b.r[k] = v
        for b in writes:
            b.w = tok
            b.r = {}

    def op(self, eng, fn, reads=(), writes=()):
        waits = self._waits(eng, reads, writes)
        self.cnt[eng] += 1
        tok = ((eng, self.epoch), self.cnt[eng])
        self.q[eng].append((waits, fn, self.sems[eng], 1))
        self._mark(tok, reads, writes)
        self.n_instr += 1
        return tok

    def dma(self, queue, key, fn, reads=(), writes=(), inc=16):
        waits = self._waits(queue, reads, writes)
        sem = self._dma_sem(key)
        self.dma_cnt[key] += inc
        tok = (key, self.dma_cnt[key])
        self.q[queue].append((waits, fn, sem, inc))
        self._mark(tok, reads, writes)
        self.n_instr += 1
        return tok

    def barrier(self, skip=()):
        for e in self.ENGS:
            waits = []
            seen = self.seen[e]
            for k in self.ENGS:
                v = self.cnt[k]
                kk = (k, self.epoch)
                if v > seen.get(kk, 0) and not (k == "pe" and e == "pe" and False):
                    seen[kk] = v
                    waits.append((self.sems[k], v))
            for k, v in self.dma_cnt.items():
                if k in skip:
                    continue
                if v > seen.get(k, 0):
                    seen[k] = v
                    waits.append((self.dma_sems[k], v))
            if waits:
                self.q[e].append((waits, None, None, 0))

    def emit(self):
        nc = self.nc
        qs = self.q

        def run(e, items):
            for waits, fn, sem, inc in items:
                for (s, v) in waits:
                    e.wait_ge(s, v)
                if fn is not None:
                    fn(e).then_inc(sem, inc)

        with nc.Block() as block:
            @block.tensor
            def _(e):
                run(e, qs["pe"])

            @block.scalar
            def _(e):
                run(e, qs["act"])

            @block.vector
            def _(e):
                run(e, qs["dve"])

            @block.gpsimd
            def _(e):
                run(e, qs["pool"])

            @block.sync
            def _(e):
                run(e, qs["sp"])


class RPool:
    def __init__(self, tiles):
        self.tiles = tiles
        self.bufs = [Buf() for _ in tiles]
        self.i = 0

    def next(self):
        j = self.i % len(self.tiles)
        self.i += 1
        return j, self.tiles[j], self.bufs[j]


def _kc(w):
    k, m = w.shape
    n = k // 128
    return np.ascontiguousarray(w.reshape(n, 128, m).transpose(1, 0, 2).reshape(128, n * m))


def layer_arrays(inp, l):
    f = np.float32
    w_in = inp["w_in"][l]
    A = {}
    A["wK_ckv"] = _kc(w_in[:, 256:384])
    kr = w_in[:, 384:416]
    pad = np.zeros((D, 128), f)
    pad[:, 64:96] = kr
    A["wK_kr"] = _kc(pad)
    pad = np.zeros((D, 128), f)
    pad[:, 64:80] = kr[:, 16:32]
    pad[:, 80:96] = kr[:, 0:16]
    A["wK_krr"] = _kc(pad)
    A["wK_sbk"] = _kc(w_in[:, 672:928])
    A["wK_fxk"] = _kc(w_in[:, 1952:2208])
    A["wK_v2"] = _kc(np.concatenate([w_in[:, 928:1184], w_in[:, 2208:2464]], 1))
    A["wK_f"] = _kc(w_in[:, 2464:2468])
    ukv = inp["mla_w_ukv"][l]
    A["w_ukv_k"] = np.ascontiguousarray(np.concatenate([ukv[:, h * 128:h * 128 + 64] for h in range(4)], 1))
    A["w_ukv_v"] = np.ascontiguousarray(np.concatenate([ukv[:, h * 128 + 64:h * 128 + 128] for h in range(4)], 1))
    A["kvn"] = np.ascontiguousarray(inp["mla_kv_norm"][l].reshape(128, 1))
    A["wQ_cq"] = _kc(w_in[:, 0:256])
    uq = inp["mla_w_uq"][l]
    A["w_uq"] = _kc(uq)
    uqr = np.zeros_like(uq)
    for h in range(4):
        o = h * 96 + 64
        uqr[:, o:o + 16] = uq[:, o + 16:o + 32]
        uqr[:, o + 16:o + 32] = uq[:, o:o + 16]
    A["w_uqr"] = _kc(uqr)
    A["qn"] = np.ascontiguousarray(inp["mla_q_norm"][l].reshape(2, 128).T)
    A["wQ_sbq"] = _kc(w_in[:, 416:672])
    A["wQ_fxq"] = _kc(w_in[:, 1696:1952])
    A["wQ_cv"] = _kc(w_in[:, 1184:1696])
    g = w_in[:, 2468:6564].reshape(D, 4, 8, 128)
    g = g.reshape(8, 128, 4, 8, 128).transpose(3, 2, 1, 0, 4)
    A["wG"] = np.ascontiguousarray(g.reshape(32, 128, 1024))
    A["bg"] = np.ascontiguousarray(inp["b_gate"][l].reshape(4, 8, 128).transpose(2, 1, 0).reshape(128, 32))
    wb = inp["w_branch"][l].reshape(4, 2, 128, D).transpose(2, 0, 1, 3)
    A["wB"] = np.ascontiguousarray(wb.reshape(128, 8 * D))
    A["wO"] = _kc(inp["w_o"][l])
    A["convw"] = np.ascontiguousarray(inp["conv_w"][l].reshape(31, 2, 128).transpose(2, 1, 0).reshape(128, 62))
    A["convb"] = np.ascontiguousarray(inp["conv_b"][l].reshape(2, 128).T)
    A["clng"] = np.ascontiguousarray(inp["conv_ln_g"][l].reshape(2, 128).T)
    A["clnb"] = np.ascontiguousarray(inp["conv_ln_b"][l].reshape(2, 128).T)
    for k in ("ln1_g", "ln1_b", "ln2_g", "ln2_b"):
        A[k] = np.ascontiguousarray(inp[k][l].reshape(1, D))
    A["bfg"] = np.ascontiguousarray(inp["b_forget"][l].reshape(1, 4))
    A["wR"] = _kc(np.concatenate([inp["w_router_group"][l], inp["w_router_expert"][l]], 1))
    A["bR"] = np.ascontiguousarray(np.concatenate([inp["b_router_group"][l], inp["b_router_expert"][l]]).reshape(1, 36))
    eg = inp["w_exp_gate"][l].reshape(32, 8, 128, 256).transpose(0, 2, 1, 3)
    A["wEg"] = np.ascontiguousarray(eg.reshape(32, 128, 2048))
    eu = inp["w_exp_up"][l].reshape(32, 8, 128, 256).transpose(0, 2, 1, 3)
    A["wEu"] = np.ascontiguousarray(eu.reshape(32, 128, 2048))
    ed = inp["w_exp_down"][l].reshape(32, 2, 128, D).transpose(0, 2, 1, 3)
    A["wEd"] = np.ascontiguousarray(ed.reshape(32, 128, 2048))
    return {k: np.asarray(v, dtype=f) for k, v in A.items()}


LAYER_SHAPES = {
    "wK_ckv": [128, 1024], "wK_kr": [128, 1024], "wK_krr": [128, 1024], "wK_sbk": [128, 2048],
    "wK_fxk": [128, 2048], "wK_v2": [128, 4096], "wK_f": [128, 32], "w_ukv_k": [128, 256],
    "w_ukv_v": [128, 256], "kvn": [128, 1], "wQ_cq": [128, 2048], "w_uq": [128, 768],
    "w_uqr": [128, 768], "qn": [128, 2], "wQ_sbq": [128, 2048], "wQ_fxq": [128, 2048],
    "wQ_cv": [128, 4096], "wG": [32, 128, 1024], "bg": [128, 32], "wB": [128, 8192],
    "wO": [128, 8192], "convw": [128, 62], "convb": [128, 2], "clng": [128, 2], "clnb": [128, 2],
    "ln1_g": [1, D], "ln1_b": [1, D], "ln2_g": [1, D], "ln2_b": [1, D], "bfg": [1, 4],
    "wR": [128, 288], "bR": [1, 36], "wEg": [32, 128, 2048], "wEu": [32, 128, 2048],
    "wEd": [32, 128, 2048],
}


def core_meta(core):
    par = core % 2
    thr = np.zeros((128, 32), np.float32)
    for s in range(4):
        for j in range(8):
            kb = 8 * s + j
            thr[:, s * 8 + j] = G_PAR[par][s] * GS - kb * 128
    sel = np.zeros((128, 2), np.float32)
    sel[:, par] = 1.0
    inv = 10000.0 ** (-(np.arange(16, dtype=np.float64)) / 16.0)
    inv2pi = np.zeros((128, 1), np.float32)
    for i in range(32):
        inv2pi[64 + i, 0] = inv[i % 16] / (2 * np.pi)
    return thr, sel, inv2pi


class Prog:
    def __init__(self, layers, debug=False):
        self.layers = layers
        self.debug = debug
        nc = bass.Bass("TRN2", target_bir_lowering=False)
        self.nc = nc
        self.S = Sched(nc)
        self.dram = {}
        self.dbuf = {}

    def din(self, name, shape, dt=F32):
        t = self.nc.dram_tensor(name, list(shape), dt, kind="ExternalInput")
        self.dram[name] = t
        return t

    def dout(self, name, shape, dt=F32):
        t = self.nc.dram_tensor(name, list(shape), dt, kind="ExternalOutput")
        self.dram[name] = t
        return t

    def dint(self, name, shape, dt):
        t = self.nc.dram_tensor(name, list(shape), dt)
        self.dram[name] = t
        return t

    def sb(self, stack, name, shape, dt):
        self._sbn = getattr(self, "_sbn", 0) + 1
        return stack.enter_context(self.nc.sbuf_tensor("s%d_%s" % (self._sbn, name), list(shape), dt))

    def pool(self, stack, name, shape, dt, n):
        return RPool([self.sb(stack, "%s%d" % (name, i), shape, dt) for i in range(n)])

    def mm(self, out, lhsT, rhs, start, stop, reads, writes):
        self.S.op("pe", lambda e, o=out, l=lhsT, r=rhs, a=start, b=stop: e.matmul(o, l, r, start=a, stop=b),
                  reads, writes)

    def tr(self, out, in_, reads, writes):
        idn = self.ident
        self.S.op("pe", lambda e, o=out, i=in_: e.transpose(o, i, idn[:]), list(reads) + [self.b_const], writes)

    def act(self, out, in_, func, reads, writes, bias=None, scale=None, accum_out=None):
        kw = {}
        if bias is not None:
            kw["bias"] = bias
        if scale is not None:
            kw["scale"] = scale
        if accum_out is not None:
            kw["accum_out"] = accum_out
        self.S.op("act", lambda e, o=out, i=in_, f=func, kw=kw: e.activation(out=o, in_=i, func=f, **kw), reads, writes)

    def tt(self, eng, out, in0, in1, op, reads, writes):
        self.S.op(eng, lambda e, o=out, a=in0, b=in1, p=op: e.tensor_tensor(out=o, in0=a, in1=b, op=p), reads, writes)

    def ts(self, eng, out, in0, s1, s2, op0, op1, reads, writes):
        if op1 is None:
            self.S.op(eng, lambda e, o=out, a=in0, x=s1, p=op0: e.tensor_single_scalar(out=o, in_=a, scalar=x, op=p),
                      reads, writes)
        else:
            self.S.op(eng, lambda e, o=out, a=in0, x=s1, y=s2, p=op0, q=op1:
                      e.tensor_scalar(out=o, in0=a, scalar1=x, scalar2=y, op0=p, op1=q), reads, writes)

    def stt(self, eng, out, in0, scalar, in1, op0, op1, reads, writes):
        self.S.op(eng, lambda e, o=out, a=in0, s=scalar, b=in1, p=op0, q=op1:
                  e.scalar_tensor_tensor(out=o, in0=a, scalar=s, in1=b, op0=p, op1=q), reads, writes)

    def cp(self, eng, out, in_, reads, writes):
        if eng == "act":
            self.S.op("act", lambda e, o=out, i=in_: e.copy(out=o, in_=i), reads, writes)
        else:
            self.S.op(eng, lambda e, o=out, i=in_: e.tensor_copy(out=o, in_=i), reads, writes)

    def dma(self, queue, key, out, in_, reads, writes):
        self.S.dma(queue, key, lambda e, o=out, i=in_: e.dma_start(out=o, in_=i), reads, writes)

    def dump(self, name, src, shape, dt, reads):
        if not self.debug:
            return
        t = self.dout("dbg_" + name, shape, dt)
        b = Buf("dbg_" + name)
        self.dma("sp", "dbg_" + name, t.ap(), src, reads, [b])

    def evac_eng(self):
        self._ev = getattr(self, "_ev", 0) + 1
        return "act" if self._ev % 2 else "dve"

    def setup_consts(self, st):
        nc, S = self.nc, self.S
        P = self
        self.b_const = Buf("const")
        bc = [self.b_const]
        self.iotf = self.sb(st, "iotf", [128, 512], F32)
        self.ident = self.sb(st, "ident", [128, 128], F32)
        self.ones_bf = self.sb(st, "ones_bf", [128, 128], BF16)
        self.ones_f = self.sb(st, "ones_f", [128, 128], F32)
        self.LT = self.sb(st, "LT", [128, 128], BF16)
        self.UT = self.sb(st, "UT", [128, 128], F32)
        self.D0 = self.sb(st, "D0", [128, 512], F32)
        self.D1 = self.sb(st, "D1", [128, 512], F32)
        self.selE = self.sb(st, "selE", [32, 32 * 128], BF16)
        self.Eh = self.sb(st, "Eh", [4, 4 * 65], BF16)
        self.epsc = self.sb(st, "epsc", [128, 1], F32)
        self.onec = self.sb(st, "onec", [128, 1], F32)
        self.thr = self.sb(st, "thr", [128, 32], F32)
        self.sel = self.sb(st, "sel", [128, 2], F32)
        self.sel8 = self.sb(st, "sel8", [128, 2], F32)
        self.inv2pi = self.sb(st, "inv2pi", [128, 1], F32)
        S.op("pool", lambda e: e.iota(self.iotf[:], [[1, 512]], base=0, channel_multiplier=-1,
                                      allow_small_or_imprecise_dtypes=True), [], bc)
        P.ts("dve", self.ident[:], self.iotf[:, 0:128], 0.0, None, ALU.is_equal, None, bc, bc)
        P.ts("dve", self.UT[:], self.iotf[:, 0:128], 0.0, None, ALU.is_ge, None, bc, bc)
        P.ts("dve", self.LT[:], self.iotf[:, 0:128], 0.0, None, ALU.is_le, None, bc, bc)
        P.ts("dve", self.D0[:], self.iotf[:], -1.0, None, ALU.mult, None, bc, bc)
        P.ts("dve", self.D1[:], self.D0[:], 1.0, None, ALU.add, None, bc, bc)
        S.op("pool", lambda e: e.memset(self.ones_bf[:], 1.0), [], bc)
        S.op("pool", lambda e: e.memset(self.ones_f[:], 1.0), [], bc)
        S.op("pool", lambda e: e.memset(self.epsc[:], EPS), [], bc)
        S.op("pool", lambda e: e.memset(self.onec[:], 1.0), [], bc)
        tst = ExitStack()
        tmp = self.sb(tst, "seltmp", [32, 32 * 128], F32)
        S.op("pool", lambda e: e.iota(tmp[:].rearrange("k (e m) -> k e m", m=128), [[1, 32], [0, 128]], base=0,
                                      channel_multiplier=-1, allow_small_or_imprecise_dtypes=True), [], bc)
        P.ts("dve", self.selE[:], tmp[:], 0.0, None, ALU.is_equal, None, bc, bc)
        S.op("pool", lambda e: e.iota(tmp[0:4, 0:260].rearrange("k (h m) -> k h m", m=65), [[1, 4], [0, 65]], base=0,
                                      channel_multiplier=-1, allow_small_or_imprecise_dtypes=True), bc, bc)
        P.ts("dve", self.Eh[:], tmp[0:4, 0:260], 0.0, None, ALU.is_equal, None, bc, bc)
        S.op("dve", lambda e: e.memset(self.Eh[:].rearrange("k (h m) -> k h m", m=65)[:, :, 0:64], 0.0), bc, bc)
        S.barrier()
        tst.close()
        thr_d = self.din("thr", [128, 32])
        sel_d = self.din("sel", [128, 2])
        inv_d = self.din("inv2pi", [128, 1])
        P.dma("sp", "c_thr", self.thr[:], thr_d.ap(), [], bc)
        P.dma("sp", "c_sel", self.sel[:], sel_d.ap(), [], bc)
        P.dma("sp", "c_inv", self.inv2pi[:], inv_d.ap(), [], bc)
        P.ts("dve", self.sel8[:], self.sel[:], -8.0, None, ALU.mult, None, bc, bc)
        self.psb = [st.enter_context(nc.psum_tensor("psb%d" % i, [128, 512], F32)) for i in range(8)]
        self.ps_bufs = [Buf("ps%d" % i) for i in range(8)]
        self.ps_i = 0

    def ps_next(self, lo=0, hi=8):
        key = (lo, hi)
        if not hasattr(self, "_psrot"):
            self._psrot = {}
        i = self._psrot.get(key, 0)
        self._psrot[key] = i + 1
        j = lo + i % (hi - lo)
        return self.psb[j], self.ps_bufs[j]

    def rope_tables(self, tmp, src, ncols, cos_out, sin_out, out_buf):
        P, S = self, self.S
        pi_, pfl, t, ki, kf, bt = tmp
        sl = slice(64, 96)
        n = ncols
        P.dma("sp", "rp_ld", pi_[sl, 0:n], src.partition_broadcast(32), [], [bt])
        P.cp("dve", pfl[sl, 0:n], pi_[sl, 0:n], [bt], [bt])
        for which, tab in ((0.0, sin_out), (0.25, cos_out)):
            P.ts("dve", t[sl, 0:n], pfl[sl, 0:n], self.inv2pi[sl, 0:1], which, ALU.mult, ALU.add, [bt, self.b_const], [bt])
            P.cp("dve", ki[sl, 0:n], t[sl, 0:n], [bt], [bt])
            P.cp("dve", kf[sl, 0:n], ki[sl, 0:n], [bt], [bt])
            P.tt("dve", t[sl, 0:n], t[sl, 0:n], kf[sl, 0:n], ALU.subtract, [bt], [bt])
            P.ts("dve", kf[sl, 0:n], t[sl, 0:n], 0.5, None, ALU.is_gt, None, [bt], [bt])
            P.tt("dve", t[sl, 0:n], t[sl, 0:n], kf[sl, 0:n], ALU.subtract, [bt], [bt])
            P.ts("dve", kf[sl, 0:n], t[sl, 0:n], -0.5, None, ALU.is_lt, None, [bt], [bt])
            P.tt("dve", t[sl, 0:n], t[sl, 0:n], kf[sl, 0:n], ALU.add, [bt], [bt])
            P.act(tab, t[sl, 0:n], AF.Sin, [bt], [bt, out_buf], scale=float(2 * np.pi * (1 - 1e-6)))

    def rope_tmp(self, st, n):
        return (self.sb(st, "rp_i", [128, n], I32), self.sb(st, "rp_f", [128, n], F32), self.sb(st, "rp_t", [128, n], F32),
                self.sb(st, "rp_ki", [128, n], I32), self.sb(st, "rp_kf", [128, n], F32), Buf("rp"))

    def setup_rope(self, st):
        self.pos_full = self.din("pos_full", [1, SEQ], I32)
        pl = self.din("pos_loc", [1, TL], I32)
        self.cosL = self.sb(st, "cosL", [128, TL], BF16)
        self.sinL = self.sb(st, "sinL", [128, TL], BF16)
        self.b_rope = Buf("rope")
        with ExitStack() as tmp:
            T = self.rope_tmp(tmp, 512)
            for c in range(4):
                cs = slice(c * 512, (c + 1) * 512)
                self.rope_tables(T, pl.ap()[:, cs], 512, self.cosL[64:96, cs], self.sinL[64:96, cs], self.b_rope)
            self.S.barrier()

    def load_w(self, st, name, shape, src, cast=True, key=None, skip=False):
        t = self.sb(st, name, shape, BF16 if cast else F32)
        b = Buf(name)
        if src is not None:
            self.dma("pool" if cast else "sp", key or ("w_" + name), t[:], src, [], [b])
        return t, b

    def transpose_tiles(self, st_pools, row_aps, dst, dst_buf, col0):
        xin = st_pools["xin"]
        tiles = []
        for t, ap in enumerate(row_aps):
            j, xt, xb = xin.next()
            self.dma("sp", "xin%d" % j, xt[:], ap, [], [xb])
            tiles.append((xt, xb))
        n = len(tiles)
        for dc in range(8):
            ps, pb = self.ps_next()
            for t, (xt, xb) in enumerate(tiles):
                self.tr(ps[:, t * 128:(t + 1) * 128], xt[:, dc * 128:(dc + 1) * 128], [xb], [pb])
            self.cp(self.evac_eng(), dst[:, dc, col0:col0 + n * 128], ps[:, 0:n * 128], [pb], [dst_buf])

    def rms_scale(self, st_pools, ps_list, pb_list, n_feat, out_tile, out_buf, ncols=512, ps_range=(0, 8)):
        sqp = st_pools["sq"]
        f32p = st_pools["f32"]
        sqs = []
        for ps, pb in zip(ps_list, pb_list):
            j, sq, sqb = sqp.next()
            self.act(sq[:, 0:ncols], ps[:, 0:ncols], AF.Square, [pb], [sqb])
            sqs.append((sq, sqb))
        pss, pbs = self.ps_next(*ps_range)
        for i, (sq, sqb) in enumerate(sqs):
            self.mm(pss[:, 0:ncols], self.ones_bf[:], sq[:, 0:ncols], i == 0, i == len(sqs) - 1,
                    [sqb, self.b_const], [pbs])
        j, sd, sdb = f32p.next()
        self.act(sd[:, 0:ncols], pss[:, 0:ncols], AF.Sqrt, [pbs, self.b_const], [sdb], bias=self.epsc[:, 0:1],
                 scale=1.0 / n_feat)
        self.S.op("dve", lambda e, o=sd[:, 0:ncols]: e.reciprocal(out=o, in_=o), [sdb], [sdb])
        for i, (ps, pb) in enumerate(zip(ps_list, pb_list)):
            self.tt("dve", out_tile[:, i, 0:ncols], ps[:, 0:ncols], sd[:, 0:ncols], ALU.mult, [pb, sdb], [out_buf])

    def emit_layer(self, l, xfull_rows, xloc_rows, out_rows, W):
        nc, S, P = self.nc, self.S, self
        bc = self.b_const
        dr = self.dr
        with ExitStack() as lst:
            negcK = self.sb(lst, "negcK", [128, 32 * 4], F32)
            negcKb = Buf("negcK")
            cqa = self.sb(lst, "cqa", [4, TL], BF16)
            cqab = Buf("cqa")
            WtT = self.sb(lst, "WtT", [32, TL], BF16)
            WtTb = [Buf("WtT%d" % s) for s in range(4)]
            actT = self.sb(lst, "actT", [128, 8, TL], BF16)
            actTb = [Buf("actT%d" % s) for s in range(4)]
            import os
            stop = int(os.environ.get("KSTOP", "9"))
            if stop < 1:
                return
            if not self.kv_exchange:
                self.stage_K(l, xfull_rows, W, negcK, negcKb, cqa, cqab)
            if self.debug:
                for nm, shp in (("KTm", [4, 96, SEQ]), ("Vm", [SEQ, 256]), ("KTs", [4, 64, SEQ]), ("Vs", [SEQ, 256]),
                                ("KTf", [4, 64, SEQ]), ("Vf", [SEQ, 256])):
                    self.dump(nm, self.dr[nm].ap(), shp, BF16, [self.dbuf[nm]])
                self.dump("negcK", negcK[:], [128, 128], F32, [negcKb])
                self.dump("cqa", cqa[:], [4, TL], BF16, [cqab])
            if stop < 2:
                return
            with ExitStack() as yst:
                yT = [self.sb(yst, "yT%d" % n, [128, 2, TL], BF16) for n in range(4)]
                yTb = [[Buf("yT%d_%d" % (n, s)) for s in range(4)] for n in range(4)]
                self.stage_Q(l, xfull_rows, xloc_rows, W, actT, actTb, yT, yTb, negcK, negcKb, cqa, cqab)
                if self.debug:
                    for n in range(4):
                        self.dump("yT%d" % n, yT[n][:], [128, 2, TL], BF16, yTb[n])
                    self.dump("xlT", actT[:], [128, 8, TL], BF16, actTb)
                    self.dump("negcK2", negcK[:], [128, 128], F32, [negcKb])
                if stop < 4:
                    return
                with ExitStack() as mst:
                    mrgT = self.sb(mst, "mrgT", [128, 8, TL], BF16)
                    mrgTb = [Buf("mrgT%d" % s) for s in range(4)]
                    self.stage_G(l, W, actT, actTb, yT, yTb, mrgT, mrgTb)
                    self.dump("mrgT", mrgT[:], [128, 8, TL], BF16, mrgTb)
                    if stop < 5:
                        return
                    self.stage_F1(l, xloc_rows, W, actT, actTb, mrgT, mrgTb, WtT, WtTb)
            self.dump("h", self.dr["h"].ap(), [TL, D], F32, [self.dbuf["h"]])
            self.dump("WtT", WtT[:], [32, TL], BF16, WtTb)
            if stop < 6:
                return
            self.stage_F2(l, out_rows, W, actT, actTb, WtT, WtTb)

    def mk_pools(self, st, xin=True):
        pools = {
            "sq": self.pool(st, "sq", [128, 512], BF16, 2),
            "f32": self.pool(st, "f32", [128, 512], F32, 6),
            "bf": self.pool(st, "bft", [128, 512], BF16, 8),
        }
        if xin:
            pools["xin"] = self.pool(st, "xin", [128, 1024], F32, 4)
        return pools

    def stage_K2(self, l, W, xlT, actTb, cqa, cqab, xloc_rows):
        S, P = self.S, self
        bc = self.b_const
        X = self.xch
        r3 = lambda name, m: W[name].ap().rearrange("p (k m) -> p k m", m=m)
        groups = [[0, 1], [2, 3], [4, 5], [6, 7]]
        with ExitStack() as st:
            pools = self.mk_pools(st, xin=False)
            Wf, bWf = self.load_w(st, "Wf", [128, 8, 4], r3("wK_f", 4))
            Wsbk, bWsbk = self.load_w(st, "Wsbk", [128, 8, 256], r3("wK_sbk", 256))
            Wfxk, bWfxk = self.load_w(st, "Wfxk", [128, 8, 256], r3("wK_fxk", 256))
            Wv2, bWv2 = self.load_w(st, "Wv2", [128, 8, 512], r3("wK_v2", 512))
            Wckv, bWckv = self.load_w(st, "Wckv", [128, 8, 128], r3("wK_ckv", 128))
            Wkr, bWkr = self.load_w(st, "Wkr", [128, 8, 128], r3("wK_kr", 128))
            Wkrr, bWkrr = self.load_w(st, "Wkrr", [128, 8, 128], r3("wK_krr", 128))
            S.op("dve", lambda e: e.tensor_scalar_mul(out=Wkrr[:, :, 64:80], in0=Wkrr[:, :, 64:80], scalar1=-1.0),
                 [bWkrr], [bWkrr])
            ukf, bukf = self.load_w(st, "ukf", [128, 512], None, cast=False, skip=True)
            kvn, bkvn = self.load_w(st, "kvn", [128, 1], W["kvn"].ap(), cast=False)
            P.dma("sp", "w_ukf", ukf[:, 0:256], W["w_ukv_k"].ap(), [], [bukf])
            P.dma("sp", "w_ukf", ukf[:, 256:512], W["w_ukv_v"].ap(), [], [bukf])
            Wukv = self.sb(st, "Wukv", [128, 512], BF16)
            bWukv = Buf("Wukv")
            P.ts("dve", Wukv[:], ukf[:], kvn[:, 0:1], None, ALU.mult, None, [bukf, bkvn], [bWukv])
            bfg, bbfg = self.load_w(st, "bfg", [128, 4], W["bfg"].ap().partition_broadcast(128), cast=False)
            ckvn_p = self.pool(st, "ckvn", [128, 1, 512], BF16, 2)
            kst_p = self.pool(st, "kst", [128, 4, 512], BF16, 2)
            krst_p = self.pool(st, "krst", [128, 512], BF16, 2)
            vst_p = self.pool(st, "vst", [128, 4, 256], BF16, 2)
            vst2_p = self.pool(st, "vst2", [128, 4, 512], BF16, 2)
            sm_p = self.pool(st, "smallK", [128, 16], F32, 4)
            spown = self.sb(st, "spown", [128, 128], F32)
            bspo = Buf("spown")
            S.op("pool", lambda e: e.memset(spown[:], 0.0), [], [bspo])
            S.op("pool", lambda e: e.memset(cqa[:], 0.0), [], [cqab])
            f32p = pools["f32"]
            sl = slice(64, 96)
            xin8 = {"xin": self.pool(st, "xin", [128, 1024], F32, 8)}
            for s in range(4):
                self.transpose_tiles(xin8, [xloc_rows(s * 4 + t) for t in range(4)], xlT, actTb[s], s * 512)
            for s in range(4):
                cols = slice(s * 512, (s + 1) * 512)
                xTb = actTb[s]
                xT = lambda dc, a=0, b=512, s=s: xlT[:, dc, s * 512 + a:s * 512 + b]
                KV, bKV = X["KVx"][s], X["bKVx"][s]
                VX, bVX = X["Vx"][s], X["bVx"][s]
                for t in range(4):
                    lt = s * 4 + t
                    ps, pb = self.ps_next()
                    for dc in range(8):
                        P.mm(ps[:, 0:4], xT(dc, t * 128, (t + 1) * 128), Wf[:, dc, :], dc == 0, dc == 7, [xTb, bWf], [pb])
                    j, sm, smb = sm_p.next()
                    P.tt("dve", sm[:, 0:4], ps[:, 0:4], bfg[:], ALU.add, [pb, bbfg], [smb])
                    P.act(sm[:, 4:8], sm[:, 0:4], AF.Exp, [smb], [smb], scale=-1.0)
                    P.act(spown[:, lt * 4:(lt + 1) * 4], sm[:, 4:8], AF.Ln, [smb, bc], [bspo], bias=self.onec[:, 0:1])
                for (Wk, bWk, rbase) in ((Wsbk, bWsbk, 384), (Wfxk, bWfxk, 640)):
                    j, kst, kstb = kst_p.next()
                    for pr in range(2):
                        ps, pb = self.ps_next()
                        for dc in range(8):
                            P.mm(ps[:], Wk[:, dc, pr * 128:(pr + 1) * 128], xT(dc), dc == 0, dc == 7, [bWk, xTb], [pb])
                        P.cp(self.evac_eng(), kst[:, pr, :], ps[:], [pb], [kstb])
                        P.dma("sp", "kst%d" % j, KV.ap()[rbase + pr * 128:rbase + (pr + 1) * 128, :], kst[:, pr, :],
                              [kstb], [bKV])
                j, vst2, vst2b = vst2_p.next()
                for t in range(4):
                    ps, pb = self.ps_next()
                    for dc in range(8):
                        P.mm(ps[:], xT(dc, t * 128, (t + 1) * 128), Wv2[:, dc, :], dc == 0, dc == 7, [xTb, bWv2], [pb])
                    P.cp(self.evac_eng(), vst2[:, t, :], ps[:], [pb], [vst2b])
                P.dma("sp", "vst2a%d" % j, VX.ap()[:, 256:768].rearrange("(t p) c -> p t c", p=128), vst2[:], [vst2b], [bVX])
                ps, pb = self.ps_next()
                for dc in range(8):
                    P.mm(ps[:], Wckv[:, dc, :], xT(dc), dc == 0, dc == 7, [bWckv, xTb], [pb])
                j, ckvn, ckvnb = ckvn_p.next()
                self.rms_scale(pools, [ps], [pb], 128, ckvn, ckvnb)
                j, kst, kstb = kst_p.next()
                for h in range(4):
                    ps, pb = self.ps_next()
                    P.mm(ps[0:64, :], Wukv[:, h * 64:(h + 1) * 64], ckvn[:, 0, :], True, True, [bWukv, ckvnb], [pb])
                    P.cp(self.evac_eng(), kst[0:64, h, :], ps[0:64, :], [pb], [kstb])
                P.dma("sp", "kst%d" % j, KV.ap()[0:384, :].rearrange("(h p) t -> p h t", p=96)[0:64], kst[0:64, :, :],
                      [kstb], [bKV])
                psa, pba = self.ps_next()
                for dc in range(8):
                    P.mm(psa[:], Wkr[:, dc, :], xT(dc), dc == 0, dc == 7, [bWkr, xTb], [pba])
                psb_, pbb = self.ps_next()
                for dc in range(8):
                    P.mm(psb_[:], Wkrr[:, dc, :], xT(dc), dc == 0, dc == 7, [bWkrr, xTb], [pbb])
                j1, t1, t1b = f32p.next()
                j2, t2, t2b = f32p.next()
                P.tt("dve", t1[sl, :], psa[sl, :], self.cosL[sl, cols], ALU.mult, [pba, self.b_rope], [t1b])
                P.tt("dve", t2[sl, :], psb_[sl, :], self.sinL[sl, cols], ALU.mult, [pbb, self.b_rope], [t2b])
                j, krst, krstb = krst_p.next()
                P.tt("dve", krst[sl, :], t1[sl, :], t2[sl, :], ALU.add, [t1b, t2b], [krstb])
                for h in range(4):
                    P.dma("sp", "krst%d" % j, KV.ap()[h * 96 + 64:h * 96 + 96, :], krst[sl, :], [krstb], [bKV])
                j, vst, vstb = vst_p.next()
                for tp in range(2):
                    ps, pb = self.ps_next()
                    for t2_ in range(2):
                        t = tp * 2 + t2_
                        P.mm(ps[:, t2_ * 256:(t2_ + 1) * 256], ckvn[:, 0, t * 128:(t + 1) * 128], Wukv[:, 256:512],
                             True, True, [ckvnb, bWukv], [pb])
                    P.cp(self.evac_eng(), vst[:, tp * 2:tp * 2 + 2, :], ps[:].rearrange("p (t c) -> p t c", c=256), [pb], [vstb])
                P.dma("sp", "vst%d" % j, VX.ap()[:, 0:256].rearrange("(t p) c -> p t c", p=128), vst[:], [vstb], [bVX])
                for (a_, ba_, o_, bo_) in ((KV, bKV, X["KVg"][s], X["bKVg"][s]), (VX, bVX, X["Vg"][s], X["bVg"][s])):
                    S.dma("pool", "ccKV%d" % s,
                          lambda e, a_=a_, o_=o_: e.collective_compute("AllGather", ALU.bypass, replica_groups=groups,
                                                                       ins=[a_.ap().opt()], outs=[o_.ap().opt()]),
                          [ba_], [X["bKVg"][s], X["bVg"][s]], inc=1)
            P.dma("sp", "spx", X["spx"].ap(), spown[:], [bspo], [X["bspx"]])
            S.dma("pool", "ccS",
                  lambda e: e.collective_compute("AllGather", ALU.bypass, replica_groups=groups,
                                                 ins=[X["spx"].ap().opt()], outs=[X["spg"].ap().opt()]),
                  [X["bspx"]], [X["bspg"]], inc=1)
            S.barrier()

    def stage_C(self, l, negcK, negcKb, cqa, cqab):
        S, P = self.S, self
        bc = self.b_const
        X, dr, db = self.xch, self.dr, self.dbuf
        with ExitStack() as st:
            spall = self.sb(st, "spall", [128, 32 * 4], F32)
            bsp = Buf("spall")
            for s in range(4):
                for r in range(2):
                    g = G_PAR[r][s]
                    P.dma("sp", "spall", spall[:, g * 16:(g + 1) * 16], X["spg"].ap()[r * 128:(r + 1) * 128, s * 16:(s + 1) * 16],
                          [X["bspg"]], [bsp])
            import os
            cumb = os.environ.get("CUMB", "1") == "1"
            if cumb:
                spre = self.sb(st, "spre", [128, 32 * 4], F32)
                bspre = Buf("spre")
                S.op("dve", lambda e: e.memset(spre[:], 0.0), [], [bspre])
                for gt in range(31):
                    P.tt("dve", spre[:, (gt + 1) * 4:(gt + 2) * 4], spre[:, gt * 4:(gt + 1) * 4],
                         spall[:, gt * 4:(gt + 1) * 4], ALU.add, [bspre, bsp], [bspre])
                ps2, pb2 = self.ps_next()
                P.mm(ps2[:, 0:128], self.UT[:], spall[:], True, False, [bc, bsp], [pb2])
                P.mm(ps2[:, 0:128], self.ones_f[:], spre[:], False, True, [bc, bspre], [pb2])
                P.cp("dve", negcK[:], ps2[:, 0:128], [pb2], [negcKb])
            for g in range(8):
                for t in range(4):
                    if cumb:
                        break
                    gt = g * 4 + t
                    ps2, pb2 = self.ps_next()
                    P.mm(ps2[:, 0:4], self.UT[:], spall[:, gt * 4:(gt + 1) * 4], True, gt == 0, [bc, bsp], [pb2])
                    for tp_ in range(gt):
                        P.mm(ps2[:, 0:4], self.ones_f[:], spall[:, tp_ * 4:(tp_ + 1) * 4], False, tp_ == gt - 1,
                             [bc, bsp], [pb2])
                    P.cp("dve", negcK[:, gt * 4:(gt + 1) * 4], ps2[:, 0:4], [pb2], [negcKb])
                par = 0 if g in G_PAR[0] else 1
                s_ = G_PAR[par].index(g)
                ps, pb = self.ps_next()
                for t in range(4):
                    gt = g * 4 + t
                    P.mm(ps[0:4, t * 128:(t + 1) * 128], negcK[:, gt * 4:(gt + 1) * 4], self.ident[:], True, True,
                         [negcKb, bc], [pb])
                P.stt("dve", cqa[:, s_ * 512:(s_ + 1) * 512], ps[0:4, :], self.sel8[0:4, par:par + 1],
                      cqa[:, s_ * 512:(s_ + 1) * 512], ALU.mult, ALU.add, [pb, bc, cqab], [cqab])
            S.barrier()


    def stage_K(self, l, xfull_rows, W, negcK, negcKb, cqa, cqab):
        S, P = self.S, self
        bc = self.b_const
        dr = self.dr
        db = self.dbuf
        r3 = lambda name, m: W[name].ap().rearrange("p (k m) -> p k m", m=m)
        with ExitStack() as st:
            pools = self.mk_pools(st)
            RT = self.rope_tmp(st, 512)
            cosg_p = self.pool(st, "cosg", [128, 512], BF16, 2)
            sing_p = self.pool(st, "sing", [128, 512], BF16, 2)
            Wf, bWf = self.load_w(st, "Wf", [128, 8, 4], r3("wK_f", 4))
            Wsbk, bWsbk = self.load_w(st, "Wsbk", [128, 8, 256], r3("wK_sbk", 256))
            Wfxk, bWfxk = self.load_w(st, "Wfxk", [128, 8, 256], r3("wK_fxk", 256))
            Wv2, bWv2 = self.load_w(st, "Wv2", [128, 8, 512], r3("wK_v2", 512))
            Wckv, bWckv = self.load_w(st, "Wckv", [128, 8, 128], r3("wK_ckv", 128))
            Wkr, bWkr = self.load_w(st, "Wkr", [128, 8, 128], r3("wK_kr", 128))
            Wkrr, bWkrr = self.load_w(st, "Wkrr", [128, 8, 128], r3("wK_krr", 128))
            S.op("dve", lambda e: e.tensor_scalar_mul(out=Wkrr[:, :, 64:80], in0=Wkrr[:, :, 64:80], scalar1=-1.0),
                 [bWkrr], [bWkrr])
            ukf, bukf = self.load_w(st, "ukf", [128, 512], None, cast=False, skip=True)
            kvn, bkvn = self.load_w(st, "kvn", [128, 1], W["kvn"].ap(), cast=False)
            P.dma("sp", "w_ukf", ukf[:, 0:256], W["w_ukv_k"].ap(), [], [bukf])
            P.dma("sp", "w_ukf", ukf[:, 256:512], W["w_ukv_v"].ap(), [], [bukf])
            Wukv = self.sb(st, "Wukv", [128, 512], BF16)
            bWukv = Buf("Wukv")
            P.ts("dve", Wukv[:], ukf[:], kvn[:, 0:1], None, ALU.mult, None, [bukf, bkvn], [bWukv])
            bfg, bbfg = self.load_w(st, "bfg", [128, 4], W["bfg"].ap().partition_broadcast(128), cast=False)
            xTg = self.pool(st, "xTg", [128, 8, 512], BF16, 2)
            ckvn_p = self.pool(st, "ckvn", [128, 1, 512], BF16, 2)
            kst_p = self.pool(st, "kst", [128, 4, 512], BF16, 2)
            krst_p = self.pool(st, "krst", [128, 512], BF16, 2)
            vst_p = self.pool(st, "vst", [128, 4, 256], BF16, 2)
            vst2_p = self.pool(st, "vst2", [128, 4, 512], BF16, 2)
            sm_p = self.pool(st, "smallK", [128, 16], F32, 4)
            spall = self.sb(st, "spall", [128, 32 * 4], F32)
            bsp = [Buf("sp%d" % g_) for g_ in range(8)]
            S.op("pool", lambda e: e.memset(cqa[:], 0.0), [], [cqab])
            f32p = pools["f32"]
            import os
            ksub = int(os.environ.get("KSUB", "9"))
            for g in range(8):
                cols = slice(g * 512, (g + 1) * 512)
                rows = xfull_rows(g)
                j, xT, xTb = xTg.next()
                self.transpose_tiles(pools, [rows[t * 128:(t + 1) * 128, :] for t in range(4)], xT, xTb, 0)
                for t in range(4):
                    gt = g * 4 + t
                    ps, pb = self.ps_next()
                    for dc in range(8):
                        P.mm(ps[:, 0:4], xT[:, dc, t * 128:(t + 1) * 128], Wf[:, dc, :], dc == 0, dc == 7,
                             [xTb, bWf], [pb])
                    j, sm, smb = sm_p.next()
                    P.tt("dve", sm[:, 0:4], ps[:, 0:4], bfg[:], ALU.add, [pb, bbfg], [smb])
                    P.act(sm[:, 4:8], sm[:, 0:4], AF.Exp, [smb], [smb], scale=-1.0)
                    P.act(spall[:, gt * 4:(gt + 1) * 4], sm[:, 4:8], AF.Ln, [smb, bc], [bsp[g]], bias=self.onec[:, 0:1])
                for (Wk, bWk, name) in ((Wsbk, bWsbk, "KTs"), (Wfxk, bWfxk, "KTf")):
                    j, kst, kstb = kst_p.next()
                    for pr in range(2):
                        ps, pb = self.ps_next()
                        for dc in range(8):
                            P.mm(ps[:], Wk[:, dc, pr * 128:(pr + 1) * 128], xT[:, dc, :], dc == 0, dc == 7,
                                 [bWk, xTb], [pb])
                        P.cp(self.evac_eng(), kst[:, pr, :], ps[:], [pb], [kstb])
                        P.dma("sp", "kst%d" % j, dr[name].ap()[2 * pr:2 * pr + 2, :, cols].rearrange("h p t -> (h p) t"),
                              kst[:, pr, :], [kstb], [db[name]])
                j, vst2, vst2b = vst2_p.next()
                for t in range(4):
                    ps, pb = self.ps_next()
                    for dc in range(8):
                        P.mm(ps[:], xT[:, dc, t * 128:(t + 1) * 128], Wv2[:, dc, :], dc == 0, dc == 7, [xTb, bWv2], [pb])
                    P.cp(self.evac_eng(), vst2[:, t, :], ps[:], [pb], [vst2b])
                P.dma("sp", "vst2a%d" % j, dr["Vs"].ap()[cols, :].rearrange("(t p) c -> p t c", p=128),
                      vst2[:, :, 0:256], [vst2b], [db["Vs"]])
                P.dma("sp", "vst2b%d" % j, dr["Vf"].ap()[cols, :].rearrange("(t p) c -> p t c", p=128),
                      vst2[:, :, 256:512], [vst2b], [db["Vf"]])
                ps, pb = self.ps_next()
                for dc in range(8):
                    P.mm(ps[:], Wckv[:, dc, :], xT[:, dc, :], dc == 0, dc == 7, [bWckv, xTb], [pb])
                j, ckvn, ckvnb = ckvn_p.next()
                self.rms_scale(pools, [ps], [pb], 128, ckvn, ckvnb)
                j, kst, kstb = kst_p.next()
                for h in range(4):
                    ps, pb = self.ps_next()
                    P.mm(ps[0:64, :], Wukv[:, h * 64:(h + 1) * 64], ckvn[:, 0, :], True, True, [bWukv, ckvnb], [pb])
                    P.cp(self.evac_eng(), kst[0:64, h, :], ps[0:64, :], [pb], [kstb])
                P.dma("sp", "kst%d" % j, dr["KTm"].ap()[:, 0:64, cols].rearrange("h p t -> p h t"), kst[0:64, :, :],
                      [kstb], [db["KTm"]])
                psa, pba = self.ps_next()
                for dc in range(8):
                    P.mm(psa[:], Wkr[:, dc, :], xT[:, dc, :], dc == 0, dc == 7, [bWkr, xTb], [pba])
                psb_, pbb = self.ps_next()
                for dc in range(8):
                    P.mm(psb_[:], Wkrr[:, dc, :], xT[:, dc, :], dc == 0, dc == 7, [bWkrr, xTb], [pbb])
                j1, t1, t1b = f32p.next()
                j2, t2, t2b = f32p.next()
                sl = slice(64, 96)
                jc, cosg, cosgb = cosg_p.next()
                js, sing, singb = sing_p.next()
                self.rope_tables(RT, self.pos_full.ap()[:, cols], 512, cosg[sl, :], sing[sl, :], cosgb)
                P.tt("dve", t1[sl, :], psa[sl, :], cosg[sl, :], ALU.mult, [pba, cosgb], [t1b])
                P.tt("dve", t2[sl, :], psb_[sl, :], sing[sl, :], ALU.mult, [pbb, cosgb], [t2b])
                j, krst, krstb = krst_p.next()
                P.tt("dve", krst[sl, :], t1[sl, :], t2[sl, :], ALU.add, [t1b, t2b], [krstb])
                for h in range(4):
                    P.dma("sp", "krst%d" % j, dr["KTm"].ap()[h, 64:96, cols], krst[sl, :], [krstb], [db["KTm"]])
                j, vst, vstb = vst_p.next()
                for tp in range(2):
                    ps, pb = self.ps_next()
                    for t2_ in range(2):
                        t = tp * 2 + t2_
                        P.mm(ps[:, t2_ * 256:(t2_ + 1) * 256], ckvn[:, 0, t * 128:(t + 1) * 128], Wukv[:, 256:512],
                             True, True, [ckvnb, bWukv], [pb])
                    P.cp(self.evac_eng(), vst[:, tp * 2:tp * 2 + 2, :],
                         ps[:].rearrange("p (t c) -> p t c", c=256), [pb], [vstb])
                P.dma("sp", "vst%d" % j, dr["Vm"].ap()[cols, :].rearrange("(t p) c -> p t c", p=128), vst[:],
                      [vstb], [db["Vm"]])
                for t in range(4):
                    gt = g * 4 + t
                    ps2, pb2 = self.ps_next()
                    P.mm(ps2[:, 0:4], self.UT[:], spall[:, gt * 4:(gt + 1) * 4], True, gt == 0, [bc, bsp[g]], [pb2])
                    for tp_ in range(gt):
                        P.mm(ps2[:, 0:4], self.ones_f[:], spall[:, tp_ * 4:(tp_ + 1) * 4], False, tp_ == gt - 1,
                             [bc, bsp[tp_ // 4]], [pb2])
                    P.cp("dve", negcK[:, gt * 4:(gt + 1) * 4], ps2[:, 0:4], [pb2], [negcKb])
                par = 0 if g in G_PAR[0] else 1
                s_ = G_PAR[par].index(g)
                ps, pb = self.ps_next()
                for t in range(4):
                    gt = g * 4 + t
                    P.mm(ps[0:4, t * 128:(t + 1) * 128], negcK[:, gt * 4:(gt + 1) * 4], self.ident[:], True, True,
                         [negcKb, bc], [pb])
                P.stt("dve", cqa[:, s_ * 512:(s_ + 1) * 512], ps[0:4, :], self.sel8[0:4, par:par + 1],
                      cqa[:, s_ * 512:(s_ + 1) * 512], ALU.mult, ALU.add, [pb, bc, cqab], [cqab])
            S.barrier()

    def stage_Q(self, l, xfull_rows, xloc_rows, W, actT, actTb, yT, yTb, negcK, negcKb, cqa, cqab):
        S, P = self.S, self
        bc = self.b_const
        dr, db = self.dr, self.dbuf
        r3 = lambda name, m: W[name].ap().rearrange("p (k m) -> p k m", m=m)
        xlT = actT
        if self.kv_exchange:
            self.stage_K2(l, W, xlT, actTb, cqa, cqab, xloc_rows)
        else:
            with ExitStack() as st:
                pools = self.mk_pools(st)
                for s in range(4):
                    self.transpose_tiles(pools, [xloc_rows(s * 4 + t) for t in range(4)], actT, actTb[s], s * 512)
                S.barrier()

        with ExitStack() as st:
            pools = self.mk_pools(st, xin=False)
            f32p, bfp = pools["f32"], pools["bf"]
            Wcv, bWcv = self.load_w(st, "Wcv", [128, 8, 512], r3("wQ_cv", 512))
            cw, bcw = self.load_w(st, "convw", [128, 2, 31], W["convw"].ap().rearrange("p (c j) -> p c j", j=31), cast=False)
            cb_, bcb = self.load_w(st, "convb", [128, 2], W["convb"].ap(), cast=False)
            cg, bcg = self.load_w(st, "clng", [128, 2], W["clng"].ap(), cast=False)
            cbt, bcbt = self.load_w(st, "clnb", [128, 2], W["clnb"].ap(), cast=False)
            dg = self.sb(st, "dg", [128, 2, 31, 128], BF16)
            bdg = Buf("dg")
            for cc in range(2):
                for j in range(31):
                    P.ts("dve", dg[:, cc, j, :], self.ident[:], cw[:, cc, j:j + 1], None, ALU.mult, None,
                         [bc, bcw], [bdg])
            hp = self.sb(st, "hp", [128, 2, 4, 544], BF16)
            bhp = [Buf("hp%d" % s) for s in range(4)]
            bhalo = Buf("halo")
            xa = self.sb(st, "xha", [128, 1024], F32)
            xb = self.sb(st, "xhb", [128, 1024], F32)
            bxa, bxb = Buf("xa"), Buf("xb")
            S.op("pool", lambda e: e.memset(xa[0:32, :], 0.0), [], [bxa])
            for s in range(4):
                ga, gb = G_PAR[0][s] - 1, G_PAR[1][s] - 1
                xfb = getattr(self, "xfull_bufs", None) or (lambda g_: [])
                if ga >= 0:
                    P.dma("sp", "xha", xa[s * 32:(s + 1) * 32, :], xfull_rows(ga)[480:512, :], xfb(ga), [bxa])
                P.dma("sp", "xhb", xb[s * 32:(s + 1) * 32, :], xfull_rows(gb)[480:512, :], xfb(gb), [bxb])
            P.ts("dve", xa[:], xa[:], self.sel[:, 0:1], None, ALU.mult, None, [bxa, bc], [bxa])
            P.stt("dve", xa[:], xb[:], self.sel[:, 1:2], xa[:], ALU.mult, ALU.add, [bxb, bc, bxa], [bxa])
            xhT = self.sb(st, "xhT", [128, 8, 128], BF16)
            bxhT = Buf("xhT")
            for dc in range(8):
                ps, pb = self.ps_next()
                P.tr(ps[:, 0:128], xa[:, dc * 128:(dc + 1) * 128], [bxa], [pb])
                P.cp(self.evac_eng(), xhT[:, dc, :], ps[:, 0:128], [pb], [bxhT])

            def glu(cc, rhs_fn, ncols, rbufs, out_ap, out_buf):
                psa, pba = self.ps_next()
                for dc in range(8):
                    P.mm(psa[:, 0:ncols], Wcv[:, dc, cc * 128:(cc + 1) * 128], rhs_fn(dc), dc == 0, dc == 7,
                         [bWcv] + rbufs, [pba])
                psg, pbg = self.ps_next()
                for dc in range(8):
                    P.mm(psg[:, 0:ncols], Wcv[:, dc, 256 + cc * 128:256 + (cc + 1) * 128], rhs_fn(dc), dc == 0, dc == 7,
                         [bWcv] + rbufs, [pbg])
                j, sg, sgb = f32p.next()
                P.act(sg[:, 0:ncols], psg[:, 0:ncols], AF.Sigmoid, [pbg], [sgb])
                P.tt("dve", out_ap, psa[:, 0:ncols], sg[:, 0:ncols], ALU.mult, [pba, sgb], [out_buf])

            for cc in range(2):
                j, hh, hhb = bfp.next()
                glu(cc, lambda dc: xhT[:, dc, :], 128, [bxhT], hh[:, 0:128], hhb)
                P.cp("dve", hp[:, cc, :, 0:32], hh[:, 0:128].rearrange("p (s t) -> p s t", t=32), [hhb], bhp)
                for s in range(4):
                    glu(cc, lambda dc, s=s: xlT[:, dc, s * 512:(s + 1) * 512], 512, [actTb[s]],
                        hp[:, cc, s, 32:544], bhp[s])
            for s in range(4):
                ys = []
                for cc in range(2):
                    ps, pb = self.ps_next(0, 4)
                    for j in range(31):
                        P.mm(ps[:], dg[:, cc, j, :], hp[:, cc, s, 2 + j:2 + j + 512], j == 0, j == 30, [bdg, bhp[s]], [pb])
                    jj, y, yb = f32p.next()
                    P.ts("dve", y[:], ps[:], cb_[:, cc:cc + 1], None, ALU.add, None, [pb, bcb], [yb])
                    ys.append((y, yb))
                pss, pbs = self.ps_next(4, 8)
                psq, pbq = self.ps_next(4, 8)
                for cc, (y, yb) in enumerate(ys):
                    j1, ybf, ybfb = bfp.next()
                    j2, ysq, ysqb = bfp.next()
                    P.cp("dve", ybf[:], y[:], [yb], [ybfb])
                    P.act(ysq[:], y[:], AF.Square, [yb], [ysqb])
                    P.mm(pss[:], self.ones_bf[:], ybf[:], cc == 0, cc == 1, [bc, ybfb], [pbs])
                    P.mm(psq[:], self.ones_bf[:], ysq[:], cc == 0, cc == 1, [bc, ysqb], [pbq])
                jm, mean, meanb = f32p.next()
                P.act(mean[:], pss[:], AF.Copy, [pbs], [meanb], scale=1.0 / 256)
                jv, var, varb = f32p.next()
                P.act(var[:], mean[:], AF.Square, [meanb], [varb])
                P.stt("dve", var[:], psq[:], 1.0 / 256, var[:], ALU.mult, ALU.subtract, [pbq, varb], [varb])
                P.act(var[:], var[:], AF.Sqrt, [varb, bc], [varb], bias=self.epsc[:, 0:1])
                S.op("dve", lambda e, o=var[:]: e.reciprocal(out=o, in_=o), [varb], [varb])
                for cc, (y, yb) in enumerate(ys):
                    P.tt("dve", y[:], y[:], mean[:], ALU.subtract, [yb, meanb], [yb])
                    P.tt("dve", y[:], y[:], var[:], ALU.mult, [yb, varb], [yb])
                    P.act(yT[2][:, cc, s * 512:(s + 1) * 512], y[:], AF.Silu, [yb, bcg, bcbt], [yTb[2][s]],
                          bias=cbt[:, cc:cc + 1], scale=cg[:, cc:cc + 1])
            S.barrier()

        if self.kv_exchange:
            self.stage_C(l, negcK, negcKb, cqa, cqab)
        import os
        if int(os.environ.get("KSTOP", "9")) < 3:
            return
        self.attn_mla(l, W, xlT, actTb, yT[0], yTb[0])
        self.attn_sbfox(l, W, xlT, actTb, yT[1], yTb[1], "sb", None, None, None, None)
        self.attn_sbfox(l, W, xlT, actTb, yT[3], yTb[3], "fox", negcK, negcKb, cqa, cqab)

    def stage_G(self, l, W, xlT, actTb, yT, yTb, mrgT, mrgTb):
        S, P = self.S, self
        with ExitStack() as st:
            pools = self.mk_pools(st, xin=False)
            f32p = pools["f32"]
            Wb, bWb = self.load_w(st, "Wb", [128, 4, 2, 1024], W["wB"].ap().rearrange("p (n c d) -> p n c d", n=4, c=2))
            bg, bbg = self.load_w(st, "bg", [128, 32], W["bg"].ap(), cast=False)
            wg_p = self.pool(st, "wg", [128, 8, 128], BF16, 3)
            acc = self.sb(st, "gacc", [128, 4, 512], F32)
            baccs = [Buf("gacc%d" % s) for s in range(4)]
            for dc in range(8):
                for n in range(4):
                    ci = dc * 4 + n
                    j, wg, wgb = wg_p.next()
                    P.dma("pool", "wg%d" % j, wg[:], W["wG"].ap()[ci].rearrange("p (k m) -> p k m", m=128), [], [wgb])
                    for s in range(4):
                        cols = slice(s * 512, (s + 1) * 512)
                        psg, pbg = self.ps_next()
                        for kc in range(8):
                            P.mm(psg[:], wg[:, kc, :], xlT[:, kc, cols], kc == 0, kc == 7, [wgb, actTb[s]], [pbg])
                        jg, gs_, gsb = f32p.next()
                        P.act(gs_[:], psg[:], AF.Sigmoid, [pbg, bbg], [gsb], bias=bg[:, ci:ci + 1])
                        psp, pbp = self.ps_next()
                        for cc in range(2):
                            P.mm(psp[:], Wb[:, n, cc, dc * 128:(dc + 1) * 128], yT[n][:, cc, cols], cc == 0, cc == 1,
                                 [bWb, yTb[n][s]], [pbp])
                        if n == 0:
                            P.tt("dve", acc[:, s, :], gs_[:], psp[:], ALU.mult, [gsb, pbp], [baccs[s]])
                        else:
                            P.tt("dve", gs_[:], gs_[:], psp[:], ALU.mult, [gsb, pbp], [gsb])
                            if n < 3:
                                P.tt("dve", acc[:, s, :], acc[:, s, :], gs_[:], ALU.add, [baccs[s], gsb], [baccs[s]])
                            else:
                                P.tt("dve", mrgT[:, dc, cols], acc[:, s, :], gs_[:], ALU.add, [baccs[s], gsb], [mrgTb[s]])
            S.barrier()

    def make_pen(self, st, pools, strict):
        S, P = self.S, self
        bc = self.b_const
        Dm = self.D1 if strict else self.D0
        penb = self.sb(st, "penb", [128, 16, 512], BF16)
        penbb = Buf("penb")
        f32p = pools["f32"]
        for i in range(16):
            jt, tmp, tmpb = f32p.next()
            P.ts("dve", tmp[:], Dm[:], self.thr[:, i:i + 1], 0.0, ALU.subtract, ALU.max, [bc], [tmpb])
            P.ts("dve", penb[:, i, :], tmp[:], NEGBIG, None, ALU.mult, None, [tmpb], [penbb])
        return penb, penbb

    def attn_core(self, kind, pools, KT, KTb, kdim, QT, QTb, qtn_p, Vt, Vtb, h, s, scale, yTn, yTnb, penb, penbb,
                  negcK=None, negcKb=None, rs_p=None):
        S, P = self.S, self
        bc = self.b_const
        f32p, bfp = pools["f32"], pools["bf"]
        NB = 8 * (s + 1)
        qc = slice(s * 512, (s + 1) * 512)
        cls = s % 2
        hp = h // 2
        acc_o, bo = self.ps_next(4, 8)
        order = list(range(NB))
        QTn = QTnb = None
        if kind == "sb":
            order = order[::-1]
            jq, QTn, QTnb = qtn_p.next()
            P.ts("dve", QTn[0:kdim, :], QT[0:kdim, h, qc], -0.125, None, ALU.mult, None, [QTb[s]], [QTnb])
        rs_state = [None]
        pv_pend = []

        def stage1(i):
            kb = order[i]
            kc = slice(kb * 128, (kb + 1) * 128)
            masked = kb >= 8 * s
            ps, pb = self.ps_next(0, 4)
            P.mm(ps[:], KT[0:kdim, kc], QT[0:kdim, h, qc], True, True, [KTb, QTb[s]], [pb])
            src, srcb = ps, pb
            pj = None
            if masked:
                pj = cls * 8 + (kb - 8 * s)
                jz, zm, zmb = f32p.next()
                P.tt("dve", zm[:], ps[:], penb[:, pj, :], ALU.add, [pb, penbb], [zmb])
                src, srcb = zm, zmb
            if kind != "sb":
                jP, Pt, Ptb = bfp.next()
                if kind == "fox":
                    P.act(Pt[:], src[:], AF.Exp, [srcb, negcKb], [Ptb], scale=scale,
                          bias=negcK[:, kb * 4 + h:kb * 4 + h + 1])
                else:
                    P.act(Pt[:], src[:], AF.Exp, [srcb], [Ptb], scale=scale)
                return (kb, kc, pj, Pt, Ptb)
            je, ee, eeb = f32p.next()
            P.act(ee[:], src[:], AF.Exp, [srcb], [eeb], scale=scale)
            jsp, sp, spb = bfp.next()
            P.act(sp[:], ee[:], AF.Ln, [eeb, bc], [spb], bias=self.onec[:, 0:1])
            return (kb, kc, pj, sp, spb)

        def stage2(i, st1):
            kb, kc, pj, t, tb = st1
            first, last = (i == 0), (i == NB - 1)
            if kind != "sb":
                P.mm(acc_o[:], Vt[:, kb, :], t[:], first, last, [Vtb, tb], [bo])
                return
            sp, spb = t, tb
            prev_rs = rs_state[0]
            psG, pbG = self.ps_next(0, 4)
            P.mm(psG[:], self.LT[:], sp[:], True, False, [bc, spb], [pbG])
            if prev_rs is not None:
                P.mm(psG[:], self.ones_bf[:], prev_rs[0][:], False, False, [bc, prev_rs[1]], [pbG])
            P.mm(psG[:], KT[0:kdim, kc], QTn[0:kdim, :], False, True, [KTb, QTnb], [pbG])
            srcG, srcGb = psG, pbG
            if pj is not None:
                jg, gm, gmb = f32p.next()
                P.tt("dve", gm[:], psG[:], penb[:, pj, :], ALU.subtract, [pbG, penbb], [gmb])
                srcG, srcGb = gm, gmb
            jA, At, Atb = bfp.next()
            P.act(At[:], srcG[:], AF.Exp, [srcGb], [Atb], scale=-1.0)
            pv_pend.append((kb, At, Atb, first, last))
            if not last:
                jr, rs, rsb = rs_p.next()
                if prev_rs is None:
                    P.cp("dve", rs[:], sp[:], [spb], [rsb])
                else:
                    P.tt("dve", rs[:], prev_rs[0][:], sp[:], ALU.add, [prev_rs[1], spb], [rsb])
                rs_state[0] = (rs, rsb)

        def stage3():
            kb, At, Atb, first, last = pv_pend.pop(0)
            P.mm(acc_o[:], Vt[:, kb, hp * 128:(hp + 1) * 128], At[:], first, last, [Vtb, Atb], [bo])

        DEPTH_PIPE = 2 if kind != "sb" else 1
        pend = []
        for i in range(NB):
            pend.append((i, stage1(i)))
            if len(pend) > DEPTH_PIPE:
                i0, st0 = pend.pop(0)
                stage2(i0, st0)
            if len(pv_pend) > 1:
                stage3()
        for i0, st0 in pend:
            stage2(i0, st0)
        while pv_pend:
            stage3()
        po = (h % 2) * 64
        dst = yTn[po:po + 64, h // 2, qc]
        if kind == "sb":
            P.cp("dve", dst, acc_o[po:po + 64, :], [bo], [yTnb[s]])
        else:
            jr, rec, recb = f32p.next()
            S.op("dve", lambda e, o=rec[0:64, :], i=acc_o[64:128, :]: e.reciprocal(out=o, in_=i), [bo], [recb])
            P.tt("dve", dst, acc_o[0:64, :], rec[0:64, :], ALU.mult, [bo, recb], [yTnb[s]])

    def kv_src(self, kind, h, g):
        X = self.xch
        r = 0 if g in G_PAR[0] else 1
        s_ = G_PAR[r].index(g)
        base, kd = {"mla": (0, 96), "sb": (384, 64), "fox": (640, 64)}[kind]
        r0 = r * 896 + base + h * kd
        return X["KVg"][s_].ap()[r0:r0 + kd, :], X["bKVg"][s_]

    def v_src(self, kind, g, c0, c1):
        X = self.xch
        r = 0 if g in G_PAR[0] else 1
        s_ = G_PAR[r].index(g)
        cb = {"mla": 0, "sb": 256, "fox": 512}[kind]
        return (X["Vg"][s_].ap()[r * 512:(r + 1) * 512, cb + c0:cb + c1].rearrange("(t p) c -> p t c", p=128),
                X["bVg"][s_])

    def attn_mla(self, l, W, xlT, actTb, yTn, yTnb):
        S, P = self.S, self
        bc = self.b_const
        dr, db = self.dr, self.dbuf
        with ExitStack() as st:
            pools = self.mk_pools(st, xin=False)
            f32p = pools["f32"]
            Wcq, bWcq = self.load_w(st, "Wcq", [128, 8, 256], W["wQ_cq"].ap().rearrange("p (k m) -> p k m", m=256))
            qn, bqn = self.load_w(st, "qn", [128, 2], W["qn"].ap(), cast=False)
            Wuq = self.sb(st, "Wuq", [128, 2, 384], BF16)
            Wuqr = self.sb(st, "Wuqr", [128, 2, 384], BF16)
            bWuq, bWuqr = Buf("Wuq"), Buf("Wuqr")
            with ExitStack() as st2:
                uf, buf_ = self.load_w(st2, "uqf", [128, 2, 384], W["w_uq"].ap().rearrange("p (k m) -> p k m", m=384), cast=False)
                ur, bur = self.load_w(st2, "uqrf", [128, 2, 384], W["w_uqr"].ap().rearrange("p (k m) -> p k m", m=384), cast=False)
                for rc in range(2):
                    P.ts("dve", Wuq[:, rc, :], uf[:, rc, :], qn[:, rc:rc + 1], None, ALU.mult, None, [buf_, bqn], [bWuq])
                    P.ts("dve", Wuqr[:, rc, :], ur[:, rc, :], qn[:, rc:rc + 1], None, ALU.mult, None, [bur, bqn], [bWuqr])
                    for h in range(4):
                        o = h * 96 + 64
                        P.ts("dve", Wuqr[:, rc, o:o + 16], Wuqr[:, rc, o:o + 16], -1.0, None, ALU.mult, None,
                             [bWuqr], [bWuqr])
                S.barrier()
            QT = self.sb(st, "QTm", [128, 4, TL], BF16)
            QTb = [Buf("QTm%d" % s) for s in range(4)]
            cqn_p = self.pool(st, "cqn", [128, 2, 512], BF16, 2)
            sl = slice(64, 96)
            for s in range(4):
                qc = slice(s * 512, (s + 1) * 512)
                tc_ = slice(SEQ + s * 512, SEQ + (s + 1) * 512)
                pss, pbs = [], []
                for rc in range(2):
                    ps, pb = self.ps_next(0, 4)
                    for dc in range(8):
                        P.mm(ps[:], Wcq[:, dc, rc * 128:(rc + 1) * 128], xlT[:, dc, qc], dc == 0, dc == 7,
                             [bWcq, actTb[s]], [pb])
                    pss.append(ps)
                    pbs.append(pb)
                j, cqn, cqnb = cqn_p.next()
                self.rms_scale(pools, pss, pbs, 256, cqn, cqnb, ps_range=(4, 8))
                for h in range(4):
                    psa, pba = self.ps_next(4, 8)
                    psb_, pbb = self.ps_next(4, 8)
                    for rc in range(2):
                        P.mm(psa[0:96, :], Wuq[:, rc, h * 96:(h + 1) * 96], cqn[:, rc, :], rc == 0, rc == 1,
                             [bWuq, cqnb], [pba])
                    for rc in range(2):
                        P.mm(psb_[0:96, :], Wuqr[:, rc, h * 96:(h + 1) * 96], cqn[:, rc, :], rc == 0, rc == 1,
                             [bWuqr, cqnb], [pbb])
                    P.cp("act", QT[0:64, h, qc], psa[0:64, :], [pba], [QTb[s]])
                    j1, t1, t1b = f32p.next()
                    j2, t2, t2b = f32p.next()
                    P.tt("dve", t1[sl, :], psa[sl, :], self.cosL[sl, qc], ALU.mult, [pba, self.b_rope], [t1b])
                    P.tt("dve", t2[sl, :], psb_[sl, :], self.sinL[sl, qc], ALU.mult, [pbb, self.b_rope], [t2b])
                    P.tt("dve", QT[sl, h, qc], t1[sl, :], t2[sl, :], ALU.add, [t1b, t2b], [QTb[s]])
            vh_p = self.pool(st, "Vh", [128, 32, 128], BF16, 2)
            for (tile_, b_) in zip(vh_p.tiles, vh_p.bufs):
                S.op("pool", lambda e, t=tile_: e.memset(t[:, :, 64:128], 1.0), [], [b_])
            kt_p = self.pool(st, "KT", [128, SEQ], BF16, 2)
            penb, penbb = self.make_pen(st, pools, False)
            for h in range(4):
                j, KT, KTb = kt_p.next()
                jv, Vt, Vtb = vh_p.next()
                if self.kv_exchange:
                    for g in range(8):
                        ka, kb_ = self.kv_src("mla", h, g)
                        P.dma("sp", "KT%d" % j, KT[0:96, g * 512:(g + 1) * 512], ka, [kb_], [KTb])
                        va, vb_ = self.v_src("mla", g, h * 64, (h + 1) * 64)
                        P.dma("sp", "Vh%d" % jv, Vt[:, g * 4:(g + 1) * 4, 0:64], va, [vb_], [Vtb])
                else:
                    P.dma("sp", "KT%d" % j, KT[0:96, :], dr["KTm"].ap()[h], [db["KTm"]], [KTb])
                    for q8 in range(8):
                        P.dma("sp", "Vh%d" % jv, Vt[:, q8 * 4:(q8 + 1) * 4, 0:64],
                              dr["Vm"].ap()[q8 * 512:(q8 + 1) * 512, h * 64:(h + 1) * 64].rearrange("(t p) c -> p t c", p=128),
                              [db["Vm"]], [Vtb])
                for s in range(4):
                    self.attn_core("mla", pools, KT, KTb, 96, QT, QTb, None, Vt, Vtb, h, s, float(96 ** -0.5), yTn, yTnb,
                                   penb, penbb)
            S.barrier()

    def attn_sbfox(self, l, W, xlT, actTb, yTn, yTnb, kind, negcK, negcKb, cqa, cqab):
        S, P = self.S, self
        bc = self.b_const
        dr, db = self.dr, self.dbuf
        fox = kind == "fox"
        kdim = 65
        with ExitStack() as st:
            pools = self.mk_pools(st, xin=False)
            Wq = self.sb(st, "Wq" + kind, [128, 8, 4, 65], BF16)
            bWq = Buf("Wq")
            S.op("pool", lambda e: e.memset(Wq[:], 0.0), [], [bWq])
            P.dma("pool", "w_Wq", Wq[:, :, :, 0:64],
                  W["wQ_fxq" if fox else "wQ_sbq"].ap().rearrange("p (k h m) -> p k h m", h=4, m=64), [], [bWq])
            QT = self.sb(st, "QT" + kind, [128, 4, TL], BF16)
            QTb = [Buf("QT%d" % s) for s in range(4)]
            qtn_p = None if fox else self.pool(st, "QTn", [128, 512], BF16, 2)
            for s in range(4):
                qc = slice(s * 512, (s + 1) * 512)
                for h in range(4):
                    ps, pb = self.ps_next(0, 4)
                    for dc in range(8):
                        P.mm(ps[0:kdim, :], Wq[:, dc, h, 0:kdim], xlT[:, dc, qc], dc == 0, (dc == 7) and not fox,
                             [bWq, actTb[s]], [pb])
                    if fox:
                        P.mm(ps[0:kdim, :], self.Eh[0:4, h * 65:(h + 1) * 65], cqa[0:4, qc], False, True,
                             [bc, cqab], [pb])
                    P.cp("act", QT[0:kdim, h, qc], ps[0:kdim, :], [pb], [QTb[s]])
            vname, kname = ("Vf", "KTf") if fox else ("Vs", "KTs")
            if fox:
                vh_p = self.pool(st, "Vhf", [128, 32, 128], BF16, 2)
                for (tile_, b_) in zip(vh_p.tiles, vh_p.bufs):
                    S.op("pool", lambda e, t=tile_: e.memset(t[:, :, 64:128], 1.0), [], [b_])
            else:
                Vt = self.sb(st, "Vt" + kind, [128, 32, 256], BF16)
                Vtb = Buf("Vt")
                for q8 in range(8):
                    if self.kv_exchange:
                        va, vb_ = self.v_src("sb", q8, 0, 256)
                        P.dma("sp", "Vt", Vt[:, q8 * 4:(q8 + 1) * 4, :], va, [vb_], [Vtb])
                    else:
                        P.dma("sp", "Vt", Vt[:, q8 * 4:(q8 + 1) * 4, :],
                              dr[vname].ap()[q8 * 512:(q8 + 1) * 512, :].rearrange("(t p) c -> p t c", p=128),
                              [db[vname]], [Vtb])
            kt_p = self.pool(st, "KT" + kind, [128, SEQ], BF16, 2)
            for (tile_, b_) in zip(kt_p.tiles, kt_p.bufs):
                S.op("pool", lambda e, t=tile_: e.memset(t[64:65, :], 1.0 if fox else 0.0), [], [b_])
            rs_p = None if fox else self.pool(st, "rsum", [128, 512], BF16, 2)
            penb, penbb = self.make_pen(st, pools, not fox)
            for h in range(4):
                j, KT, KTb = kt_p.next()
                if self.kv_exchange:
                    for g in range(8):
                        ka, kb_ = self.kv_src(kind, h, g)
                        P.dma("sp", "KT%d" % j, KT[0:64, g * 512:(g + 1) * 512], ka, [kb_], [KTb])
                else:
                    P.dma("sp", "KT%d" % j, KT[0:64, :], dr[kname].ap()[h, 0:64, :], [db[kname]], [KTb])
                if fox:
                    jv, Vt, Vtb = vh_p.next()
                    for q8 in range(8):
                        if self.kv_exchange:
                            va, vb_ = self.v_src("fox", q8, h * 64, (h + 1) * 64)
                            P.dma("sp", "Vh%d" % jv, Vt[:, q8 * 4:(q8 + 1) * 4, 0:64], va, [vb_], [Vtb])
                        else:
                            P.dma("sp", "Vh%d" % jv, Vt[:, q8 * 4:(q8 + 1) * 4, 0:64],
                                  dr[vname].ap()[q8 * 512:(q8 + 1) * 512, h * 64:(h + 1) * 64].rearrange("(t p) c -> p t c", p=128),
                                  [db[vname]], [Vtb])
                for s in range(4):
                    self.attn_core(kind, pools, KT, KTb, kdim, QT, QTb, qtn_p, Vt, Vtb, h, s, 0.125, yTn, yTnb,
                                   penb, penbb, negcK=negcK, negcKb=negcKb, rs_p=rs_p)
            S.barrier()

    def ln_a(self, r, rb, smp, junk_p):
        S, P = self.S, self
        j, sm, smb = smp.next()
        jj, junk, junkb = junk_p.next()
        S.op("dve", lambda e, o=sm[:, 0:1], i=r[:]: e.reduce_sum(out=o, in_=i, axis=AX.X), [rb], [smb])
        P.ts("dve", sm[:, 1:2], sm[:, 0:1], -1.0 / D, None, ALU.mult, None, [smb], [smb])
        P.act(r[:], r[:], AF.Identity, [rb, smb], [rb], bias=sm[:, 1:2])
        P.act(junk[:], r[:], AF.Square, [rb], [junkb])
        return (sm, smb, junk, junkb)

    def ln_b(self, r, rb, gbc, bbc, gb_bufs, st_):
        S, P = self.S, self
        sm, smb, junk, junkb = st_
        S.op("dve", lambda e, o=sm[:, 2:3], i=junk[:]: e.reduce_sum(out=o, in_=i, axis=AX.X), [junkb], [smb])
        P.act(sm[:, 3:4], sm[:, 2:3], AF.Sqrt, [smb, self.b_const], [smb], bias=self.epsc[:, 0:1], scale=1.0 / D)
        S.op("dve", lambda e, o=sm[:, 4:5], i=sm[:, 3:4]: e.reciprocal(out=o, in_=i), [smb], [smb])
        P.stt("dve", r[:], r[:], sm[:, 4:5], gbc[:], ALU.mult, ALU.mult, [rb, smb] + gb_bufs, [rb])
        P.tt("pool", r[:], r[:], bbc[:], ALU.add, [rb] + gb_bufs, [rb])

    def stage_F1(self, l, xloc_rows, W, actT, actTb, mrgT, mrgTb, WtT, WtTb):
        S, P = self.S, self
        bc = self.b_const
        dr, db = self.dr, self.dbuf
        with ExitStack() as st:
            Wo, bWo = self.load_w(st, "Wo", [128, 8, 1024], W["wO"].ap().rearrange("p (k m) -> p k m", m=1024))
            g1, bg1 = self.load_w(st, "ln1g", [128, D], W["ln1_g"].ap().partition_broadcast(128), cast=False)
            b1, bb1 = self.load_w(st, "ln1b", [128, D], W["ln1_b"].ap().partition_broadcast(128), cast=False)
            wR, bwR = self.load_w(st, "wR", [128, 8, 36], W["wR"].ap().rearrange("p (k m) -> p k m", m=36), cast=False)
            bR, bbR = self.load_w(st, "bR", [128, 36], W["bR"].ap().partition_broadcast(128), cast=False)
            xr_p = self.pool(st, "xr", [128, D], F32, 3)
            r_p = self.pool(st, "r", [128, D], F32, 4)
            hTf_p = self.pool(st, "hTf", [128, 8, 128], F32, 3)
            sm_p = self.pool(st, "smF", [128, 128], F32, 4)
            sm2_p = self.pool(st, "smF2", [128, 64], F32, 4)
            smln = self.pool(st, "smln", [128, 8], F32, 4)
            junk_p = self.pool(st, "junk", [128, D], F32, 2)
            gbb = Buf("gb")
            def phaseA(tile):
                s = tile // 4
                tcs = slice(tile * 128, (tile + 1) * 128)
                j, xr, xrb = xr_p.next()
                P.dma("sp", "xr%d" % j, xr[:], xloc_rows(tile), [], [xrb])
                jr, r, rb = r_p.next()
                for half in range(2):
                    ps, pb = self.ps_next()
                    for kc in range(8):
                        P.mm(ps[:], mrgT[:, kc, tcs], Wo[:, kc, half * 512:(half + 1) * 512], kc == 0, kc == 7,
                             [mrgTb[s], bWo], [pb])
                    P.stt("dve", r[:, half * 512:(half + 1) * 512], xr[:, half * 512:(half + 1) * 512], ALPHA, ps[:],
                          ALU.mult, ALU.add, [xrb, pb], [rb])
                lnst = self.ln_a(r, rb, smln, junk_p)
                return dict(tile=tile, s=s, tcs=tcs, r=r, rb=rb, jr=jr, lnst=lnst)

            def phaseB(st_):
                tile, s, tcs, r, rb, jr = st_["tile"], st_["s"], st_["tcs"], st_["r"], st_["rb"], st_["jr"]
                self.ln_b(r, rb, g1, b1, [bg1, bb1], st_["lnst"])
                P.dma("sp", "r%d" % jr, dr["h"].ap()[tile * 128:(tile + 1) * 128, :], r[:], [rb], [db["h"]])
                jh, hTf, hTfb = hTf_p.next()
                for q in range(2):
                    ps, pb = self.ps_next()
                    for i in range(4):
                        dc = q * 4 + i
                        P.tr(ps[:, i * 128:(i + 1) * 128], r[:, dc * 128:(dc + 1) * 128], [rb], [pb])
                    P.cp("act", hTf[:, q * 4:(q + 1) * 4, :], ps[:].rearrange("p (i t) -> p i t", t=128), [pb], [hTfb])
                    P.cp("dve", actT[:, q * 4:(q + 1) * 4, tcs], hTf[:, q * 4:(q + 1) * 4, :], [hTfb],
                         [actTb[s]])
                st_["hTf"], st_["hTfb"] = hTf, hTfb

            def phaseC(st_):
                s, tcs, hTf, hTfb = st_["s"], st_["tcs"], st_["hTf"], st_["hTfb"]
                ps, pb = self.ps_next()
                for kc in range(8):
                    P.mm(ps[:, 0:36], hTf[:, kc, :], wR[:, kc, :], kc == 0, kc == 7, [hTfb, bwR], [pb])
                j, sm, smb = sm_p.next()
                j2, s2, s2b = sm2_p.next()
                B = [smb]
                P.tt("dve", sm[:, 0:36], ps[:, 0:36], bR[:], ALU.add, [pb, bbR], B)
                c = lambda i: sm[:, i:i + 1]
                S.op("dve", lambda e, o=c(112), i=sm[:, 0:4]: e.reduce_max(out=o, in_=i, axis=AX.X), B, B)
                P.ts("dve", c(113), c(112), -1.0, None, ALU.mult, None, B, B)
                P.act(sm[:, 36:40], sm[:, 0:4], AF.Exp, B, B, bias=c(113))
                S.op("dve", lambda e, o=c(114), i=sm[:, 36:40]: e.reduce_sum(out=o, in_=i, axis=AX.X), B, B)
                S.op("dve", lambda e, o=c(115), i=c(114): e.reciprocal(out=o, in_=i), B, B)
                P.ts("dve", sm[:, 40:44], sm[:, 0:4], c(112), None, ALU.is_equal, None, B, B)
                P.ts("dve", sm[:, 44:48], sm[:, 40:44], 1.0, 1e9, ALU.subtract, ALU.mult, B, B)
                for g in range(4):
                    P.ts("dve", sm[:, 48 + g * 8:56 + g * 8], sm[:, 4 + g * 8:12 + g * 8], sm[:, 44 + g:45 + g], None,
                         ALU.add, None, B, B)
                S.op("dve", lambda e, o=c(116), i=sm[:, 48:80]: e.reduce_max(out=o, in_=i, axis=AX.X), B, B)
                P.ts("dve", s2[:, 0:32], sm[:, 48:80], c(116), None, ALU.is_equal, None, B + [s2b], [s2b])
                P.stt("dve", sm[:, 80:112], s2[:, 0:32], -1e9, sm[:, 48:80], ALU.mult, ALU.add, B + [s2b], B)
                S.op("dve", lambda e, o=c(118), i=sm[:, 80:112]: e.reduce_max(out=o, in_=i, axis=AX.X), B, B)
                P.ts("dve", s2[:, 32:64], sm[:, 80:112], c(118), None, ALU.is_equal, None, B + [s2b], [s2b])
                P.ts("dve", c(117), c(116), -1.0, None, ALU.mult, None, B, B)
                P.act(c(119), c(118), AF.Exp, B, B, bias=c(117))
                P.ts("dve", c(120), c(119), 1.0, None, ALU.add, None, B, B)
                S.op("dve", lambda e, o=c(120): e.reciprocal(out=o, in_=o), B, B)
                P.tt("dve", c(121), c(115), c(120), ALU.mult, B, B)
                P.tt("dve", c(122), c(121), c(119), ALU.mult, B, B)
                P.ts("dve", s2[:, 0:32], s2[:, 0:32], c(121), None, ALU.mult, None, B + [s2b], [s2b])
                P.stt("dve", s2[:, 0:32], s2[:, 32:64], c(122), s2[:, 0:32], ALU.mult, ALU.add, B + [s2b], [s2b])
                ps, pb = self.ps_next()
                P.mm(ps[0:32, 0:128], s2[:, 0:32], self.ident[:], True, True, [s2b, bc], [pb])
                P.cp("act", WtT[0:32, tcs], ps[0:32, 0:128], [pb], [WtTb[s]])

            states = {}
            for step in range(16 + 2):
                if step < 16:
                    states[step] = phaseA(step)
                if 0 <= step - 1 < 16:
                    phaseB(states[step - 1])
                if 0 <= step - 2 < 16:
                    phaseC(states.pop(step - 2))
            S.barrier()

    def stage_F2(self, l, out_rows, W, actT, actTb, WtT, WtTb):
        S, P = self.S, self
        bc = self.b_const
        dr, db = self.dr, self.dbuf
        hT = actT
        with ExitStack() as st:
            acc = self.sb(st, "acc", [128, 16, D], F32)
            accb = [Buf("acc%d" % t) for t in range(16)]
            with ExitStack() as st2:
                hid_p = self.pool(st2, "hid", [128, 2, TL], BF16, 3)
                wgu_p = self.pool(st2, "wgu", [128, 8, 512], BF16, 2)
                wd_p = self.pool(st2, "wd", [128, 2, D], BF16, 3)
                wbc_p = self.pool(st2, "wbc", [128, TL], BF16, 1)
                sg_p = self.pool(st2, "sg", [128, 512], F32, 3)
                t_p = self.pool(st2, "tF", [128, 512], F32, 3)
                for eg in range(16):
                    hids, wds = [], []
                    for ei in range(2):
                        e_ = eg * 2 + ei
                        j, wgu, wgub = wgu_p.next()
                        P.dma("pool", "wgu%d" % j, wgu[:, :, 0:256], W["wEg"].ap()[e_].rearrange("p (k m) -> p k m", m=256),
                              [], [wgub])
                        P.dma("pool", "wgu%d" % j, wgu[:, :, 256:512], W["wEu"].ap()[e_].rearrange("p (k m) -> p k m", m=256),
                              [], [wgub])
                        j, wd, wdb = wd_p.next()
                        P.dma("pool", "wd%d" % j, wd[:], W["wEd"].ap()[e_].rearrange("p (k m) -> p k m", m=D), [], [wdb])
                        jw, wbc, wbcb = wbc_p.next()
                        for s in range(4):
                            cs = slice(s * 512, (s + 1) * 512)
                            ps, pb = self.ps_next()
                            P.mm(ps[:], self.selE[0:32, e_ * 128:(e_ + 1) * 128], WtT[0:32, cs], True, True,
                                 [bc, WtTb[s]], [pb])
                            P.cp("act", wbc[:, cs], ps[:], [pb], [wbcb])
                        jh, hid, hidb = hid_p.next()
                        for fc in range(2):
                            for s in range(4):
                                cs = slice(s * 512, (s + 1) * 512)
                                psg, pbg = self.ps_next()
                                for kc in range(8):
                                    P.mm(psg[:], wgu[:, kc, fc * 128:(fc + 1) * 128], hT[:, kc, cs], kc == 0, kc == 7,
                                         [wgub, actTb[s]], [pbg])
                                psu, pbu = self.ps_next()
                                for kc in range(8):
                                    P.mm(psu[:], wgu[:, kc, 256 + fc * 128:256 + (fc + 1) * 128], hT[:, kc, cs], kc == 0,
                                         kc == 7, [wgub, actTb[s]], [pbu])
                                j1, sg, sgb = sg_p.next()
                                P.act(sg[:], psg[:], AF.Silu, [pbg], [sgb])
                                j2, tt_, ttb = t_p.next()
                                P.tt("dve", tt_[:], sg[:], psu[:], ALU.mult, [sgb, pbu], [ttb])
                                P.tt("pool", hid[:, fc, cs], tt_[:], wbc[:, cs], ALU.mult, [ttb, wbcb], [hidb])
                        hids.append((hid, hidb))
                        wds.append((wd, wdb))
                    for tile in range(16):
                        tcs = slice(tile * 128, (tile + 1) * 128)
                        for half in range(2):
                            ps, pb = self.ps_next()
                            k = 0
                            for ei in range(2):
                                for fc in range(2):
                                    P.mm(ps[:], hids[ei][0][:, fc, tcs], wds[ei][0][:, fc, half * 512:(half + 1) * 512],
                                         k == 0, k == 3, [hids[ei][1], wds[ei][1]], [pb])
                                    k += 1
                            dst = acc[:, tile, half * 512:(half + 1) * 512]
                            if eg == 0:
                                P.cp("dve", dst, ps[:], [pb], [accb[tile]])
                            else:
                                P.tt("dve", dst, dst, ps[:], ALU.add, [pb, accb[tile]], [accb[tile]])
                S.barrier()
            with ExitStack() as st3:
                g2, bg2 = self.load_w(st3, "ln2g", [128, D], W["ln2_g"].ap().partition_broadcast(128), cast=False)
                b2, bb2 = self.load_w(st3, "ln2b", [128, D], W["ln2_b"].ap().partition_broadcast(128), cast=False)
                xr_p = self.pool(st3, "hr", [128, D], F32, 4)
                smln = self.pool(st3, "smln2", [128, 8], F32, 4)
                junk_p = self.pool(st3, "junk2", [128, D], F32, 2)

                hr_q = {}

                def hload(tile):
                    j, xr, xrb = xr_p.next()
                    P.dma("sp", "hr%d" % j, xr[:], dr["h"].ap()[tile * 128:(tile + 1) * 128, :], [db["h"]], [xrb])
                    hr_q[tile] = (xr, xrb)

                hload(0)
                hload(1)

                def l2a(tile):
                    if tile + 2 < 16:
                        hload(tile + 2)
                    xr, xrb = hr_q.pop(tile)
                    r = acc[:, tile, :]
                    P.stt("dve", r, xr[:], ALPHA, r, ALU.mult, ALU.add, [xrb, accb[tile]], [accb[tile]])
                    return self.ln_a(acc[:, tile, :], accb[tile], smln, junk_p)

                def l2b(tile, st_):
                    self.ln_b(acc[:, tile, :], accb[tile], g2, b2, [bg2, bb2], st_)
                    ob = self.out_bufs[tile // 4] if getattr(self, "out_bufs", None) else db["out"]
                    P.dma("sp", "oacc%d" % (tile % 4), out_rows(tile), acc[:, tile, :], [accb[tile]], [ob])
                    if tile % 4 == 3 and getattr(self, "after_slot", None):
                        self.after_slot(tile // 4)

                prev = None
                for tile in range(16):
                    cur = (tile, l2a(tile))
                    if prev is not None:
                        l2b(*prev)
                    prev = cur
                l2b(*prev)
                S.barrier()

    def build(self):
        nc, S = self.nc, self.S
        x_full = self.din("x_full", [SEQ, D])
        x_loc = self.din("x_loc", [TL, D])
        out = self.dout("out", [TL, D])
        Ws = {}
        import os
        stop = int(os.environ.get("KSTOP", "9"))
        for l in self.layers:
            Ws[l] = {k: self.din("%s_l%d" % (k, l), shp) for k, shp in LAYER_SHAPES.items()
                     if stop >= 6 or k not in ("wEg", "wEu", "wEd")}
        self.dr = {
            "KTm": self.dint("KTm", [4, 96, SEQ], BF16), "Vm": self.dint("Vm", [SEQ, 256], BF16),
            "KTs": self.dint("KTs", [4, 64, SEQ], BF16), "Vs": self.dint("Vs", [SEQ, 256], BF16),
            "KTf": self.dint("KTf", [4, 64, SEQ], BF16), "Vf": self.dint("Vf", [SEQ, 256], BF16),
            "h": self.dint("hbuf", [TL, D], F32),
        }
        self.dbuf = {k: Buf(k) for k in list(self.dr.keys()) + ["out"]}
        import os
        self.kv_exchange = os.environ.get("KVX", "1") == "1"
        if self.kv_exchange:
            self.xch = {
                "KVx": [self.dint("KVx%d" % s_, [896, GS], BF16) for s_ in range(4)],
                "KVg": [self.dint("KVg%d" % s_, [2 * 896, GS], BF16) for s_ in range(4)],
                "Vx": [self.dint("Vx%d" % s_, [GS, 768], BF16) for s_ in range(4)],
                "Vg": [self.dint("Vg%d" % s_, [2 * GS, 768], BF16) for s_ in range(4)],
                "spx": self.dint("spx", [128, 128], F32), "spg": self.dint("spg", [256, 128], F32),
                "bKVx": [Buf() for _ in range(4)], "bKVg": [Buf() for _ in range(4)],
                "bVx": [Buf() for _ in range(4)], "bVg": [Buf() for _ in range(4)],
                "bspx": Buf(), "bspg": Buf(),
            }
        with ExitStack() as st:
            self.setup_consts(st)
            self.setup_rope(st)
            if len(self.layers) == 1:
                l = self.layers[0]
                self.emit_layer(l, lambda g: x_full.ap()[g * GS:(g + 1) * GS, :],
                                lambda t: x_loc.ap()[t * 128:(t + 1) * 128, :],
                                lambda t: out.ap()[t * 128:(t + 1) * 128, :], Ws[l])
            else:
                xl1 = [self.dint("xl1_%d" % s_, [GS, D], F32) for s_ in range(4)]
                xg = [self.dint("xg_%d" % s_, [2 * GS, D], F32) for s_ in range(4)]
                bxl1 = [Buf("xl1_%d" % s_) for s_ in range(4)]
                bxg = [Buf("xg_%d" % s_) for s_ in range(4)]
                self.dbuf["out"] = None
                self.out_bufs = bxl1
                groups = [[0, 1], [2, 3], [4, 5], [6, 7]]

                def gather(s_):
                    S.dma("pool", "cc%d" % s_,
                          lambda e, s_=s_: e.collective_compute("AllGather", ALU.bypass, replica_groups=groups,
                                                                ins=[xl1[s_].ap().opt()], outs=[xg[s_].ap().opt()]),
                          [bxl1[s_]], [bxg[s_]], inc=1)
                self.after_slot = gather
                self.emit_layer(0, lambda g: x_full.ap()[g * GS:(g + 1) * GS, :],
                                lambda t: x_loc.ap()[t * 128:(t + 1) * 128, :],
                                lambda t: xl1[t // 4].ap()[(t % 4) * 128:(t % 4 + 1) * 128, :], Ws[0])
                self.after_slot = None
                if self.kv_exchange and os.environ.get("BSKIP", "1") == "1":
                    S.barrier(skip=("cc0", "cc1", "cc2", "cc3"))
                    self.xfull_bufs = lambda g_: [bxg[G_PAR[0].index(g_)] if g_ in G_PAR[0] else bxg[G_PAR[1].index(g_)]]
                else:
                    S.barrier()
                S.new_epoch()
                self.out_bufs = None
                self.dbuf["out"] = Buf("out")

                def xfull1(g):
                    if g in G_PAR[0]:
                        return xg[G_PAR[0].index(g)].ap()[0:GS, :]
                    return xg[G_PAR[1].index(g)].ap()[GS:2 * GS, :]
                self.emit_layer(1, xfull1, lambda t: xl1[t // 4].ap()[(t % 4) * 128:(t % 4 + 1) * 128, :],
                                lambda t: out.ap()[t * 128:(t + 1) * 128, :], Ws[1])
            S.barrier()
            S.emit()
        S.close()
        return nc


_PROG_CACHE = {}


def _get_prog(layers):
    key = tuple(layers)
    if key not in _PROG_CACHE:
        _PROG_CACHE[key] = Prog(list(layers)).build()
    return _PROG_CACHE[key]


def _idx(par):
    return np.concatenate([np.arange(g * GS, (g + 1) * GS) for g in G_PAR[par]])


def kernel(**inputs):
    inputs = {k: np.asarray(v) for k, v in inputs.items()}
    x = np.ascontiguousarray(inputs["x"], dtype=np.float32)
    positions = inputs["positions"]
    nc = _get_prog((0, 1))
    la = {}
    for l in range(DEPTH):
        for k, v in layer_arrays(inputs, l).items():
            la["%s_l%d" % (k, l)] = v
    in_maps = []
    for core in range(N_CORES):
        b, par = core // 2, core % 2
        thr, sel, inv2pi = core_meta(core)
        idx = _idx(par)
        m = {"x_full": np.ascontiguousarray(x[b]), "x_loc": np.ascontiguousarray(x[b][idx]),
             "pos_full": np.ascontiguousarray(positions[b].reshape(1, SEQ).astype(np.int32)),
             "pos_loc": np.ascontiguousarray(positions[b][idx].reshape(1, TL).astype(np.int32)),
             "thr": thr, "sel": sel, "inv2pi": inv2pi}
        m.update(la)
        in_maps.append(m)
    res = run_bass_kernel_spmd(nc, in_maps, core_ids=list(range(N_CORES)))
    out = np.empty_like(x)
    for core in range(N_CORES):
        b, par = core // 2, core % 2
        out[b][_idx(par)] = res.results[core]["out"]
    return out
```

```python
import numpy as np
from contextlib import ExitStack
import concourse.bass as bass
import concourse.mybir as mybir
from concourse.bass_utils import run_bass_kernel_spmd

F32 = mybir.dt.float32
BF16 = mybir.dt.bfloat16
I32 = mybir.dt.int32
ALU = mybir.AluOpType
AF = mybir.ActivationFunctionType
AX = mybir.AxisListType

D = 1024
SEQ = 4096
TL = 2048
GS = 512
NSLOT = 4
G_PAR = ((0, 3, 4, 7), (1, 2, 5, 6))
ALPHA = float((2.0 * 2) ** 0.25)
EPS = 1e-5
NEGBIG = -30000.0
DEPTH = 2
N_CORES = 8


class Buf:
    __slots__ = ("name", "w", "r")

    def __init__(self, name=""):
        self.name = name
        self.w = None
        self.r = {}


class Sched:
    ENGS = ("pe", "act", "dve", "pool", "sp")

    def __init__(self, nc):
        self.nc = nc
        self.q = {e: [] for e in self.ENGS}
        self.cnt = {e: 0 for e in self.ENGS}
        self.sems = {}
        self._ctx = []
        for e in self.ENGS:
            cm = nc.semaphore("s_" + e)
            self.sems[e] = cm.__enter__()
            self._ctx.append(cm)
        self.dma_sems = {}
        self.dma_cnt = {}
        self.seen = {e: {} for e in self.ENGS}
        self.n_instr = 0
        self.epoch = 0

    def new_epoch(self):
        self.epoch += 1
        for e in self.ENGS:
            cm = self.nc.semaphore("s_%s_%d" % (e, self.epoch))
            self.sems[e] = cm.__enter__()
            self._ctx.append(cm)
            self.cnt[e] = 0
        for e in self.ENGS:
            self.seen[e] = {k: v for k, v in self.seen[e].items() if not isinstance(k, tuple)}

    def close(self):
        for cm in reversed(self._ctx):
            cm.__exit__(None, None, None)

    def _dma_sem(self, key):
        if key not in self.dma_sems:
            cm = self.nc.semaphore("d_" + str(key))
            self.dma_sems[key] = cm.__enter__()
            self._ctx.append(cm)
            self.dma_cnt[key] = 0
        return self.dma_sems[key]

    def _semh(self, key):
        return self.sems[key[0]] if isinstance(key, tuple) else self.dma_sems[key]

    def _waits(self, eng, reads, writes):
        need = {}
        for b in reads:
            if b.w is not None:
                k, v = b.w
                if v > need.get(k, 0):
                    need[k] = v
        for b in writes:
            if b.w is not None:
                k, v = b.w
                if v > need.get(k, 0):
                    need[k] = v
            for k, v in b.r.items():
                if v > need.get(k, 0):
                    need[k] = v
        out = []
        seen = self.seen[eng]
        for k, v in need.items():
            if isinstance(k, tuple):
                if k[1] < self.epoch:
                    continue
                if k[0] == "pe" and eng == "pe":
                    continue
            if seen.get(k, 0) >= v:
                continue
            seen[k] = v
            out.append((self._semh(k), v))
        return out

    def _mark(self, tok, reads, writes):
        k, v = tok
        for b in reads:
            if v > b.r.get(k, 0):
                b.r[k] = v
        for b in writes:
            b.w = tok
            b.r = {}

    def op(self, eng, fn, reads=(), writes=()):
        waits = self._waits(eng, reads, writes)
        self.cnt[eng] += 1
        tok = ((eng, self.epoch), self.cnt[eng])
        self.q[eng].append((waits, fn, self.sems[eng], 1))
        self._mark(tok, reads, writes)
        self.n_instr += 1
        return tok

    def dma(self, queue, key, fn, reads=(), writes=(), inc=16):
        waits = self._waits(queue, reads, writes)
        sem = self._dma_sem(key)
        self.dma_cnt[key] += inc
        tok = (key, self.dma_cnt[key])
        self.q[queue].append((waits, fn, sem, inc))
        self._mark(tok, reads, writes)
        self.n_instr += 1
        return tok

    def barrier(self, skip=()):
        for e in self.ENGS:
            waits = []
            seen = self.seen[e]
            for k in self.ENGS:
                v = self.cnt[k]
                kk = (k, self.epoch)
                if v > seen.get(kk, 0) and not (k == "pe" and e == "pe" and False):
                    seen[kk] = v
                    waits.append((self.sems[k], v))
            for k, v in self.dma_cnt.items():
                if k in skip:
                    continue
                if v > seen.get(k, 0):
                    seen[k] = v
                    waits.append((self.dma_sems[k], v))
            if waits:
                self.q[e].append((waits, None, None, 0))

    def emit(self):
        nc = self.nc
        qs = self.q

        def run(e, items):
            for waits, fn, sem, inc in items:
                for (s, v) in waits:
                    e.wait_ge(s, v)
                if fn is not None:
                    fn(e).then_inc(sem, inc)

        with nc.Block() as block:
            @block.tensor
            def _(e):
                run(e, qs["pe"])

            @block.scalar
            def _(e):
                run(e, qs["act"])

            @block.vector
            def _(e):
                run(e, qs["dve"])

            @block.gpsimd
            def _(e):
                run(e, qs["pool"])

            @block.sync
            def _(e):
                run(e, qs["sp"])


class RPool:
    def __init__(self, tiles):
        self.tiles = tiles
        self.bufs = [Buf() for _ in tiles]
        self.i = 0

    def next(self):
        j = self.i % len(self.tiles)
        self.i += 1
        return j, self.tiles[j], self.bufs[j]


def _kc(w):
    k, m = w.shape
    n = k // 128
    return np.ascontiguousarray(w.reshape(n, 128, m).transpose(1, 0, 2).reshape(128, n * m))


def layer_arrays(inp, l):
    f = np.float32
    w_in = inp["w_in"][l]
    A = {}
    A["wK_ckv"] = _kc(w_in[:, 256:384])
    kr = w_in[:, 384:416]
    pad = np.zeros((D, 128), f)
    pad[:, 64:96] = kr
    A["wK_kr"] = _kc(pad)
    pad = np.zeros((D, 128), f)
    pad[:, 64:80] = kr[:, 16:32]
    pad[:, 80:96] = kr[:, 0:16]
    A["wK_krr"] = _kc(pad)
    A["wK_sbk"] = _kc(w_in[:, 672:928])
    A["wK_fxk"] = _kc(w_in[:, 1952:2208])
    A["wK_v2"] = _kc(np.concatenate([w_in[:, 928:1184], w_in[:, 2208:2464]], 1))
    A["wK_f"] = _kc(w_in[:, 2464:2468])
    ukv = inp["mla_w_ukv"][l]
    A["w_ukv_k"] = np.ascontiguousarray(np.concatenate([ukv[:, h * 128:h * 128 + 64] for h in range(4)], 1))
    A["w_ukv_v"] = np.ascontiguousarray(np.concatenate([ukv[:, h * 128 + 64:h * 128 + 128] for h in range(4)], 1))
    A["kvn"] = np.ascontiguousarray(inp["mla_kv_norm"][l].reshape(128, 1))
    A["wQ_cq"] = _kc(w_in[:, 0:256])
    uq = inp["mla_w_uq"][l]
    A["w_uq"] = _kc(uq)
    uqr = np.zeros_like(uq)
    for h in range(4):
        o = h * 96 + 64
        uqr[:, o:o + 16] = uq[:, o + 16:o + 32]
        uqr[:, o + 16:o + 32] = uq[:, o:o + 16]
    A["w_uqr"] = _kc(uqr)
    A["qn"] = np.ascontiguousarray(inp["mla_q_norm"][l].reshape(2, 128).T)
    A["wQ_sbq"] = _kc(w_in[:, 416:672])
    A["wQ_fxq"] = _kc(w_in[:, 1696:1952])
    A["wQ_cv"] = _kc(w_in[:, 1184:1696])
    g = w_in[:, 2468:6564].reshape(D, 4, 8, 128)
    g = g.reshape(8, 128, 4, 8, 128).transpose(3, 2, 1, 0, 4)
    A["wG"] = np.ascontiguousarray(g.reshape(32, 128, 1024))
    A["bg"] = np.ascontiguousarray(inp["b_gate"][l].reshape(4, 8, 128).transpose(2, 1, 0).reshape(128, 32))
    wb = inp["w_branch"][l].reshape(4, 2, 128, D).transpose(2, 0, 1, 3)
    A["wB"] = np.ascontiguousarray(wb.reshape(128, 8 * D))
    A["wO"] = _kc(inp["w_o"][l])
    A["convw"] = np.ascontiguousarray(inp["conv_w"][l].reshape(31, 2, 128).transpose(2, 1, 0).reshape(128, 62))
    A["convb"] = np.ascontiguousarray(inp["conv_b"][l].reshape(2, 128).T)
    A["clng"] = np.ascontiguousarray(inp["conv_ln_g"][l].reshape(2, 128).T)
    A["clnb"] = np.ascontiguousarray(inp["conv_ln_b"][l].reshape(2, 128).T)
    for k in ("ln1_g", "ln1_b", "ln2_g", "ln2_b"):
        A[k] = np.ascontiguousarray(inp[k][l].reshape(1, D))
    A["bfg"] = np.ascontiguousarray(inp["b_forget"][l].reshape(1, 4))
    A["wR"] = _kc(np.concatenate([inp["w_router_group"][l], inp["w_router_expert"][l]], 1))
    A["bR"] = np.ascontiguousarray(np.concatenate([inp["b_router_group"][l], inp["b_router_expert"][l]]).reshape(1, 36))
    eg = inp["w_exp_gate"][l].reshape(32, 8, 128, 256).transpose(0, 2, 1, 3)
    A["wEg"] = np.ascontiguousarray(eg.reshape(32, 128, 2048))
    eu = inp["w_exp_up"][l].reshape(32, 8, 128, 256).transpose(0, 2, 1, 3)
    A["wEu"] = np.ascontiguousarray(eu.reshape(32, 128, 2048))
    ed = inp["w_exp_down"][l].reshape(32, 2, 128, D).transpose(0, 2, 1, 3)
    A["wEd"] = np.ascontiguousarray(ed.reshape(32, 128, 2048))
    return {k: np.asarray(v, dtype=f) for k, v in A.items()}


LAYER_SHAPES = {
    "wK_ckv": [128, 1024], "wK_kr": [128, 1024], "wK_krr": [128, 1024], "wK_sbk": [128, 2048],
    "wK_fxk": [128, 2048], "wK_v2": [128, 4096], "wK_f": [128, 32], "w_ukv_k": [128, 256],
    "w_ukv_v": [128, 256], "kvn": [128, 1], "wQ_cq": [128, 2048], "w_uq": [128, 768],
    "w_uqr": [128, 768], "qn": [128, 2], "wQ_sbq": [128, 2048], "wQ_fxq": [128, 2048],
    "wQ_cv": [128, 4096], "wG": [32, 128, 1024], "bg": [128, 32], "wB": [128, 8192],
    "wO": [128, 8192], "convw": [128, 62], "convb": [128, 2], "clng": [128, 2], "clnb": [128, 2],
    "ln1_g": [1, D], "ln1_b": [1, D], "ln2_g": [1, D], "ln2_b": [1, D], "bfg": [1, 4],
    "wR": [128, 288], "bR": [1, 36], "wEg": [32, 128, 2048], "wEu": [32, 128, 2048],
    "wEd": [32, 128, 2048],
}


def core_meta(core):
    par = core % 2
    thr = np.zeros((128, 32), np.float32)
    for s in range(4):
        for j in range(8):
            kb = 8 * s + j
            thr[:, s * 8 + j] = G_PAR[par][s] * GS - kb * 128
    sel = np.zeros((128, 2), np.float32)
    sel[:, par] = 1.0
    inv = 10000.0 ** (-(np.arange(16, dtype=np.float64)) / 16.0)
    inv2pi = np.zeros((128, 1), np.float32)
    for i in range(32):
        inv2pi[64 + i, 0] = inv[i % 16] / (2 * np.pi)
    return thr, sel, inv2pi


class Prog:
    def __init__(self, layers, debug=False):
        self.layers = layers
        self.debug = debug
        nc = bass.Bass("TRN2", target_bir_lowering=False)
        self.nc = nc
        self.S = Sched(nc)
        self.dram = {}
        self.dbuf = {}

    def din(self, name, shape, dt=F32):
        t = self.nc.dram_tensor(name, list(shape), dt, kind="ExternalInput")
        self.dram[name] = t
        return t

    def dout(self, name, shape, dt=F32):
        t = self.nc.dram_tensor(name, list(shape), dt, kind="ExternalOutput")
        self.dram[name] = t
        return t

    def dint(self, name, shape, dt):
        t = self.nc.dram_tensor(name, list(shape), dt)
        self.dram[name] = t
        return t

    def sb(self, stack, name, shape, dt):
        self._sbn = getattr(self, "_sbn", 0) + 1
        return stack.enter_context(self.nc.sbuf_tensor("s%d_%s" % (self._sbn, name), list(shape), dt))

    def pool(self, stack, name, shape, dt, n):
        return RPool([self.sb(stack, "%s%d" % (name, i), shape, dt) for i in range(n)])

    def mm(self, out, lhsT, rhs, start, stop, reads, writes):
        self.S.op("pe", lambda e, o=out, l=lhsT, r=rhs, a=start, b=stop: e.matmul(o, l, r, start=a, stop=b),
                  reads, writes)

    def tr(self, out, in_, reads, writes):
        idn = self.ident
        self.S.op("pe", lambda e, o=out, i=in_: e.transpose(o, i, idn[:]), list(reads) + [self.b_const], writes)

    def act(self, out, in_, func, reads, writes, bias=None, scale=None, accum_out=None):
        kw = {}
        if bias is not None:
            kw["bias"] = bias
        if scale is not None:
            kw["scale"] = scale
        if accum_out is not None:
            kw["accum_out"] = accum_out
        self.S.op("act", lambda e, o=out, i=in_, f=func, kw=kw: e.activation(out=o, in_=i, func=f, **kw), reads, writes)

    def tt(self, eng, out, in0, in1, op, reads, writes):
        self.S.op(eng, lambda e, o=out, a=in0, b=in1, p=op: e.tensor_tensor(out=o, in0=a, in1=b, op=p), reads, writes)

    def ts(self, eng, out, in0, s1, s2, op0, op1, reads, writes):
        if op1 is None:
            self.S.op(eng, lambda e, o=out, a=in0, x=s1, p=op0: e.tensor_single_scalar(out=o, in_=a, scalar=x, op=p),
                      reads, writes)
        else:
            self.S.op(eng, lambda e, o=out, a=in0, x=s1, y=s2, p=op0, q=op1:
                      e.tensor_scalar(out=o, in0=a, scalar1=x, scalar2=y, op0=p, op1=q), reads, writes)

    def stt(self, eng, out, in0, scalar, in1, op0, op1, reads, writes):
        self.S.op(eng, lambda e, o=out, a=in0, s=scalar, b=in1, p=op0, q=op1:
                  e.scalar_tensor_tensor(out=o, in0=a, scalar=s, in1=b, op0=p, op1=q), reads, writes)

    def cp(self, eng, out, in_, reads, writes):
        if eng == "act":
            self.S.op("act", lambda e, o=out, i=in_: e.copy(out=o, in_=i), reads, writes)
        else:
            self.S.op(eng, lambda e, o=out, i=in_: e.tensor_copy(out=o, in_=i), reads, writes)

    def dma(self, queue, key, out, in_, reads, writes):
        self.S.dma(queue, key, lambda e, o=out, i=in_: e.dma_start(out=o, in_=i), reads, writes)

    def dump(self, name, src, shape, dt, reads):
        if not self.debug:
            return
        t = self.dout("dbg_" + name, shape, dt)
        b = Buf("dbg_" + name)
        self.dma("sp", "dbg_" + name, t.ap(), src, reads, [b])

    def evac_eng(self):
        self._ev = getattr(self, "_ev", 0) + 1
        return "act" if self._ev % 2 else "dve"

    def setup_consts(self, st):
        nc, S = self.nc, self.S
        P = self
        self.b_const = Buf("const")
        bc = [self.b_const]
        self.iotf = self.sb(st, "iotf", [128, 512], F32)
        self.ident = self.sb(st, "ident", [128, 128], F32)
        self.ones_bf = self.sb(st, "ones_bf", [128, 128], BF16)
        self.ones_f = self.sb(st, "ones_f", [128, 128], F32)
        self.LT = self.sb(st, "LT", [128, 128], BF16)
        self.UT = self.sb(st, "UT", [128, 128], F32)
        self.D0 = self.sb(st, "D0", [128, 512], F32)
        self.D1 = self.sb(st, "D1", [128, 512], F32)
        self.selE = self.sb(st, "selE", [32, 32 * 128], BF16)
        self.Eh = self.sb(st, "Eh", [4, 4 * 65], BF16)
        self.epsc = self.sb(st, "epsc", [128, 1], F32)
        self.onec = self.sb(st, "onec", [128, 1], F32)
        self.thr = self.sb(st, "thr", [128, 32], F32)
        self.sel = self.sb(st, "sel", [128, 2], F32)
        self.sel8 = self.sb(st, "sel8", [128, 2], F32)
        self.inv2pi = self.sb(st, "inv2pi", [128, 1], F32)
        S.op("pool", lambda e: e.iota(self.iotf[:], [[1, 512]], base=0, channel_multiplier=-1,
                                      allow_small_or_imprecise_dtypes=True), [], bc)
        P.ts("dve", self.ident[:], self.iotf[:, 0:128], 0.0, None, ALU.is_equal, None, bc, bc)
        P.ts("dve", self.UT[:], self.iotf[:, 0:128], 0.0, None, ALU.is_ge, None, bc, bc)
        P.ts("dve", self.LT[:], self.iotf[:, 0:128], 0.0, None, ALU.is_le, None, bc, bc)
        P.ts("dve", self.D0[:], self.iotf[:], -1.0, None, ALU.mult, None, bc, bc)
        P.ts("dve", self.D1[:], self.D0[:], 1.0, None, ALU.add, None, bc, bc)
        S.op("pool", lambda e: e.memset(self.ones_bf[:], 1.0), [], bc)
        S.op("pool", lambda e: e.memset(self.ones_f[:], 1.0), [], bc)
        S.op("pool", lambda e: e.memset(self.epsc[:], EPS), [], bc)
        S.op("pool", lambda e: e.memset(self.onec[:], 1.0), [], bc)
        tst = ExitStack()
        tmp = self.sb(tst, "seltmp", [32, 32 * 128], F32)
        S.op("pool", lambda e: e.iota(tmp[:].rearrange("k (e m) -> k e m", m=128), [[1, 32], [0, 128]], base=0,
                                      channel_multiplier=-1, allow_small_or_imprecise_dtypes=True), [], bc)
        P.ts("dve", self.selE[:], tmp[:], 0.0, None, ALU.is_equal, None, bc, bc)
        S.op("pool", lambda e: e.iota(tmp[0:4, 0:260].rearrange("k (h m) -> k h m", m=65), [[1, 4], [0, 65]], base=0,
                                      channel_multiplier=-1, allow_small_or_imprecise_dtypes=True), bc, bc)
        P.ts("dve", self.Eh[:], tmp[0:4, 0:260], 0.0, None, ALU.is_equal, None, bc, bc)
        S.op("dve", lambda e: e.memset(self.Eh[:].rearrange("k (h m) -> k h m", m=65)[:, :, 0:64], 0.0), bc, bc)
        S.barrier()
        tst.close()
        thr_d = self.din("thr", [128, 32])
        sel_d = self.din("sel", [128, 2])
        inv_d = self.din("inv2pi", [128, 1])
        P.dma("sp", "c_thr", self.thr[:], thr_d.ap(), [], bc)
        P.dma("sp", "c_sel", self.sel[:], sel_d.ap(), [], bc)
        P.dma("sp", "c_inv", self.inv2pi[:], inv_d.ap(), [], bc)
        P.ts("dve", self.sel8[:], self.sel[:], -8.0, None, ALU.mult, None, bc, bc)
        self.psb = [st.enter_context(nc.psum_tensor("psb%d" % i, [128, 512], F32)) for i in range(8)]
        self.ps_bufs = [Buf("ps%d" % i) for i in range(8)]
        self.ps_i = 0

    def ps_next(self, lo=0, hi=8):
        key = (lo, hi)
        if not hasattr(self, "_psrot"):
            self._psrot = {}
        i = self._psrot.get(key, 0)
        self._psrot[key] = i + 1
        j = lo + i % (hi - lo)
        return self.psb[j], self.ps_bufs[j]

    def rope_tables(self, tmp, src, ncols, cos_out, sin_out, out_buf):
        P, S = self, self.S
        pi_, pfl, t, ki, kf, bt = tmp
        sl = slice(64, 96)
        n = ncols
        P.dma("sp", "rp_ld", pi_[sl, 0:n], src.partition_broadcast(32), [], [bt])
        P.cp("dve", pfl[sl, 0:n], pi_[sl, 0:n], [bt], [bt])
        for which, tab in ((0.0, sin_out), (0.25, cos_out)):
            P.ts("dve", t[sl, 0:n], pfl[sl, 0:n], self.inv2pi[sl, 0:1], which, ALU.mult, ALU.add, [bt, self.b_const], [bt])
            P.cp("dve", ki[sl, 0:n], t[sl, 0:n], [bt], [bt])
            P.cp("dve", kf[sl, 0:n], ki[sl, 0:n], [bt], [bt])
            P.tt("dve", t[sl, 0:n], t[sl, 0:n], kf[sl, 0:n], ALU.subtract, [bt], [bt])
            P.ts("dve", kf[sl, 0:n], t[sl, 0:n], 0.5, None, ALU.is_gt, None, [bt], [bt])
            P.tt("dve", t[sl, 0:n], t[sl, 0:n], kf[sl, 0:n], ALU.subtract, [bt], [bt])
            P.ts("dve", kf[sl, 0:n], t[sl, 0:n], -0.5, None, ALU.is_lt, None, [bt], [bt])
            P.tt("dve", t[sl, 0:n], t[sl, 0:n], kf[sl, 0:n], ALU.add, [bt], [bt])
            P.act(tab, t[sl, 0:n], AF.Sin, [bt], [bt, out_buf], scale=float(2 * np.pi * (1 - 1e-6)))

    def rope_tmp(self, st, n):
        return (self.sb(st, "rp_i", [128, n], I32), self.sb(st, "rp_f", [128, n], F32), self.sb(st, "rp_t", [128, n], F32),
                self.sb(st, "rp_ki", [128, n], I32), self.sb(st, "rp_kf", [128, n], F32), Buf("rp"))

    def setup_rope(self, st):
        self.pos_full = self.din("pos_full", [1, SEQ], I32)
        pl = self.din("pos_loc", [1, TL], I32)
        self.cosL = self.sb(st, "cosL", [128, TL], BF16)
        self.sinL = self.sb(st, "sinL", [128, TL], BF16)
        self.b_rope = Buf("rope")
        with ExitStack() as tmp:
            T = self.rope_tmp(tmp, 512)
            for c in range(4):
                cs = slice(c * 512, (c + 1) * 512)
                self.rope_tables(T, pl.ap()[:, cs], 512, self.cosL[64:96, cs], self.sinL[64:96, cs], self.b_rope)
            self.S.barrier()

    def load_w(self, st, name, shape, src, cast=True, key=None, skip=False):
        t = self.sb(st, name, shape, BF16 if cast else F32)
        b = Buf(name)
        if src is not None:
            self.dma("pool" if cast else "sp", key or ("w_" + name), t[:], src, [], [b])
        return t, b

    def transpose_tiles(self, st_pools, row_aps, dst, dst_buf, col0):
        xin = st_pools["xin"]
        tiles = []
        for t, ap in enumerate(row_aps):
            j, xt, xb = xin.next()
            self.dma("sp", "xin%d" % j, xt[:], ap, [], [xb])
            tiles.append((xt, xb))
        n = len(tiles)
        for dc in range(8):
            ps, pb = self.ps_next()
            for t, (xt, xb) in enumerate(tiles):
                self.tr(ps[:, t * 128:(t + 1) * 128], xt[:, dc * 128:(dc + 1) * 128], [xb], [pb])
            self.cp(self.evac_eng(), dst[:, dc, col0:col0 + n * 128], ps[:, 0:n * 128], [pb], [dst_buf])

    def rms_scale(self, st_pools, ps_list, pb_list, n_feat, out_tile, out_buf, ncols=512, ps_range=(0, 8)):
        sqp = st_pools["sq"]
        f32p = st_pools["f32"]
        sqs = []
        for ps, pb in zip(ps_list, pb_list):
            j, sq, sqb = sqp.next()
            self.act(sq[:, 0:ncols], ps[:, 0:ncols], AF.Square, [pb], [sqb])
            sqs.append((sq, sqb))
        pss, pbs = self.ps_next(*ps_range)
        for i, (sq, sqb) in enumerate(sqs):
            self.mm(pss[:, 0:ncols], self.ones_bf[:], sq[:, 0:ncols], i == 0, i == len(sqs) - 1,
                    [sqb, self.b_const], [pbs])
        j, sd, sdb = f32p.next()
        self.act(sd[:, 0:ncols], pss[:, 0:ncols], AF.Sqrt, [pbs, self.b_const], [sdb], bias=self.epsc[:, 0:1],
                 scale=1.0 / n_feat)
        self.S.op("dve", lambda e, o=sd[:, 0:ncols]: e.reciprocal(out=o, in_=o), [sdb], [sdb])
        for i, (ps, pb) in enumerate(zip(ps_list, pb_list)):
            self.tt("dve", out_tile[:, i, 0:ncols], ps[:, 0:ncols], sd[:, 0:ncols], ALU.mult, [pb, sdb], [out_buf])

    def emit_layer(self, l, xfull_rows, xloc_rows, out_rows, W):
        nc, S, P = self.nc, self.S, self
        bc = self.b_const
        dr = self.dr
        with ExitStack() as lst:
            negcK = self.sb(lst, "negcK", [128, 32 * 4], F32)
            negcKb = Buf("negcK")
            cqa = self.sb(lst, "cqa", [4, TL], BF16)
            cqab = Buf("cqa")
            WtT = self.sb(lst, "WtT", [32, TL], BF16)
            WtTb = [Buf("WtT%d" % s) for s in range(4)]
            actT = self.sb(lst, "actT", [128, 8, TL], BF16)
            actTb = [Buf("actT%d" % s) for s in range(4)]
            import os
            stop = int(os.environ.get("KSTOP", "9"))
            if stop < 1:
                return
            if not self.kv_exchange:
                self.stage_K(l, xfull_rows, W, negcK, negcKb, cqa, cqab)
            if self.debug:
                for nm, shp in (("KTm", [4, 96, SEQ]), ("Vm", [SEQ, 256]), ("KTs", [4, 64, SEQ]), ("Vs", [SEQ, 256]),
                                ("KTf", [4, 64, SEQ]), ("Vf", [SEQ, 256])):
                    self.dump(nm, self.dr[nm].ap(), shp, BF16, [self.dbuf[nm]])
                self.dump("negcK", negcK[:], [128, 128], F32, [negcKb])
                self.dump("cqa", cqa[:], [4, TL], BF16, [cqab])
            if stop < 2:
                return
            with ExitStack() as yst:
                yT = [self.sb(yst, "yT%d" % n, [128, 2, TL], BF16) for n in range(4)]
                yTb = [[Buf("yT%d_%d" % (n, s)) for s in range(4)] for n in range(4)]
                self.stage_Q(l, xfull_rows, xloc_rows, W, actT, actTb, yT, yTb, negcK, negcKb, cqa, cqab)
                if self.debug:
                    for n in range(4):
                        self.dump("yT%d" % n, yT[n][:], [128, 2, TL], BF16, yTb[n])
                    self.dump("xlT", actT[:], [128, 8, TL], BF16, actTb)
                    self.dump("negcK2", negcK[:], [128, 128], F32, [negcKb])
                if stop < 4:
                    return
                with ExitStack() as mst:
                    mrgT = self.sb(mst, "mrgT", [128, 8, TL], BF16)
                    mrgTb = [Buf("mrgT%d" % s) for s in range(4)]
                    self.stage_G(l, W, actT, actTb, yT, yTb, mrgT, mrgTb)
                    self.dump("mrgT", mrgT[:], [128, 8, TL], BF16, mrgTb)
                    if stop < 5:
                        return
                    self.stage_F1(l, xloc_rows, W, actT, actTb, mrgT, mrgTb, WtT, WtTb)
            self.dump("h", self.dr["h"].ap(), [TL, D], F32, [self.dbuf["h"]])
            self.dump("WtT", WtT[:], [32, TL], BF16, WtTb)
            if stop < 6:
                return
            self.stage_F2(l, out_rows, W, actT, actTb, WtT, WtTb)

    def mk_pools(self, st, xin=True):
        pools = {
            "sq": self.pool(st, "sq", [128, 512], BF16, 2),
            "f32": self.pool(st, "f32", [128, 512], F32, 6),
            "bf": self.pool(st, "bft", [128, 512], BF16, 8),
        }
        if xin:
            pools["xin"] = self.pool(st, "xin", [128, 1024], F32, 4)
        return pools

    def stage_K2(self, l, W, xlT, actTb, cqa, cqab, xloc_rows):
        S, P = self.S, self
        bc = self.b_const
        X = self.xch
        r3 = lambda name, m: W[name].ap().rearrange("p (k m) -> p k m", m=m)
        groups = [[0, 1], [2, 3], [4, 5], [6, 7]]
        with ExitStack() as st:
            pools = self.mk_pools(st, xin=False)
            Wf, bWf = self.load_w(st, "Wf", [128, 8, 4], r3("wK_f", 4))
            Wsbk, bWsbk = self.load_w(st, "Wsbk", [128, 8, 256], r3("wK_sbk", 256))
            Wfxk, bWfxk = self.load_w(st, "Wfxk", [128, 8, 256], r3("wK_fxk", 256))
            Wv2, bWv2 = self.load_w(st, "Wv2", [128, 8, 512], r3("wK_v2", 512))
            Wckv, bWckv = self.load_w(st, "Wckv", [128, 8, 128], r3("wK_ckv", 128))
            Wkr, bWkr = self.load_w(st, "Wkr", [128, 8, 128], r3("wK_kr", 128))
            Wkrr, bWkrr = self.load_w(st, "Wkrr", [128, 8, 128], r3("wK_krr", 128))
            S.op("dve", lambda e: e.tensor_scalar_mul(out=Wkrr[:, :, 64:80], in0=Wkrr[:, :, 64:80], scalar1=-1.0),
                 [bWkrr], [bWkrr])
            ukf, bukf = self.load_w(st, "ukf", [128, 512], None, cast=False, skip=True)
            kvn, bkvn = self.load_w(st, "kvn", [128, 1], W["kvn"].ap(), cast=False)
            P.dma("sp", "w_ukf", ukf[:, 0:256], W["w_ukv_k"].ap(), [], [bukf])
            P.dma("sp", "w_ukf", ukf[:, 256:512], W["w_ukv_v"].ap(), [], [bukf])
            Wukv = self.sb(st, "Wukv", [128, 512], BF16)
            bWukv = Buf("Wukv")
            P.ts("dve", Wukv[:], ukf[:], kvn[:, 0:1], None, ALU.mult, None, [bukf, bkvn], [bWukv])
            bfg, bbfg = self.load_w(st, "bfg", [128, 4], W["bfg"].ap().partition_broadcast(128), cast=False)
            ckvn_p = self.pool(st, "ckvn", [128, 1, 512], BF16, 2)
            kst_p = self.pool(st, "kst", [128, 4, 512], BF16, 2)
            krst_p = self.pool(st, "krst", [128, 512], BF16, 2)
            vst_p = self.pool(st, "vst", [128, 4, 256], BF16, 2)
            vst2_p = self.pool(st, "vst2", [128, 4, 512], BF16, 2)
            sm_p = self.pool(st, "smallK", [128, 16], F32, 4)
            spown = self.sb(st, "spown", [128, 128], F32)
            bspo = Buf("spown")
            S.op("pool", lambda e: e.memset(spown[:], 0.0), [], [bspo])
            S.op("pool", lambda e: e.memset(cqa[:], 0.0), [], [cqab])
            f32p = pools["f32"]
            sl = slice(64, 96)
            xin8 = {"xin": self.pool(st, "xin", [128, 1024], F32, 8)}
            for s in range(4):
                self.transpose_tiles(xin8, [xloc_rows(s * 4 + t) for t in range(4)], xlT, actTb[s], s * 512)
            for s in range(4):
                cols = slice(s * 512, (s + 1) * 512)
                xTb = actTb[s]
                xT = lambda dc, a=0, b=512, s=s: xlT[:, dc, s * 512 + a:s * 512 + b]
                KV, bKV = X["KVx"][s], X["bKVx"][s]
                VX, bVX = X["Vx"][s], X["bVx"][s]
                for t in range(4):
                    lt = s * 4 + t
                    ps, pb = self.ps_next()
                    for dc in range(8):
                        P.mm(ps[:, 0:4], xT(dc, t * 128, (t + 1) * 128), Wf[:, dc, :], dc == 0, dc == 7, [xTb, bWf], [pb])
                    j, sm, smb = sm_p.next()
                    P.tt("dve", sm[:, 0:4], ps[:, 0:4], bfg[:], ALU.add, [pb, bbfg], [smb])
                    P.act(sm[:, 4:8], sm[:, 0:4], AF.Exp, [smb], [smb], scale=-1.0)
                    P.act(spown[:, lt * 4:(lt + 1) * 4], sm[:, 4:8], AF.Ln, [smb, bc], [bspo], bias=self.onec[:, 0:1])
                for (Wk, bWk, rbase) in ((Wsbk, bWsbk, 384), (Wfxk, bWfxk, 640)):
                    j, kst, kstb = kst_p.next()
                    for pr in range(2):
                        ps, pb = self.ps_next()
                        for dc in range(8):
                            P.mm(ps[:], Wk[:, dc, pr * 128:(pr + 1) * 128], xT(dc), dc == 0, dc == 7, [bWk, xTb], [pb])
                        P.cp(self.evac_eng(), kst[:, pr, :], ps[:], [pb], [kstb])
                        P.dma("sp", "kst%d" % j, KV.ap()[rbase + pr * 128:rbase + (pr + 1) * 128, :], kst[:, pr, :],
                              [kstb], [bKV])
                j, vst2, vst2b = vst2_p.next()
                for t in range(4):
                    ps, pb = self.ps_next()
                    for dc in range(8):
                        P.mm(ps[:], xT(dc, t * 128, (t + 1) * 128), Wv2[:, dc, :], dc == 0, dc == 7, [xTb, bWv2], [pb])
                    P.cp(self.evac_eng(), vst2[:, t, :], ps[:], [pb], [vst2b])
                P.dma("sp", "vst2a%d" % j, VX.ap()[:, 256:768].rearrange("(t p) c -> p t c", p=128), vst2[:], [vst2b], [bVX])
                ps, pb = self.ps_next()
                for dc in range(8):
                    P.mm(ps[:], Wckv[:, dc, :], xT(dc), dc == 0, dc == 7, [bWckv, xTb], [pb])
                j, ckvn, ckvnb = ckvn_p.next()
                self.rms_scale(pools, [ps], [pb], 128, ckvn, ckvnb)
                j, kst, kstb = kst_p.next()
                for h in range(4):
                    ps, pb = self.ps_next()
                    P.mm(ps[0:64, :], Wukv[:, h * 64:(h + 1) * 64], ckvn[:, 0, :], True, True, [bWukv, ckvnb], [pb])
                    P.cp(self.evac_eng(), kst[0:64, h, :], ps[0:64, :], [pb], [kstb])
                P.dma("sp", "kst%d" % j, KV.ap()[0:384, :].rearrange("(h p) t -> p h t", p=96)[0:64], kst[0:64, :, :],
                      [kstb], [bKV])
                psa, pba = self.ps_next()
                for dc in range(8):
                    P.mm(psa[:], Wkr[:, dc, :], xT(dc), dc == 0, dc == 7, [bWkr, xTb], [pba])
                psb_, pbb = self.ps_next()
                for dc in range(8):
                    P.mm(psb_[:], Wkrr[:, dc, :], xT(dc), dc == 0, dc == 7, [bWkrr, xTb], [pbb])
                j1, t1, t1b = f32p.next()
                j2, t2, t2b = f32p.next()
                P.tt("dve", t1[sl, :], psa[sl, :], self.cosL[sl, cols], ALU.mult, [pba, self.b_rope], [t1b])
                P.tt("dve", t2[sl, :], psb_[sl, :], self.sinL[sl, cols], ALU.mult, [pbb, self.b_rope], [t2b])
                j, krst, krstb = krst_p.next()
                P.tt("dve", krst[sl, :], t1[sl, :], t2[sl, :], ALU.add, [t1b, t2b], [krstb])
                for h in range(4):
                    P.dma("sp", "krst%d" % j, KV.ap()[h * 96 + 64:h * 96 + 96, :], krst[sl, :], [krstb], [bKV])
                j, vst, vstb = vst_p.next()
                for tp in range(2):
                    ps, pb = self.ps_next()
                    for t2_ in range(2):
                        t = tp * 2 + t2_
                        P.mm(ps[:, t2_ * 256:(t2_ + 1) * 256], ckvn[:, 0, t * 128:(t + 1) * 128], Wukv[:, 256:512],
                             True, True, [ckvnb, bWukv], [pb])
                    P.cp(self.evac_eng(), vst[:, tp * 2:tp * 2 + 2, :], ps[:].rearrange("p (t c) -> p t c", c=256), [pb], [vstb])
                P.dma("sp", "vst%d" % j, VX.ap()[:, 0:256].rearrange("(t p) c -> p t c", p=128), vst[:], [vstb], [bVX])
                for (a_, ba_, o_, bo_) in ((KV, bKV, X["KVg"][s], X["bKVg"][s]), (VX, bVX, X["Vg"][s], X["bVg"][s])):
                    S.dma("pool", "ccKV%d" % s,
                          lambda e, a_=a_, o_=o_: e.collective_compute("AllGather", ALU.bypass, replica_groups=groups,
                                                                       ins=[a_.ap().opt()], outs=[o_.ap().opt()]),
                          [ba_], [X["bKVg"][s], X["bVg"][s]], inc=1)
            P.dma("sp", "spx", X["spx"].ap(), spown[:], [bspo], [X["bspx"]])
            S.dma("pool", "ccS",
                  lambda e: e.collective_compute("AllGather", ALU.bypass, replica_groups=groups,
                                                 ins=[X["spx"].ap().opt()], outs=[X["spg"].ap().opt()]),
                  [X["bspx"]], [X["bspg"]], inc=1)
            S.barrier()

    def stage_C(self, l, negcK, negcKb, cqa, cqab):
        S, P = self.S, self
        bc = self.b_const
        X, dr, db = self.xch, self.dr, self.dbuf
        with ExitStack() as st:
            spall = self.sb(st, "spall", [128, 32 * 4], F32)
            bsp = Buf("spall")
            for s in range(4):
                for r in range(2):
                    g = G_PAR[r][s]
                    P.dma("sp", "spall", spall[:, g * 16:(g + 1) * 16], X["spg"].ap()[r * 128:(r + 1) * 128, s * 16:(s + 1) * 16],
                          [X["bspg"]], [bsp])
            import os
            cumb = os.environ.get("CUMB", "1") == "1"
            if cumb:
                spre = self.sb(st, "spre", [128, 32 * 4], F32)
                bspre = Buf("spre")
                S.op("dve", lambda e: e.memset(spre[:], 0.0), [], [bspre])
                for gt in range(31):
                    P.tt("dve", spre[:, (gt + 1) * 4:(gt + 2) * 4], spre[:, gt * 4:(gt + 1) * 4],
                         spall[:, gt * 4:(gt + 1) * 4], ALU.add, [bspre, bsp], [bspre])
                ps2, pb2 = self.ps_next()
                P.mm(ps2[:, 0:128], self.UT[:], spall[:], True, False, [bc, bsp], [pb2])
                P.mm(ps2[:, 0:128], self.ones_f[:], spre[:], False, True, [bc, bspre], [pb2])
                P.cp("dve", negcK[:], ps2[:, 0:128], [pb2], [negcKb])
            for g in range(8):
                for t in range(4):
                    if cumb:
                        break
                    gt = g * 4 + t
                    ps2, pb2 = self.ps_next()
                    P.mm(ps2[:, 0:4], self.UT[:], spall[:, gt * 4:(gt + 1) * 4], True, gt == 0, [bc, bsp], [pb2])
                    for tp_ in range(gt):
                        P.mm(ps2[:, 0:4], self.ones_f[:], spall[:, tp_ * 4:(tp_ + 1) * 4], False, tp_ == gt - 1,
                             [bc, bsp], [pb2])
                    P.cp("dve", negcK[:, gt * 4:(gt + 1) * 4], ps2[:, 0:4], [pb2], [negcKb])
                par = 0 if g in G_PAR[0] else 1
                s_ = G_PAR[par].index(g)
                ps, pb = self.ps_next()
                for t in range(4):
                    gt = g * 4 + t
                    P.mm(ps[0:4, t * 128:(t + 1) * 128], negcK[:, gt * 4:(gt + 1) * 4], self.ident[:], True, True,
                         [negcKb, bc], [pb])
                P.stt("dve", cqa[:, s_ * 512:(s_ + 1) * 512], ps[0:4, :], self.sel8[0:4, par:par + 1],
                      cqa[:, s_ * 512:(s_ + 1) * 512], ALU.mult, ALU.add, [pb, bc, cqab], [cqab])
            S.barrier()


    def stage_K(self, l, xfull_rows, W, negcK, negcKb, cqa, cqab):
        S, P = self.S, self
        bc = self.b_const
        dr = self.dr
        db = self.dbuf
        r3 = lambda name, m: W[name].ap().rearrange("p (k m) -> p k m", m=m)
        with ExitStack() as st:
            pools = self.mk_pools(st)
            RT = self.rope_tmp(st, 512)
            cosg_p = self.pool(st, "cosg", [128, 512], BF16, 2)
            sing_p = self.pool(st, "sing", [128, 512], BF16, 2)
            Wf, bWf = self.load_w(st, "Wf", [128, 8, 4], r3("wK_f", 4))
            Wsbk, bWsbk = self.load_w(st, "Wsbk", [128, 8, 256], r3("wK_sbk", 256))
            Wfxk, bWfxk = self.load_w(st, "Wfxk", [128, 8, 256], r3("wK_fxk", 256))
            Wv2, bWv2 = self.load_w(st, "Wv2", [128, 8, 512], r3("wK_v2", 512))
            Wckv, bWckv = self.load_w(st, "Wckv", [128, 8, 128], r3("wK_ckv", 128))
            Wkr, bWkr = self.load_w(st, "Wkr", [128, 8, 128], r3("wK_kr", 128))
            Wkrr, bWkrr = self.load_w(st, "Wkrr", [128, 8, 128], r3("wK_krr", 128))
            S.op("dve", lambda e: e.tensor_scalar_mul(out=Wkrr[:, :, 64:80], in0=Wkrr[:, :, 64:80], scalar1=-1.0),
                 [bWkrr], [bWkrr])
            ukf, bukf = self.load_w(st, "ukf", [128, 512], None, cast=False, skip=True)
            kvn, bkvn = self.load_w(st, "kvn", [128, 1], W["kvn"].ap(), cast=False)
            P.dma("sp", "w_ukf", ukf[:, 0:256], W["w_ukv_k"].ap(), [], [bukf])
            P.dma("sp", "w_ukf", ukf[:, 256:512], W["w_ukv_v"].ap(), [], [bukf])
            Wukv = self.sb(st, "Wukv", [128, 512], BF16)
            bWukv = Buf("Wukv")
            P.ts("dve", Wukv[:], ukf[:], kvn[:, 0:1], None, ALU.mult, None, [bukf, bkvn], [bWukv])
            bfg, bbfg = self.load_w(st, "bfg", [128, 4], W["bfg"].ap().partition_broadcast(128), cast=False)
            xTg = self.pool(st, "xTg", [128, 8, 512], BF16, 2)
            ckvn_p = self.pool(st, "ckvn", [128, 1, 512], BF16, 2)
            kst_p = self.pool(st, "kst", [128, 4, 512], BF16, 2)
            krst_p = self.pool(st, "krst", [128, 512], BF16, 2)
            vst_p = self.pool(st, "vst", [128, 4, 256], BF16, 2)
            vst2_p = self.pool(st, "vst2", [128, 4, 512], BF16, 2)
            sm_p = self.pool(st, "smallK", [128, 16], F32, 4)
            spall = self.sb(st, "spall", [128, 32 * 4], F32)
            bsp = [Buf("sp%d" % g_) for g_ in range(8)]
            S.op("pool", lambda e: e.memset(cqa[:], 0.0), [], [cqab])
            f32p = pools["f32"]
            import os
            ksub = int(os.environ.get("KSUB", "9"))
            for g in range(8):
                cols = slice(g * 512, (g + 1) * 512)
                rows = xfull_rows(g)
                j, xT, xTb = xTg.next()
                self.transpose_tiles(pools, [rows[t * 128:(t + 1) * 128, :] for t in range(4)], xT, xTb, 0)
                for t in range(4):
                    gt = g * 4 + t
                    ps, pb = self.ps_next()
                    for dc in range(8):
                        P.mm(ps[:, 0:4], xT[:, dc, t * 128:(t + 1) * 128], Wf[:, dc, :], dc == 0, dc == 7,
                             [xTb, bWf], [pb])
                    j, sm, smb = sm_p.next()
                    P.tt("dve", sm[:, 0:4], ps[:, 0:4], bfg[:], ALU.add, [pb, bbfg], [smb])
                    P.act(sm[:, 4:8], sm[:, 0:4], AF.Exp, [smb], [smb], scale=-1.0)
                    P.act(spall[:, gt * 4:(gt + 1) * 4], sm[:, 4:8], AF.Ln, [smb, bc], [bsp[g]], bias=self.onec[:, 0:1])
                for (Wk, bWk, name) in ((Wsbk, bWsbk, "KTs"), (Wfxk, bWfxk, "KTf")):
                    j, kst, kstb = kst_p.next()
                    for pr in range(2):
                        ps, pb = self.ps_next()
                        for dc in range(8):
                            P.mm(ps[:], Wk[:, dc, pr * 128:(pr + 1) * 128], xT[:, dc, :], dc == 0, dc == 7,
                                 [bWk, xTb], [pb])
                        P.cp(self.evac_eng(), kst[:, pr, :], ps[:], [pb], [kstb])
                        P.dma("sp", "kst%d" % j, dr[name].ap()[2 * pr:2 * pr + 2, :, cols].rearrange("h p t -> (h p) t"),
                              kst[:, pr, :], [kstb], [db[name]])
                j, vst2, vst2b = vst2_p.next()
                for t in range(4):
                    ps, pb = self.ps_next()
                    for dc in range(8):
                        P.mm(ps[:], xT[:, dc, t * 128:(t + 1) * 128], Wv2[:, dc, :], dc == 0, dc == 7, [xTb, bWv2], [pb])
                    P.cp(self.evac_eng(), vst2[:, t, :], ps[:], [pb], [vst2b])
                P.dma("sp", "vst2a%d" % j, dr["Vs"].ap()[cols, :].rearrange("(t p) c -> p t c", p=128),
                      vst2[:, :, 0:256], [vst2b], [db["Vs"]])
                P.dma("sp", "vst2b%d" % j, dr["Vf"].ap()[cols, :].rearrange("(t p) c -> p t c", p=128),
                      vst2[:, :, 256:512], [vst2b], [db["Vf"]])
                ps, pb = self.ps_next()
                for dc in range(8):
                    P.mm(ps[:], Wckv[:, dc, :], xT[:, dc, :], dc == 0, dc == 7, [bWckv, xTb], [pb])
                j, ckvn, ckvnb = ckvn_p.next()
                self.rms_scale(pools, [ps], [pb], 128, ckvn, ckvnb)
                j, kst, kstb = kst_p.next()
                for h in range(4):
                    ps, pb = self.ps_next()
                    P.mm(ps[0:64, :], Wukv[:, h * 64:(h + 1) * 64], ckvn[:, 0, :], True, True, [bWukv, ckvnb], [pb])
                    P.cp(self.evac_eng(), kst[0:64, h, :], ps[0:64, :], [pb], [kstb])
                P.dma("sp", "kst%d" % j, dr["KTm"].ap()[:, 0:64, cols].rearrange("h p t -> p h t"), kst[0:64, :, :],
                      [kstb], [db["KTm"]])
                psa, pba = self.ps_next()
                for dc in range(8):
                    P.mm(psa[:], Wkr[:, dc, :], xT[:, dc, :], dc == 0, dc == 7, [bWkr, xTb], [pba])
                psb_, pbb = self.ps_next()
                for dc in range(8):
                    P.mm(psb_[:], Wkrr[:, dc, :], xT[:, dc, :], dc == 0, dc == 7, [bWkrr, xTb], [pbb])
                j1, t1, t1b = f32p.next()
                j2, t2, t2b = f32p.next()
                sl = slice(64, 96)
                jc, cosg, cosgb = cosg_p.next()
                js, sing, singb = sing_p.next()
                self.rope_tables(RT, self.pos_full.ap()[:, cols], 512, cosg[sl, :], sing[sl, :], cosgb)
                P.tt("dve", t1[sl, :], psa[sl, :], cosg[sl, :], ALU.mult, [pba, cosgb], [t1b])
                P.tt("dve", t2[sl, :], psb_[sl, :], sing[sl, :], ALU.mult, [pbb, cosgb], [t2b])
                j, krst, krstb = krst_p.next()
                P.tt("dve", krst[sl, :], t1[sl, :], t2[sl, :], ALU.add, [t1b, t2b], [krstb])
                for h in range(4):
                    P.dma("sp", "krst%d" % j, dr["KTm"].ap()[h, 64:96, cols], krst[sl, :], [krstb], [db["KTm"]])
                j, vst, vstb = vst_p.next()
                for tp in range(2):
                    ps, pb = self.ps_next()
                    for t2_ in range(2):
                        t = tp * 2 + t2_
                        P.mm(ps[:, t2_ * 256:(t2_ + 1) * 256], ckvn[:, 0, t * 128:(t + 1) * 128], Wukv[:, 256:512],
                             True, True, [ckvnb, bWukv], [pb])
                    P.cp(self.evac_eng(), vst[:, tp * 2:tp * 2 + 2, :],
                         ps[:].rearrange("p (t c) -> p t c", c=256), [pb], [vstb])
                P.dma("sp", "vst%d" % j, dr["Vm"].ap()[cols, :].rearrange("(t p) c -> p t c", p=128), vst[:],
                      [vstb], [db["Vm"]])
                for t in range(4):
                    gt = g * 4 + t
                    ps2, pb2 = self.ps_next()
                    P.mm(ps2[:, 0:4], self.UT[:], spall[:, gt * 4:(gt + 1) * 4], True, gt == 0, [bc, bsp[g]], [pb2])
                    for tp_ in range(gt):
                        P.mm(ps2[:, 0:4], self.ones_f[:], spall[:, tp_ * 4:(tp_ + 1) * 4], False, tp_ == gt - 1,
                             [bc, bsp[tp_ // 4]], [pb2])
                    P.cp("dve", negcK[:, gt * 4:(gt + 1) * 4], ps2[:, 0:4], [pb2], [negcKb])
                par = 0 if g in G_PAR[0] else 1
                s_ = G_PAR[par].index(g)
                ps, pb = self.ps_next()
                for t in range(4):
                    gt = g * 4 + t
                    P.mm(ps[0:4, t * 128:(t + 1) * 128], negcK[:, gt * 4:(gt + 1) * 4], self.ident[:], True, True,
                         [negcKb, bc], [pb])
                P.stt("dve", cqa[:, s_ * 512:(s_ + 1) * 512], ps[0:4, :], self.sel8[0:4, par:par + 1],
                      cqa[:, s_ * 512:(s_ + 1) * 512], ALU.mult, ALU.add, [pb, bc, cqab], [cqab])
            S.barrier()

    def stage_Q(self, l, xfull_rows, xloc_rows, W, actT, actTb, yT, yTb, negcK, negcKb, cqa, cqab):
        S, P = self.S, self
        bc = self.b_const
        dr, db = self.dr, self.dbuf
        r3 = lambda name, m: W[name].ap().rearrange("p (k m) -> p k m", m=m)
        xlT = actT
        if self.kv_exchange:
            self.stage_K2(l, W, xlT, actTb, cqa, cqab, xloc_rows)
        else:
            with ExitStack() as st:
                pools = self.mk_pools(st)
                for s in range(4):
                    self.transpose_tiles(pools, [xloc_rows(s * 4 + t) for t in range(4)], actT, actTb[s], s * 512)
                S.barrier()

        with ExitStack() as st:
            pools = self.mk_pools(st, xin=False)
            f32p, bfp = pools["f32"], pools["bf"]
            Wcv, bWcv = self.load_w(st, "Wcv", [128, 8, 512], r3("wQ_cv", 512))
            cw, bcw = self.load_w(st, "convw", [128, 2, 31], W["convw"].ap().rearrange("p (c j) -> p c j", j=31), cast=False)
            cb_, bcb = self.load_w(st, "convb", [128, 2], W["convb"].ap(), cast=False)
            cg, bcg = self.load_w(st, "clng", [128, 2], W["clng"].ap(), cast=False)
            cbt, bcbt = self.load_w(st, "clnb", [128, 2], W["clnb"].ap(), cast=False)
            dg = self.sb(st, "dg", [128, 2, 31, 128], BF16)
            bdg = Buf("dg")
            for cc in range(2):
                for j in range(31):
                    P.ts("dve", dg[:, cc, j, :], self.ident[:], cw[:, cc, j:j + 1], None, ALU.mult, None,
                         [bc, bcw], [bdg])
            hp = self.sb(st, "hp", [128, 2, 4, 544], BF16)
            bhp = [Buf("hp%d" % s) for s in range(4)]
            bhalo = Buf("halo")
            xa = self.sb(st, "xha", [128, 1024], F32)
            xb = self.sb(st, "xhb", [128, 1024], F32)
            bxa, bxb = Buf("xa"), Buf("xb")
            S.op("pool", lambda e: e.memset(xa[0:32, :], 0.0), [], [bxa])
            for s in range(4):
                ga, gb = G_PAR[0][s] - 1, G_PAR[1][s] - 1
                xfb = getattr(self, "xfull_bufs", None) or (lambda g_: [])
                if ga >= 0:
                    P.dma("sp", "xha", xa[s * 32:(s + 1) * 32, :], xfull_rows(ga)[480:512, :], xfb(ga), [bxa])
                P.dma("sp", "xhb", xb[s * 32:(s + 1) * 32, :], xfull_rows(gb)[480:512, :], xfb(gb), [bxb])
            P.ts("dve", xa[:], xa[:], self.sel[:, 0:1], None, ALU.mult, None, [bxa, bc], [bxa])
            P.stt("dve", xa[:], xb[:], self.sel[:, 1:2], xa[:], ALU.mult, ALU.add, [bxb, bc, bxa], [bxa])
            xhT = self.sb(st, "xhT", [128, 8, 128], BF16)
            bxhT = Buf("xhT")
            for dc in range(8):
                ps, pb = self.ps_next()
                P.tr(ps[:, 0:128], xa[:, dc * 128:(dc + 1) * 128], [bxa], [pb])
                P.cp(self.evac_eng(), xhT[:, dc, :], ps[:, 0:128], [pb], [bxhT])

            def glu(cc, rhs_fn, ncols, rbufs, out_ap, out_buf):
                psa, pba = self.ps_next()
                for dc in range(8):
                    P.mm(psa[:, 0:ncols], Wcv[:, dc, cc * 128:(cc + 1) * 128], rhs_fn(dc), dc == 0, dc == 7,
                         [bWcv] + rbufs, [pba])
                psg, pbg = self.ps_next()
                for dc in range(8):
                    P.mm(psg[:, 0:ncols], Wcv[:, dc, 256 + cc * 128:256 + (cc + 1) * 128], rhs_fn(dc), dc == 0, dc == 7,
                         [bWcv] + rbufs, [pbg])
                j, sg, sgb = f32p.next()
                P.act(sg[:, 0:ncols], psg[:, 0:ncols], AF.Sigmoid, [pbg], [sgb])
                P.tt("dve", out_ap, psa[:, 0:ncols], sg[:, 0:ncols], ALU.mult, [pba, sgb], [out_buf])

            for cc in range(2):
                j, hh, hhb = bfp.next()
                glu(cc, lambda dc: xhT[:, dc, :], 128, [bxhT], hh[:, 0:128], hhb)
                P.cp("dve", hp[:, cc, :, 0:32], hh[:, 0:128].rearrange("p (s t) -> p s t", t=32), [hhb], bhp)
                for s in range(4):
                    glu(cc, lambda dc, s=s: xlT[:, dc, s * 512:(s + 1) * 512], 512, [actTb[s]],
                        hp[:, cc, s, 32:544], bhp[s])
            for s in range(4):
                ys = []
                for cc in range(2):
                    ps, pb = self.ps_next(0, 4)
                    for j in range(31):
                        P.mm(ps[:], dg[:, cc, j, :], hp[:, cc, s, 2 + j:2 + j + 512], j == 0, j == 30, [bdg, bhp[s]], [pb])
                    jj, y, yb = f32p.next()
                    P.ts("dve", y[:], ps[:], cb_[:, cc:cc + 1], None, ALU.add, None, [pb, bcb], [yb])
                    ys.append((y, yb))
                pss, pbs = self.ps_next(4, 8)
                psq, pbq = self.ps_next(4, 8)
                for cc, (y, yb) in enumerate(ys):
                    j1, ybf, ybfb = bfp.next()
                    j2, ysq, ysqb = bfp.next()
                    P.cp("dve", ybf[:], y[:], [yb], [ybfb])
                    P.act(ysq[:], y[:], AF.Square, [yb], [ysqb])
                    P.mm(pss[:], self.ones_bf[:], ybf[:], cc == 0, cc == 1, [bc, ybfb], [pbs])
                    P.mm(psq[:], self.ones_bf[:], ysq[:], cc == 0, cc == 1, [bc, ysqb], [pbq])
                jm, mean, meanb = f32p.next()
                P.act(mean[:], pss[:], AF.Copy, [pbs], [meanb], scale=1.0 / 256)
                jv, var, varb = f32p.next()
                P.act(var[:], mean[:], AF.Square, [meanb], [varb])
                P.stt("dve", var[:], psq[:], 1.0 / 256, var[:], ALU.mult, ALU.subtract, [pbq, varb], [varb])
                P.act(var[:], var[:], AF.Sqrt, [varb, bc], [varb], bias=self.epsc[:, 0:1])
                S.op("dve", lambda e, o=var[:]: e.reciprocal(out=o, in_=o), [varb], [varb])
                for cc, (y, yb) in enumerate(ys):
                    P.tt("dve", y[:], y[:], mean[:], ALU.subtract, [yb, meanb], [yb])
                    P.tt("dve", y[:], y[:], var[:], ALU.mult, [yb, varb], [yb])
                    P.act(yT[2][:, cc, s * 512:(s + 1) * 512], y[:], AF.Silu, [yb, bcg, bcbt], [yTb[2][s]],
                          bias=cbt[:, cc:cc + 1], scale=cg[:, cc:cc + 1])
            S.barrier()

        if self.kv_exchange:
            self.stage_C(l, negcK, negcKb, cqa, cqab)
        import os
        if int(os.environ.get("KSTOP", "9")) < 3:
            return
        self.attn_mla(l, W, xlT, actTb, yT[0], yTb[0])
        self.attn_sbfox(l, W, xlT, actTb, yT[1], yTb[1], "sb", None, None, None, None)
        self.attn_sbfox(l, W, xlT, actTb, yT[3], yTb[3], "fox", negcK, negcKb, cqa, cqab)

    def stage_G(self, l, W, xlT, actTb, yT, yTb, mrgT, mrgTb):
        S, P = self.S, self
        with ExitStack() as st:
            pools = self.mk_pools(st, xin=False)
            f32p = pools["f32"]
            Wb, bWb = self.load_w(st, "Wb", [128, 4, 2, 1024], W["wB"].ap().rearrange("p (n c d) -> p n c d", n=4, c=2))
            bg, bbg = self.load_w(st, "bg", [128, 32], W["bg"].ap(), cast=False)
            wg_p = self.pool(st, "wg", [128, 8, 128], BF16, 3)
            acc = self.sb(st, "gacc", [128, 4, 512], F32)
            baccs = [Buf("gacc%d" % s) for s in range(4)]
            for dc in range(8):
                for n in range(4):
                    ci = dc * 4 + n
                    j, wg, wgb = wg_p.next()
                    P.dma("pool", "wg%d" % j, wg[:], W["wG"].ap()[ci].rearrange("p (k m) -> p k m", m=128), [], [wgb])
                    for s in range(4):
                        cols = slice(s * 512, (s + 1) * 512)
                        psg, pbg = self.ps_next()
                        for kc in range(8):
                            P.mm(psg[:], wg[:, kc, :], xlT[:, kc, cols], kc == 0, kc == 7, [wgb, actTb[s]], [pbg])
                        jg, gs_, gsb = f32p.next()
                        P.act(gs_[:], psg[:], AF.Sigmoid, [pbg, bbg], [gsb], bias=bg[:, ci:ci + 1])
                        psp, pbp = self.ps_next()
                        for cc in range(2):
                            P.mm(psp[:], Wb[:, n, cc, dc * 128:(dc + 1) * 128], yT[n][:, cc, cols], cc == 0, cc == 1,
                                 [bWb, yTb[n][s]], [pbp])
                        if n == 0:
                            P.tt("dve", acc[:, s, :], gs_[:], psp[:], ALU.mult, [gsb, pbp], [baccs[s]])
                        else:
                            P.tt("dve", gs_[:], gs_[:], psp[:], ALU.mult, [gsb, pbp], [gsb])
                            if n < 3:
                                P.tt("dve", acc[:, s, :], acc[:, s, :], gs_[:], ALU.add, [baccs[s], gsb], [baccs[s]])
                            else:
                                P.tt("dve", mrgT[:, dc, cols], acc[:, s, :], gs_[:], ALU.add, [baccs[s], gsb], [mrgTb[s]])
            S.barrier()

    def make_pen(self, st, pools, strict):
        S, P = self.S, self
        bc = self.b_const
        Dm = self.D1 if strict else self.D0
        penb = self.sb(st, "penb", [128, 16, 512], BF16)
        penbb = Buf("penb")
        f32p = pools["f32"]
        for i in range(16):
            jt, tmp, tmpb = f32p.next()
            P.ts("dve", tmp[:], Dm[:], self.thr[:, i:i + 1], 0.0, ALU.subtract, ALU.max, [bc], [tmpb])
            P.ts("dve", penb[:, i, :], tmp[:], NEGBIG, None, ALU.mult, None, [tmpb], [penbb])
        return penb, penbb

    def attn_core(self, kind, pools, KT, KTb, kdim, QT, QTb, qtn_p, Vt, Vtb, h, s, scale, yTn, yTnb, penb, penbb,
                  negcK=None, negcKb=None, rs_p=None):
        S, P = self.S, self
        bc = self.b_const
        f32p, bfp = pools["f32"], pools["bf"]
        NB = 8 * (s + 1)
        qc = slice(s * 512, (s + 1) * 512)
        cls = s % 2
        hp = h // 2
        acc_o, bo = self.ps_next(4, 8)
        order = list(range(NB))
        QTn = QTnb = None
        if kind == "sb":
            order = order[::-1]
            jq, QTn, QTnb = qtn_p.next()
            P.ts("dve", QTn[0:kdim, :], QT[0:kdim, h, qc], -0.125, None, ALU.mult, None, [QTb[s]], [QTnb])
        rs_state = [None]
        pv_pend = []

        def stage1(i):
            kb = order[i]
            kc = slice(kb * 128, (kb + 1) * 128)
            masked = kb >= 8 * s
            ps, pb = self.ps_next(0, 4)
            P.mm(ps[:], KT[0:kdim, kc], QT[0:kdim, h, qc], True, True, [KTb, QTb[s]], [pb])
            src, srcb = ps, pb
            pj = None
            if masked:
                pj = cls * 8 + (kb - 8 * s)
                jz, zm, zmb = f32p.next()
                P.tt("dve", zm[:], ps[:], penb[:, pj, :], ALU.add, [pb, penbb], [zmb])
                src, srcb = zm, zmb
            if kind != "sb":
                jP, Pt, Ptb = bfp.next()
                if kind == "fox":
                    P.act(Pt[:], src[:], AF.Exp, [srcb, negcKb], [Ptb], scale=scale,
                          bias=negcK[:, kb * 4 + h:kb * 4 + h + 1])
                else:
                    P.act(Pt[:], src[:], AF.Exp, [srcb], [Ptb], scale=scale)
                return (kb, kc, pj, Pt, Ptb)
            je, ee, eeb = f32p.next()
            P.act(ee[:], src[:], AF.Exp, [srcb], [eeb], scale=scale)
            jsp, sp, spb = bfp.next()
            P.act(sp[:], ee[:], AF.Ln, [eeb, bc], [spb], bias=self.onec[:, 0:1])
            return (kb, kc, pj, sp, spb)

        def stage2(i, st1):
            kb, kc, pj, t, tb = st1
            first, last = (i == 0), (i == NB - 1)
            if kind != "sb":
                P.mm(acc_o[:], Vt[:, kb, :], t[:], first, last, [Vtb, tb], [bo])
                return
            sp, spb = t, tb
            prev_rs = rs_state[0]
            psG, pbG = self.ps_next(0, 4)
            P.mm(psG[:], self.LT[:], sp[:], True, False, [bc, spb], [pbG])
            if prev_rs is not None:
                P.mm(psG[:], self.ones_bf[:], prev_rs[0][:], False, False, [bc, prev_rs[1]], [pbG])
            P.mm(psG[:], KT[0:kdim, kc], QTn[0:kdim, :], False, True, [KTb, QTnb], [pbG])
            srcG, srcGb = psG, pbG
            if pj is not None:
                jg, gm, gmb = f32p.next()
                P.tt("dve", gm[:], psG[:], penb[:, pj, :], ALU.subtract, [pbG, penbb], [gmb])
                srcG, srcGb = gm, gmb
            jA, At, Atb = bfp.next()
            P.act(At[:], srcG[:], AF.Exp, [srcGb], [Atb], scale=-1.0)
            pv_pend.append((kb, At, Atb, first, last))
            if not last:
                jr, rs, rsb = rs_p.next()
                if prev_rs is None:
                    P.cp("dve", rs[:], sp[:], [spb], [rsb])
                else:
                    P.tt("dve", rs[:], prev_rs[0][:], sp[:], ALU.add, [prev_rs[1], spb], [rsb])
                rs_state[0] = (rs, rsb)

        def stage3():
            kb, At, Atb, first, last = pv_pend.pop(0)
            P.mm(acc_o[:], Vt[:, kb, hp * 128:(hp + 1) * 128], At[:], first, last, [Vtb, Atb], [bo])

        DEPTH_PIPE = 2 if kind != "sb" else 1
        pend = []
        for i in range(NB):
            pend.append((i, stage1(i)))
            if len(pend) > DEPTH_PIPE:
                i0, st0 = pend.pop(0)
                stage2(i0, st0)
            if len(pv_pend) > 1:
                stage3()
        for i0, st0 in pend:
            stage2(i0, st0)
        while pv_pend:
            stage3()
        po = (h % 2) * 64
        dst = yTn[po:po + 64, h // 2, qc]
        if kind == "sb":
            P.cp("dve", dst, acc_o[po:po + 64, :], [bo], [yTnb[s]])
        else:
            jr, rec, recb = f32p.next()
            S.op("dve", lambda e, o=rec[0:64, :], i=acc_o[64:128, :]: e.reciprocal(out=o, in_=i), [bo], [recb])
            P.tt("dve", dst, acc_o[0:64, :], rec[0:64, :], ALU.mult, [bo, recb], [yTnb[s]])

    def kv_src(self, kind, h, g):
        X = self.xch
        r = 0 if g in G_PAR[0] else 1
        s_ = G_PAR[r].index(g)
        base, kd = {"mla": (0, 96), "sb": (384, 64), "fox": (640, 64)}[kind]
        r0 = r * 896 + base + h * kd
        return X["KVg"][s_].ap()[r0:r0 + kd, :], X["bKVg"][s_]

    def v_src(self, kind, g, c0, c1):
        X = self.xch
        r = 0 if g in G_PAR[0] else 1
        s_ = G_PAR[r].index(g)
        cb = {"mla": 0, "sb": 256, "fox": 512}[kind]
        return (X["Vg"][s_].ap()[r * 512:(r + 1) * 512, cb + c0:cb + c1].rearrange("(t p) c -> p t c", p=128),
                X["bVg"][s_])

    def attn_mla(self, l, W, xlT, actTb, yTn, yTnb):
        S, P = self.S, self
        bc = self.b_const
        dr, db = self.dr, self.dbuf
        with ExitStack() as st:
            pools = self.mk_pools(st, xin=False)
            f32p = pools["f32"]
            Wcq, bWcq = self.load_w(st, "Wcq", [128, 8, 256], W["wQ_cq"].ap().rearrange("p (k m) -> p k m", m=256))
            qn, bqn = self.load_w(st, "qn", [128, 2], W["qn"].ap(), cast=False)
            Wuq = self.sb(st, "Wuq", [128, 2, 384], BF16)
            Wuqr = self.sb(st, "Wuqr", [128, 2, 384], BF16)
            bWuq, bWuqr = Buf("Wuq"), Buf("Wuqr")
            with ExitStack() as st2:
                uf, buf_ = self.load_w(st2, "uqf", [128, 2, 384], W["w_uq"].ap().rearrange("p (k m) -> p k m", m=384), cast=False)
                ur, bur = self.load_w(st2, "uqrf", [128, 2, 384], W["w_uqr"].ap().rearrange("p (k m) -> p k m", m=384), cast=False)
                for rc in range(2):
                    P.ts("dve", Wuq[:, rc, :], uf[:, rc, :], qn[:, rc:rc + 1], None, ALU.mult, None, [buf_, bqn], [bWuq])
                    P.ts("dve", Wuqr[:, rc, :], ur[:, rc, :], qn[:, rc:rc + 1], None, ALU.mult, None, [bur, bqn], [bWuqr])
                    for h in range(4):
                        o = h * 96 + 64
                        P.ts("dve", Wuqr[:, rc, o:o + 16], Wuqr[:, rc, o:o + 16], -1.0, None, ALU.mult, None,
                             [bWuqr], [bWuqr])
                S.barrier()
            QT = self.sb(st, "QTm", [128, 4, TL], BF16)
            QTb = [Buf("QTm%d" % s) for s in range(4)]
            cqn_p = self.pool(st, "cqn", [128, 2, 512], BF16, 2)
            sl = slice(64, 96)
            for s in range(4):
                qc = slice(s * 512, (s + 1) * 512)
                tc_ = slice(SEQ + s * 512, SEQ + (s + 1) * 512)
                pss, pbs = [], []
                for rc in range(2):
                    ps, pb = self.ps_next(0, 4)
                    for dc in range(8):
                        P.mm(ps[:], Wcq[:, dc, rc * 128:(rc + 1) * 128], xlT[:, dc, qc], dc == 0, dc == 7,
                             [bWcq, actTb[s]], [pb])
                    pss.append(ps)
                    pbs.append(pb)
                j, cqn, cqnb = cqn_p.next()
                self.rms_scale(pools, pss, pbs, 256, cqn, cqnb, ps_range=(4, 8))
                for h in range(4):
                    psa, pba = self.ps_next(4, 8)
                    psb_, pbb = self.ps_next(4, 8)
                    for rc in range(2):
                        P.mm(psa[0:96, :], Wuq[:, rc, h * 96:(h + 1) * 96], cqn[:, rc, :], rc == 0, rc == 1,
                             [bWuq, cqnb], [pba])
                    for rc in range(2):
                        P.mm(psb_[0:96, :], Wuqr[:, rc, h * 96:(h + 1) * 96], cqn[:, rc, :], rc == 0, rc == 1,
                             [bWuqr, cqnb], [pbb])
                    P.cp("act", QT[0:64, h, qc], psa[0:64, :], [pba], [QTb[s]])
                    j1, t1, t1b = f32p.next()
                    j2, t2, t2b = f32p.next()
                    P.tt("dve", t1[sl, :], psa[sl, :], self.cosL[sl, qc], ALU.mult, [pba, self.b_rope], [t1b])
                    P.tt("dve", t2[sl, :], psb_[sl, :], self.sinL[sl, qc], ALU.mult, [pbb, self.b_rope], [t2b])
                    P.tt("dve", QT[sl, h, qc], t1[sl, :], t2[sl, :], ALU.add, [t1b, t2b], [QTb[s]])
            vh_p = self.pool(st, "Vh", [128, 32, 128], BF16, 2)
            for (tile_, b_) in zip(vh_p.tiles, vh_p.bufs):
                S.op("pool", lambda e, t=tile_: e.memset(t[:, :, 64:128], 1.0), [], [b_])
            kt_p = self.pool(st, "KT", [128, SEQ], BF16, 2)
            penb, penbb = self.make_pen(st, pools, False)
            for h in range(4):
                j, KT, KTb = kt_p.next()
                jv, Vt, Vtb = vh_p.next()
                if self.kv_exchange:
                    for g in range(8):
                        ka, kb_ = self.kv_src("mla", h, g)
                        P.dma("sp", "KT%d" % j, KT[0:96, g * 512:(g + 1) * 512], ka, [kb_], [KTb])
                        va, vb_ = self.v_src("mla", g, h * 64, (h + 1) * 64)
                        P.dma("sp", "Vh%d" % jv, Vt[:, g * 4:(g + 1) * 4, 0:64], va, [vb_], [Vtb])
                else:
                    P.dma("sp", "KT%d" % j, KT[0:96, :], dr["KTm"].ap()[h], [db["KTm"]], [KTb])
                    for q8 in range(8):
                        P.dma("sp", "Vh%d" % jv, Vt[:, q8 * 4:(q8 + 1) * 4, 0:64],
                              dr["Vm"].ap()[q8 * 512:(q8 + 1) * 512, h * 64:(h + 1) * 64].rearrange("(t p) c -> p t c", p=128),
                              [db["Vm"]], [Vtb])
                for s in range(4):
                    self.attn_core("mla", pools, KT, KTb, 96, QT, QTb, None, Vt, Vtb, h, s, float(96 ** -0.5), yTn, yTnb,
                                   penb, penbb)
            S.barrier()

    def attn_sbfox(self, l, W, xlT, actTb, yTn, yTnb, kind, negcK, negcKb, cqa, cqab):
        S, P = self.S, self
        bc = self.b_const
        dr, db = self.dr, self.dbuf
        fox = kind == "fox"
        kdim = 65
        with ExitStack() as st:
            pools = self.mk_pools(st, xin=False)
            Wq = self.sb(st, "Wq" + kind, [128, 8, 4, 65], BF16)
            bWq = Buf("Wq")
            S.op("pool", lambda e: e.memset(Wq[:], 0.0), [], [bWq])
            P.dma("pool", "w_Wq", Wq[:, :, :, 0:64],
                  W["wQ_fxq" if fox else "wQ_sbq"].ap().rearrange("p (k h m) -> p k h m", h=4, m=64), [], [bWq])
            QT = self.sb(st, "QT" + kind, [128, 4, TL], BF16)
            QTb = [Buf("QT%d" % s) for s in range(4)]
            qtn_p = None if fox else self.pool(st, "QTn", [128, 512], BF16, 2)
            for s in range(4):
                qc = slice(s * 512, (s + 1) * 512)
                for h in range(4):
                    ps, pb = self.ps_next(0, 4)
                    for dc in range(8):
                        P.mm(ps[0:kdim, :], Wq[:, dc, h, 0:kdim], xlT[:, dc, qc], dc == 0, (dc == 7) and not fox,
                             [bWq, actTb[s]], [pb])
                    if fox:
                        P.mm(ps[0:kdim, :], self.Eh[0:4, h * 65:(h + 1) * 65], cqa[0:4, qc], False, True,
                             [bc, cqab], [pb])
                    P.cp("act", QT[0:kdim, h, qc], ps[0:kdim, :], [pb], [QTb[s]])
            vname, kname = ("Vf", "KTf") if fox else ("Vs", "KTs")
            if fox:
                vh_p = self.pool(st, "Vhf", [128, 32, 128], BF16, 2)
                for (tile_, b_) in zip(vh_p.tiles, vh_p.bufs):
                    S.op("pool", lambda e, t=tile_: e.memset(t[:, :, 64:128], 1.0), [], [b_])
            else:
                Vt = self.sb(st, "Vt" + kind, [128, 32, 256], BF16)
                Vtb = Buf("Vt")
                for q8 in range(8):
                    if self.kv_exchange:
                        va, vb_ = self.v_src("sb", q8, 0, 256)
                        P.dma("sp", "Vt", Vt[:, q8 * 4:(q8 + 1) * 4, :], va, [vb_], [Vtb])
                    else:
                        P.dma("sp", "Vt", Vt[:, q8 * 4:(q8 + 1) * 4, :],
                              dr[vname].ap()[q8 * 512:(q8 + 1) * 512, :].rearrange("(t p) c -> p t c", p=128),
                              [db[vname]], [Vtb])
            kt_p = self.pool(st, "KT" + kind, [128, SEQ], BF16, 2)
            for (tile_, b_) in zip(kt_p.tiles, kt_p.bufs):
                S.op("pool", lambda e, t=tile_: e.memset(t[64:65, :], 1.0 if fox else 0.0), [], [b_])
            rs_p = None if fox else self.pool(st, "rsum", [128, 512], BF16, 2)
            penb, penbb = self.make_pen(st, pools, not fox)
            for h in range(4):
                j, KT, KTb = kt_p.next()
                if self.kv_exchange:
                    for g in range(8):
                        ka, kb_ = self.kv_src(kind, h, g)
                        P.dma("sp", "KT%d" % j, KT[0:64, g * 512:(g + 1) * 512], ka, [kb_], [KTb])
                else:
                    P.dma("sp", "KT%d" % j, KT[0:64, :], dr[kname].ap()[h, 0:64, :], [db[kname]], [KTb])
                if fox:
                    jv, Vt, Vtb = vh_p.next()
                    for q8 in range(8):
                        if self.kv_exchange:
                            va, vb_ = self.v_src("fox", q8, h * 64, (h + 1) * 64)
                            P.dma("sp", "Vh%d" % jv, Vt[:, q8 * 4:(q8 + 1) * 4, 0:64], va, [vb_], [Vtb])
                        else:
                            P.dma("sp", "Vh%d" % jv, Vt[:, q8 * 4:(q8 + 1) * 4, 0:64],
                                  dr[vname].ap()[q8 * 512:(q8 + 1) * 512, h * 64:(h + 1) * 64].rearrange("(t p) c -> p t c", p=128),
                                  [db[vname]], [Vtb])
                for s in range(4):
                    self.attn_core(kind, pools, KT, KTb, kdim, QT, QTb, qtn_p, Vt, Vtb, h, s, 0.125, yTn, yTnb,
                                   penb, penbb, negcK=negcK, negcKb=negcKb, rs_p=rs_p)
            S.barrier()

    def ln_a(self, r, rb, smp, junk_p):
        S, P = self.S, self
        j, sm, smb = smp.next()
        jj, junk, junkb = junk_p.next()
        S.op("dve", lambda e, o=sm[:, 0:1], i=r[:]: e.reduce_sum(out=o, in_=i, axis=AX.X), [rb], [smb])
        P.ts("dve", sm[:, 1:2], sm[:, 0:1], -1.0 / D, None, ALU.mult, None, [smb], [smb])
        P.act(r[:], r[:], AF.Identity, [rb, smb], [rb], bias=sm[:, 1:2])
        P.act(junk[:], r[:], AF.Square, [rb], [junkb])
        return (sm, smb, junk, junkb)

    def ln_b(self, r, rb, gbc, bbc, gb_bufs, st_):
        S, P = self.S, self
        sm, smb, junk, junkb = st_
        S.op("dve", lambda e, o=sm[:, 2:3], i=junk[:]: e.reduce_sum(out=o, in_=i, axis=AX.X), [junkb], [smb])
        P.act(sm[:, 3:4], sm[:, 2:3], AF.Sqrt, [smb, self.b_const], [smb], bias=self.epsc[:, 0:1], scale=1.0 / D)
        S.op("dve", lambda e, o=sm[:, 4:5], i=sm[:, 3:4]: e.reciprocal(out=o, in_=i), [smb], [smb])
        P.stt("dve", r[:], r[:], sm[:, 4:5], gbc[:], ALU.mult, ALU.mult, [rb, smb] + gb_bufs, [rb])
        P.tt("pool", r[:], r[:], bbc[:], ALU.add, [rb] + gb_bufs, [rb])

    def stage_F1(self, l, xloc_rows, W, actT, actTb, mrgT, mrgTb, WtT, WtTb):
        S, P = self.S, self
        bc = self.b_const
        dr, db = self.dr, self.dbuf
        with ExitStack() as st:
            Wo, bWo = self.load_w(st, "Wo", [128, 8, 1024], W["wO"].ap().rearrange("p (k m) -> p k m", m=1024))
            g1, bg1 = self.load_w(st, "ln1g", [128, D], W["ln1_g"].ap().partition_broadcast(128), cast=False)
            b1, bb1 = self.load_w(st, "ln1b", [128, D], W["ln1_b"].ap().partition_broadcast(128), cast=False)
            wR, bwR = self.load_w(st, "wR", [128, 8, 36], W["wR"].ap().rearrange("p (k m) -> p k m", m=36), cast=False)
            bR, bbR = self.load_w(st, "bR", [128, 36], W["bR"].ap().partition_broadcast(128), cast=False)
            xr_p = self.pool(st, "xr", [128, D], F32, 3)
            r_p = self.pool(st, "r", [128, D], F32, 4)
            hTf_p = self.pool(st, "hTf", [128, 8, 128], F32, 3)
            sm_p = self.pool(st, "smF", [128, 128], F32, 4)
            sm2_p = self.pool(st, "smF2", [128, 64], F32, 4)
            smln = self.pool(st, "smln", [128, 8], F32, 4)
            junk_p = self.pool(st, "junk", [128, D], F32, 2)
            gbb = Buf("gb")
            def phaseA(tile):
                s = tile // 4
                tcs = slice(tile * 128, (tile + 1) * 128)
                j, xr, xrb = xr_p.next()
                P.dma("sp", "xr%d" % j, xr[:], xloc_rows(tile), [], [xrb])
                jr, r, rb = r_p.next()
                for half in range(2):
                    ps, pb = self.ps_next()
                    for kc in range(8):
                        P.mm(ps[:], mrgT[:, kc, tcs], Wo[:, kc, half * 512:(half + 1) * 512], kc == 0, kc == 7,
                             [mrgTb[s], bWo], [pb])
                    P.stt("dve", r[:, half * 512:(half + 1) * 512], xr[:, half * 512:(half + 1) * 512], ALPHA, ps[:],
                          ALU.mult, ALU.add, [xrb, pb], [rb])
                lnst = self.ln_a(r, rb, smln, junk_p)
                return dict(tile=tile, s=s, tcs=tcs, r=r, rb=rb, jr=jr, lnst=lnst)

            def phaseB(st_):
                tile, s, tcs, r, rb, jr = st_["tile"], st_["s"], st_["tcs"], st_["r"], st_["rb"], st_["jr"]
                self.ln_b(r, rb, g1, b1, [bg1, bb1], st_["lnst"])
                P.dma("sp", "r%d" % jr, dr["h"].ap()[tile * 128:(tile + 1) * 128, :], r[:], [rb], [db["h"]])
                jh, hTf, hTfb = hTf_p.next()
                for q in range(2):
                    ps, pb = self.ps_next()
                    for i in range(4):
                        dc = q * 4 + i
                        P.tr(ps[:, i * 128:(i + 1) * 128], r[:, dc * 128:(dc + 1) * 128], [rb], [pb])
                    P.cp("act", hTf[:, q * 4:(q + 1) * 4, :], ps[:].rearrange("p (i t) -> p i t", t=128), [pb], [hTfb])
                    P.cp("dve", actT[:, q * 4:(q + 1) * 4, tcs], hTf[:, q * 4:(q + 1) * 4, :], [hTfb],
                         [actTb[s]])
                st_["hTf"], st_["hTfb"] = hTf, hTfb

            def phaseC(st_):
                s, tcs, hTf, hTfb = st_["s"], st_["tcs"], st_["hTf"], st_["hTfb"]
                ps, pb = self.ps_next()
                for kc in range(8):
                    P.mm(ps[:, 0:36], hTf[:, kc, :], wR[:, kc, :], kc == 0, kc == 7, [hTfb, bwR], [pb])
                j, sm, smb = sm_p.next()
                j2, s2, s2b = sm2_p.next()
                B = [smb]
                P.tt("dve", sm[:, 0:36], ps[:, 0:36], bR[:], ALU.add, [pb, bbR], B)
                c = lambda i: sm[:, i:i + 1]
                S.op("dve", lambda e, o=c(112), i=sm[:, 0:4]: e.reduce_max(out=o, in_=i, axis=AX.X), B, B)
                P.ts("dve", c(113), c(112), -1.0, None, ALU.mult, None, B, B)
                P.act(sm[:, 36:40], sm[:, 0:4], AF.Exp, B, B, bias=c(113))
                S.op("dve", lambda e, o=c(114), i=sm[:, 36:40]: e.reduce_sum(out=o, in_=i, axis=AX.X), B, B)
                S.op("dve", lambda e, o=c(115), i=c(114): e.reciprocal(out=o, in_=i), B, B)
                P.ts("dve", sm[:, 40:44], sm[:, 0:4], c(112), None, ALU.is_equal, None, B, B)
                P.ts("dve", sm[:, 44:48], sm[:, 40:44], 1.0, 1e9, ALU.subtract, ALU.mult, B, B)
                for g in range(4):
                    P.ts("dve", sm[:, 48 + g * 8:56 + g * 8], sm[:, 4 + g * 8:12 + g * 8], sm[:, 44 + g:45 + g], None,
                         ALU.add, None, B, B)
                S.op("dve", lambda e, o=c(116), i=sm[:, 48:80]: e.reduce_max(out=o, in_=i, axis=AX.X), B, B)
                P.ts("dve", s2[:, 0:32], sm[:, 48:80], c(116), None, ALU.is_equal, None, B + [s2b], [s2b])
                P.stt("dve", sm[:, 80:112], s2[:, 0:32], -1e9, sm[:, 48:80], ALU.mult, ALU.add, B + [s2b], B)
                S.op("dve", lambda e, o=c(118), i=sm[:, 80:112]: e.reduce_max(out=o, in_=i, axis=AX.X), B, B)
                P.ts("dve", s2[:, 32:64], sm[:, 80:112], c(118), None, ALU.is_equal, None, B + [s2b], [s2b])
                P.ts("dve", c(117), c(116), -1.0, None, ALU.mult, None, B, B)
                P.act(c(119), c(118), AF.Exp, B, B, bias=c(117))
                P.ts("dve", c(120), c(119), 1.0, None, ALU.add, None, B, B)
                S.op("dve", lambda e, o=c(120): e.reciprocal(out=o, in_=o), B, B)
                P.tt("dve", c(121), c(115), c(120), ALU.mult, B, B)
                P.tt("dve", c(122), c(121), c(119), ALU.mult, B, B)
                P.ts("dve", s2[:, 0:32], s2[:, 0:32], c(121), None, ALU.mult, None, B + [s2b], [s2b])
                P.stt("dve", s2[:, 0:32], s2[:, 32:64], c(122), s2[:, 0:32], ALU.mult, ALU.add, B + [s2b], [s2b])
                ps, pb = self.ps_next()
                P.mm(ps[0:32, 0:128], s2[:, 0:32], self.ident[:], True, True, [s2b, bc], [pb])
                P.cp("act", WtT[0:32, tcs], ps[0:32, 0:128], [pb], [WtTb[s]])

            states = {}
            for step in range(16 + 2):
                if step < 16:
                    states[step] = phaseA(step)
                if 0 <= step - 1 < 16:
                    phaseB(states[step - 1])
                if 0 <= step - 2 < 16:
                    phaseC(states.pop(step - 2))
            S.barrier()

    def stage_F2(self, l, out_rows, W, actT, actTb, WtT, WtTb):
        S, P = self.S, self
        bc = self.b_const
        dr, db = self.dr, self.dbuf
        hT = actT
        with ExitStack() as st:
            acc = self.sb(st, "acc", [128, 16, D], F32)
            accb = [Buf("acc%d" % t) for t in range(16)]
            with ExitStack() as st2:
                hid_p = self.pool(st2, "hid", [128, 2, TL], BF16, 3)
                wgu_p = self.pool(st2, "wgu", [128, 8, 512], BF16, 2)
                wd_p = self.pool(st2, "wd", [128, 2, D], BF16, 3)
                wbc_p = self.pool(st2, "wbc", [128, TL], BF16, 1)
                sg_p = self.pool(st2, "sg", [128, 512], F32, 3)
                t_p = self.pool(st2, "tF", [128, 512], F32, 3)
                pend_down = [None]

                def down(eg, hids, wds):
                    for tile in range(16):
                        tcs = slice(tile * 128, (tile + 1) * 128)
                        for half in range(2):
                            ps, pb = self.ps_next()
                            k = 0
                            for ei in range(2):
                                for fc in range(2):
                                    P.mm(ps[:], hids[ei][0][:, fc, tcs], wds[ei][0][:, fc, half * 512:(half + 1) * 512],
                                         k == 0, k == 3, [hids[ei][1], wds[ei][1]], [pb])
                                    k += 1
                            dst = acc[:, tile, half * 512:(half + 1) * 512]
                            if eg == 0:
                                P.cp("dve", dst, ps[:], [pb], [accb[tile]])
                            else:
                                P.tt("dve", dst, dst, ps[:], ALU.add, [pb, accb[tile]], [accb[tile]])

                for eg in range(16):
                    hids, wds = [], []
                    for ei in range(2):
                        e_ = eg * 2 + ei
                        j, wgu, wgub = wgu_p.next()
                        P.dma("pool", "wgu%d" % j, wgu[:, :, 0:256], W["wEg"].ap()[e_].rearrange("p (k m) -> p k m", m=256),
                              [], [wgub])
                        P.dma("pool", "wgu%d" % j, wgu[:, :, 256:512], W["wEu"].ap()[e_].rearrange("p (k m) -> p k m", m=256),
                              [], [wgub])
                        j, wd, wdb = wd_p.next()
                        P.dma("pool", "wd%d" % j, wd[:], W["wEd"].ap()[e_].rearrange("p (k m) -> p k m", m=D), [], [wdb])
                        jw, wbc, wbcb = wbc_p.next()
                        for s in range(4):
                            cs = slice(s * 512, (s + 1) * 512)
                            ps, pb = self.ps_next()
                            P.mm(ps[:], self.selE[0:32, e_ * 128:(e_ + 1) * 128], WtT[0:32, cs], True, True,
                                 [bc, WtTb[s]], [pb])
                            P.cp("act", wbc[:, cs], ps[:], [pb], [wbcb])
                        jh, hid, hidb = hid_p.next()
                        for fc in range(2):
                            for s in range(4):
                                cs = slice(s * 512, (s + 1) * 512)
                                psg, pbg = self.ps_next()
                                for kc in range(8):
                                    P.mm(psg[:], wgu[:, kc, fc * 128:(fc + 1) * 128], hT[:, kc, cs], kc == 0, kc == 7,
                                         [wgub, actTb[s]], [pbg])
                                psu, pbu = self.ps_next()
                                for kc in range(8):
                                    P.mm(psu[:], wgu[:, kc, 256 + fc * 128:256 + (fc + 1) * 128], hT[:, kc, cs], kc == 0,
                                         kc == 7, [wgub, actTb[s]], [pbu])
                                j1, sg, sgb = sg_p.next()
                                P.act(sg[:], psg[:], AF.Silu, [pbg], [sgb])
                                j2, tt_, ttb = t_p.next()
                                P.tt("dve", tt_[:], sg[:], psu[:], ALU.mult, [sgb, pbu], [ttb])
                                P.tt("pool", hid[:, fc, cs], tt_[:], wbc[:, cs], ALU.mult, [ttb, wbcb], [hidb])
                        hids.append((hid, hidb))
                        wds.append((wd, wdb))
                        if ei == 0 and pend_down[0] is not None:
                            down(*pend_down[0])
                            pend_down[0] = None
                    pend_down[0] = (eg, hids, wds)
                down(*pend_down[0])
                S.barrier()
            with ExitStack() as st3:
                g2, bg2 = self.load_w(st3, "ln2g", [128, D], W["ln2_g"].ap().partition_broadcast(128), cast=False)
                b2, bb2 = self.load_w(st3, "ln2b", [128, D], W["ln2_b"].ap().partition_broadcast(128), cast=False)
                xr_p = self.pool(st3, "hr", [128, D], F32, 4)
                smln = self.pool(st3, "smln2", [128, 8], F32, 4)
                junk_p = self.pool(st3, "junk2", [128, D], F32, 2)

                hr_q = {}

                def hload(tile):
                    j, xr, xrb = xr_p.next()
                    P.dma("sp", "hr%d" % j, xr[:], dr["h"].ap()[tile * 128:(tile + 1) * 128, :], [db["h"]], [xrb])
                    hr_q[tile] = (xr, xrb)

                hload(0)
                hload(1)

                def l2a(tile):
                    if tile + 2 < 16:
                        hload(tile + 2)
                    xr, xrb = hr_q.pop(tile)
                    r = acc[:, tile, :]
                    P.stt("dve", r, xr[:], ALPHA, r, ALU.mult, ALU.add, [xrb, accb[tile]], [accb[tile]])
                    return self.ln_a(acc[:, tile, :], accb[tile], smln, junk_p)

                def l2b(tile, st_):
                    self.ln_b(acc[:, tile, :], accb[tile], g2, b2, [bg2, bb2], st_)
                    ob = self.out_bufs[tile // 4] if getattr(self, "out_bufs", None) else db["out"]
                    P.dma("sp", "oacc%d" % (tile % 4), out_rows(tile), acc[:, tile, :], [accb[tile]], [ob])
                    if tile % 4 == 3 and getattr(self, "after_slot", None):
                        self.after_slot(tile // 4)

                prev = None
                for tile in range(16):
                    cur = (tile, l2a(tile))
                    if prev is not None:
                        l2b(*prev)
                    prev = cur
                l2b(*prev)
                S.barrier()

    def build(self):
        nc, S = self.nc, self.S
        x_full = self.din("x_full", [SEQ, D])
        x_loc = self.din("x_loc", [TL, D])
        out = self.dout("out", [TL, D])
        Ws = {}
        import os
        stop = int(os.environ.get("KSTOP", "9"))
        for l in self.layers:
            Ws[l] = {k: self.din("%s_l%d" % (k, l), shp) for k, shp in LAYER_SHAPES.items()
                     if stop >= 6 or k not in ("wEg", "wEu", "wEd")}
        self.dr = {
            "KTm": self.dint("KTm", [4, 96, SEQ], BF16), "Vm": self.dint("Vm", [SEQ, 256], BF16),
            "KTs": self.dint("KTs", [4, 64, SEQ], BF16), "Vs": self.dint("Vs", [SEQ, 256], BF16),
            "KTf": self.dint("KTf", [4, 64, SEQ], BF16), "Vf": self.dint("Vf", [SEQ, 256], BF16),
            "h": self.dint("hbuf", [TL, D], F32),
        }
        self.dbuf = {k: Buf(k) for k in list(self.dr.keys()) + ["out"]}
        import os
        self.kv_exchange = os.environ.get("KVX", "1") == "1"
        if self.kv_exchange:
            self.xch = {
                "KVx": [self.dint("KVx%d" % s_, [896, GS], BF16) for s_ in range(4)],
                "KVg": [self.dint("KVg%d" % s_, [2 * 896, GS], BF16) for s_ in range(4)],
                "Vx": [self.dint("Vx%d" % s_, [GS, 768], BF16) for s_ in range(4)],
                "Vg": [self.dint("Vg%d" % s_, [2 * GS, 768], BF16) for s_ in range(4)],
                "spx": self.dint("spx", [128, 128], F32), "spg": self.dint("spg", [256, 128], F32),
                "bKVx": [Buf() for _ in range(4)], "bKVg": [Buf() for _ in range(4)],
                "bVx": [Buf() for _ in range(4)], "bVg": [Buf() for _ in range(4)],
                "bspx": Buf(), "bspg": Buf(),
            }
        with ExitStack() as st:
            self.setup_consts(st)
            self.setup_rope(st)
            if len(self.layers) == 1:
                l = self.layers[0]
                self.emit_layer(l, lambda g: x_full.ap()[g * GS:(g + 1) * GS, :],
                                lambda t: x_loc.ap()[t * 128:(t + 1) * 128, :],
                                lambda t: out.ap()[t * 128:(t + 1) * 128, :], Ws[l])
            else:
                xl1 = [self.dint("xl1_%d" % s_, [GS, D], F32) for s_ in range(4)]
                xg = [self.dint("xg_%d" % s_, [2 * GS, D], F32) for s_ in range(4)]
                bxl1 = [Buf("xl1_%d" % s_) for s_ in range(4)]
                bxg = [Buf("xg_%d" % s_) for s_ in range(4)]
                self.dbuf["out"] = None
                self.out_bufs = bxl1
                groups = [[0, 1], [2, 3], [4, 5], [6, 7]]

                def gather(s_):
                    S.dma("pool", "cc%d" % s_,
                          lambda e, s_=s_: e.collective_compute("AllGather", ALU.bypass, replica_groups=groups,
                                                                ins=[xl1[s_].ap().opt()], outs=[xg[s_].ap().opt()]),
                          [bxl1[s_]], [bxg[s_]], inc=1)
                self.after_slot = gather
                self.emit_layer(0, lambda g: x_full.ap()[g * GS:(g + 1) * GS, :],
                                lambda t: x_loc.ap()[t * 128:(t + 1) * 128, :],
                                lambda t: xl1[t // 4].ap()[(t % 4) * 128:(t % 4 + 1) * 128, :], Ws[0])
                self.after_slot = None
                if self.kv_exchange and os.environ.get("BSKIP", "1") == "1":
                    S.barrier(skip=("cc0", "cc1", "cc2", "cc3"))
                    self.xfull_bufs = lambda g_: [bxg[G_PAR[0].index(g_)] if g_ in G_PAR[0] else bxg[G_PAR[1].index(g_)]]
                else:
                    S.barrier()
                S.new_epoch()
                self.out_bufs = None
                self.dbuf["out"] = Buf("out")

                def xfull1(g):
                    if g in G_PAR[0]:
                        return xg[G_PAR[0].index(g)].ap()[0:GS, :]
                    return xg[G_PAR[1].index(g)].ap()[GS:2 * GS, :]
                self.emit_layer(1, xfull1, lambda t: xl1[t // 4].ap()[(t % 4) * 128:(t % 4 + 1) * 128, :],
                                lambda t: out.ap()[t * 128:(t + 1) * 128, :], Ws[1])
            S.barrier()
            S.emit()
        S.close()
        return nc


_PROG_CACHE = {}


def _get_prog(layers):
    key = tuple(layers)
    if key not in _PROG_CACHE:
        _PROG_CACHE[key] = Prog(list(layers)).build()
    return _PROG_CACHE[key]


def _idx(par):
    return np.concatenate([np.arange(g * GS, (g + 1) * GS) for g in G_PAR[par]])


def kernel(**inputs):
    inputs = {k: np.asarray(v) for k, v in inputs.items()}
    x = np.ascontiguousarray(inputs["x"], dtype=np.float32)
    positions = inputs["positions"]
    nc = _get_prog((0, 1))
    la = {}
    for l in range(DEPTH):
        for k, v in layer_arrays(inputs, l).items():
            la["%s_l%d" % (k, l)] = v
    in_maps = []
    for core in range(N_CORES):
        b, par = core // 2, core % 2
        thr, sel, inv2pi = core_meta(core)
        idx = _idx(par)
        m = {"x_full": np.ascontiguousarray(x[b]), "x_loc": np.ascontiguousarray(x[b][idx]),
             "pos_full": np.ascontiguousarray(positions[b].reshape(1, SEQ).astype(np.int32)),
             "pos_loc": np.ascontiguousarray(positions[b][idx].reshape(1, TL).astype(np.int32)),
             "thr": thr, "sel": sel, "inv2pi": inv2pi}
        m.update(la)
        in_maps.append(m)
    res = run_bass_kernel_spmd(nc, in_maps, core_ids=list(range(N_CORES)))
    out = np.empty_like(x)
    for core in range(N_CORES):
        b, par = core // 2, core % 2
        out[b][_idx(par)] = res.results[core]["out"]
    return out
```

```python
import numpy as np
from contextlib import ExitStack
import concourse.bass as bass
import concourse.mybir as mybir
from concourse.bass_utils import run_bass_kernel_spmd

F32 = mybir.dt.float32
BF16 = mybir.dt.bfloat16
I32 = mybir.dt.int32
ALU = mybir.AluOpType
AF = mybir.ActivationFunctionType
AX = mybir.AxisListType

D = 1024
SEQ = 4096
TL = 2048
GS = 512
NSLOT = 4
G_PAR = ((0, 3, 4, 7), (1, 2, 5, 6))
ALPHA = float((2.0 * 2) ** 0.25)
EPS = 1e-5
NEGBIG = -30000.0
DEPTH = 2
N_CORES = 8


class Buf:
    __slots__ = ("name", "w", "r")

    def __init__(self, name=""):
        self.name = name
        self.w = None
        self.r = {}


class Sched:
    ENGS = ("pe", "act", "dve", "pool", "sp")

    def __init__(self, nc):
        self.nc = nc
        self.q = {e: [] for e in self.ENGS}
        self.cnt = {e: 0 for e in self.ENGS}
        self.sems = {}
        self._ctx = []
        for e in self.ENGS:
            cm = nc.semaphore("s_" + e)
            self.sems[e] = cm.__enter__()
            self._ctx.append(cm)
        self.dma_sems = {}
        self.dma_cnt = {}
        self.seen = {e: {} for e in self.ENGS}
        self.n_instr = 0
        self.epoch = 0

    def new_epoch(self):
        self.epoch += 1
        for e in self.ENGS:
            cm = self.nc.semaphore("s_%s_%d" % (e, self.epoch))
            self.sems[e] = cm.__enter__()
            self._ctx.append(cm)
            self.cnt[e] = 0
        for e in self.ENGS:
            self.seen[e] = {k: v for k, v in self.seen[e].items() if not isinstance(k, tuple)}

    def close(self):
        for cm in reversed(self._ctx):
            cm.__exit__(None, None, None)

    def _dma_sem(self, key):
        if key not in self.dma_sems:
            cm = self.nc.semaphore("d_" + str(key))
            self.dma_sems[key] = cm.__enter__()
            self._ctx.append(cm)
            self.dma_cnt[key] = 0
        return self.dma_sems[key]

    def _semh(self, key):
        return self.sems[key[0]] if isinstance(key, tuple) else self.dma_sems[key]

    def _waits(self, eng, reads, writes):
        need = {}
        for b in reads:
            if b.w is not None:
                k, v = b.w
                if v > need.get(k, 0):
                    need[k] = v
        for b in writes:
            if b.w is not None:
                k, v = b.w
                if v > need.get(k, 0):
                    need[k] = v
            for k, v in b.r.items():
                if v > need.get(k, 0):
                    need[k] = v
        out = []
        seen = self.seen[eng]
        for k, v in need.items():
            if isinstance(k, tuple):
                if k[1] < self.epoch:
                    continue
                if k[0] == "pe" and eng == "pe":
                    continue
            if seen.get(k, 0) >= v:
                continue
            seen[k] = v
            out.append((self._semh(k), v))
        return out

    def _mark(self, tok, reads, writes):
        k, v = tok
        for b in reads:
            if v > b.r.get(k, 0):
                b.r[k] = v
        for b in writes:
            b.w = tok
            b.r = {}

    def op(self, eng, fn, reads=(), writes=()):
        waits = self._waits(eng, reads, writes)
        self.cnt[eng] += 1
        tok = ((eng, self.epoch), self.cnt[eng])
        self.q[eng].append((waits, fn, self.sems[eng], 1))
        self._mark(tok, reads, writes)
        self.n_instr += 1
        return tok

    def dma(self, queue, key, fn, reads=(), writes=(), inc=16):
        waits = self._waits(queue, reads, writes)
        sem = self._dma_sem(key)
        self.dma_cnt[key] += inc
        tok = (key, self.dma_cnt[key])
        self.q[queue].append((waits, fn, sem, inc))
        self._mark(tok, reads, writes)
        self.n_instr += 1
        return tok

    def barrier(self, skip=()):
        for e in self.ENGS:
            waits = []
            seen = self.seen[e]
            for k in self.ENGS:
                v = self.cnt[k]
                kk = (k, self.epoch)
                if v > seen.get(kk, 0) and not (k == "pe" and e == "pe" and False):
                    seen[kk] = v
                    waits.append((self.sems[k], v))
            for k, v in self.dma_cnt.items():
                if k in skip:
                    continue
                if v > seen.get(k, 0):
                    seen[k] = v
                    waits.append((self.dma_sems[k], v))
            if waits:
                self.q[e].append((waits, None, None, 0))

    def emit(self):
        nc = self.nc
        qs = self.q

        def run(e, items):
            for waits, fn, sem, inc in items:
                for (s, v) in waits:
                    e.wait_ge(s, v)
                if fn is not None:
                    fn(e).then_inc(sem, inc)

        with nc.Block() as block:
            @block.tensor
            def _(e):
                run(e, qs["pe"])

            @block.scalar
            def _(e):
                run(e, qs["act"])

            @block.vector
            def _(e):
                run(e, qs["dve"])

            @block.gpsimd
            def _(e):
                run(e, qs["pool"])

            @block.sync
            def _(e):
                run(e, qs["sp"])


class RPool:
    def __init__(self, tiles):
        self.tiles = tiles
        self.bufs = [Buf() for _ in tiles]
        self.i = 0

    def next(self):
        j = self.i % len(self.tiles)
        self.i += 1
        return j, self.tiles[j], self.bufs[j]


def _kc(w):
    k, m = w.shape
    n = k // 128
    return np.ascontiguousarray(w.reshape(n, 128, m).transpose(1, 0, 2).reshape(128, n * m))


def layer_arrays(inp, l):
    f = np.float32
    w_in = inp["w_in"][l]
    A = {}
    A["wK_ckv"] = _kc(w_in[:, 256:384])
    kr = w_in[:, 384:416]
    pad = np.zeros((D, 128), f)
    pad[:, 64:96] = kr
    A["wK_kr"] = _kc(pad)
    pad = np.zeros((D, 128), f)
    pad[:, 64:80] = kr[:, 16:32]
    pad[:, 80:96] = kr[:, 0:16]
    A["wK_krr"] = _kc(pad)
    A["wK_sbk"] = _kc(w_in[:, 672:928])
    A["wK_fxk"] = _kc(w_in[:, 1952:2208])
    A["wK_v2"] = _kc(np.concatenate([w_in[:, 928:1184], w_in[:, 2208:2464]], 1))
    A["wK_f"] = _kc(w_in[:, 2464:2468])
    ukv = inp["mla_w_ukv"][l]
    A["w_ukv_k"] = np.ascontiguousarray(np.concatenate([ukv[:, h * 128:h * 128 + 64] for h in range(4)], 1))
    A["w_ukv_v"] = np.ascontiguousarray(np.concatenate([ukv[:, h * 128 + 64:h * 128 + 128] for h in range(4)], 1))
    A["kvn"] = np.ascontiguousarray(inp["mla_kv_norm"][l].reshape(128, 1))
    A["wQ_cq"] = _kc(w_in[:, 0:256])
    uq = inp["mla_w_uq"][l]
    A["w_uq"] = _kc(uq)
    uqr = np.zeros_like(uq)
    for h in range(4):
        o = h * 96 + 64
        uqr[:, o:o + 16] = uq[:, o + 16:o + 32]
        uqr[:, o + 16:o + 32] = uq[:, o:o + 16]
    A["w_uqr"] = _kc(uqr)
    A["qn"] = np.ascontiguousarray(inp["mla_q_norm"][l].reshape(2, 128).T)
    A["wQ_sbq"] = _kc(w_in[:, 416:672])
    A["wQ_fxq"] = _kc(w_in[:, 1696:1952])
    A["wQ_cv"] = _kc(w_in[:, 1184:1696])
    g = w_in[:, 2468:6564].reshape(D, 4, 8, 128)
    g = g.reshape(8, 128, 4, 8, 128).transpose(3, 2, 1, 0, 4)
    A["wG"] = np.ascontiguousarray(g.reshape(32, 128, 1024))
    A["bg"] = np.ascontiguousarray(inp["b_gate"][l].reshape(4, 8, 128).transpose(2, 1, 0).reshape(128, 32))
    wb = inp["w_branch"][l].reshape(4, 2, 128, D).transpose(2, 0, 1, 3)
    A["wB"] = np.ascontiguousarray(wb.reshape(128, 8 * D))
    A["wO"] = _kc(inp["w_o"][l])
    A["convw"] = np.ascontiguousarray(inp["conv_w"][l].reshape(31, 2, 128).transpose(2, 1, 0).reshape(128, 62))
    A["convb"] = np.ascontiguousarray(inp["conv_b"][l].reshape(2, 128).T)
    A["clng"] = np.ascontiguousarray(inp["conv_ln_g"][l].reshape(2, 128).T)
    A["clnb"] = np.ascontiguousarray(inp["conv_ln_b"][l].reshape(2, 128).T)
    for k in ("ln1_g", "ln1_b", "ln2_g", "ln2_b"):
        A[k] = np.ascontiguousarray(inp[k][l].reshape(1, D))
    A["bfg"] = np.ascontiguousarray(inp["b_forget"][l].reshape(1, 4))
    A["wR"] = _kc(np.concatenate([inp["w_router_group"][l], inp["w_router_expert"][l]], 1))
    A["bR"] = np.ascontiguousarray(np.concatenate([inp["b_router_group"][l], inp["b_router_expert"][l]]).reshape(1, 36))
    eg = inp["w_exp_gate"][l].reshape(32, 8, 128, 256).transpose(0, 2, 1, 3)
    A["wEg"] = np.ascontiguousarray(eg.reshape(32, 128, 2048))
    eu = inp["w_exp_up"][l].reshape(32, 8, 128, 256).transpose(0, 2, 1, 3)
    A["wEu"] = np.ascontiguousarray(eu.reshape(32, 128, 2048))
    ed = inp["w_exp_down"][l].reshape(32, 2, 128, D).transpose(0, 2, 1, 3)
    A["wEd"] = np.ascontiguousarray(ed.reshape(32, 128, 2048))
    return {k: np.asarray(v, dtype=f) for k, v in A.items()}


LAYER_SHAPES = {
    "wK_ckv": [128, 1024], "wK_kr": [128, 1024], "wK_krr": [128, 1024], "wK_sbk": [128, 2048],
    "wK_fxk": [128, 2048], "wK_v2": [128, 4096], "wK_f": [128, 32], "w_ukv_k": [128, 256],
    "w_ukv_v": [128, 256], "kvn": [128, 1], "wQ_cq": [128, 2048], "w_uq": [128, 768],
    "w_uqr": [128, 768], "qn": [128, 2], "wQ_sbq": [128, 2048], "wQ_fxq": [128, 2048],
    "wQ_cv": [128, 4096], "wG": [32, 128, 1024], "bg": [128, 32], "wB": [128, 8192],
    "wO": [128, 8192], "convw": [128, 62], "convb": [128, 2], "clng": [128, 2], "clnb": [128, 2],
    "ln1_g": [1, D], "ln1_b": [1, D], "ln2_g": [1, D], "ln2_b": [1, D], "bfg": [1, 4],
    "wR": [128, 288], "bR": [1, 36], "wEg": [32, 128, 2048], "wEu": [32, 128, 2048],
    "wEd": [32, 128, 2048],
}


def core_meta(core):
    par = core % 2
    thr = np.zeros((128, 32), np.float32)
    for s in range(4):
        for j in range(8):
            kb = 8 * s + j
            thr[:, s * 8 + j] = G_PAR[par][s] * GS - kb * 128
    sel = np.zeros((128, 2), np.float32)
    sel[:, par] = 1.0
    inv = 10000.0 ** (-(np.arange(16, dtype=np.float64)) / 16.0)
    inv2pi = np.zeros((128, 1), np.float32)
    for i in range(32):
        inv2pi[64 + i, 0] = inv[i % 16] / (2 * np.pi)
    return thr, sel, inv2pi


class Prog:
    def __init__(self, layers, debug=False):
        self.layers = layers
        self.debug = debug
        nc = bass.Bass("TRN2", target_bir_lowering=False)
        self.nc = nc
        self.S = Sched(nc)
        self.dram = {}
        self.dbuf = {}

    def din(self, name, shape, dt=F32):
        t = self.nc.dram_tensor(name, list(shape), dt, kind="ExternalInput")
        self.dram[name] = t
        return t

    def dout(self, name, shape, dt=F32):
        t = self.nc.dram_tensor(name, list(shape), dt, kind="ExternalOutput")
        self.dram[name] = t
        return t

    def dint(self, name, shape, dt):
        t = self.nc.dram_tensor(name, list(shape), dt)
        self.dram[name] = t
        return t

    def sb(self, stack, name, shape, dt):
        self._sbn = getattr(self, "_sbn", 0) + 1
        return stack.enter_context(self.nc.sbuf_tensor("s%d_%s" % (self._sbn, name), list(shape), dt))

    def pool(self, stack, name, shape, dt, n):
        return RPool([self.sb(stack, "%s%d" % (name, i), shape, dt) for i in range(n)])

    def mm(self, out, lhsT, rhs, start, stop, reads, writes):
        self.S.op("pe", lambda e, o=out, l=lhsT, r=rhs, a=start, b=stop: e.matmul(o, l, r, start=a, stop=b),
                  reads, writes)

    def tr(self, out, in_, reads, writes):
        idn = self.ident
        self.S.op("pe", lambda e, o=out, i=in_: e.transpose(o, i, idn[:]), list(reads) + [self.b_const], writes)

    def act(self, out, in_, func, reads, writes, bias=None, scale=None, accum_out=None):
        kw = {}
        if bias is not None:
            kw["bias"] = bias
        if scale is not None:
            kw["scale"] = scale
        if accum_out is not None:
            kw["accum_out"] = accum_out
        self.S.op("act", lambda e, o=out, i=in_, f=func, kw=kw: e.activation(out=o, in_=i, func=f, **kw), reads, writes)

    def tt(self, eng, out, in0, in1, op, reads, writes):
        self.S.op(eng, lambda e, o=out, a=in0, b=in1, p=op: e.tensor_tensor(out=o, in0=a, in1=b, op=p), reads, writes)

    def ts(self, eng, out, in0, s1, s2, op0, op1, reads, writes):
        if op1 is None:
            self.S.op(eng, lambda e, o=out, a=in0, x=s1, p=op0: e.tensor_single_scalar(out=o, in_=a, scalar=x, op=p),
                      reads, writes)
        else:
            self.S.op(eng, lambda e, o=out, a=in0, x=s1, y=s2, p=op0, q=op1:
                      e.tensor_scalar(out=o, in0=a, scalar1=x, scalar2=y, op0=p, op1=q), reads, writes)

    def stt(self, eng, out, in0, scalar, in1, op0, op1, reads, writes):
        self.S.op(eng, lambda e, o=out, a=in0, s=scalar, b=in1, p=op0, q=op1:
                  e.scalar_tensor_tensor(out=o, in0=a, scalar=s, in1=b, op0=p, op1=q), reads, writes)

    def cp(self, eng, out, in_, reads, writes):
        if eng == "act":
            self.S.op("act", lambda e, o=out, i=in_: e.copy(out=o, in_=i), reads, writes)
        else:
            self.S.op(eng, lambda e, o=out, i=in_: e.tensor_copy(out=o, in_=i), reads, writes)

    def dma(self, queue, key, out, in_, reads, writes):
        self.S.dma(queue, key, lambda e, o=out, i=in_: e.dma_start(out=o, in_=i), reads, writes)

    def dump(self, name, src, shape, dt, reads):
        if not self.debug:
            return
        t = self.dout("dbg_" + name, shape, dt)
        b = Buf("dbg_" + name)
        self.dma("sp", "dbg_" + name, t.ap(), src, reads, [b])

    def evac_eng(self):
        self._ev = getattr(self, "_ev", 0) + 1
        return "act" if self._ev % 2 else "dve"

    def setup_consts(self, st):
        nc, S = self.nc, self.S
        P = self
        self.b_const = Buf("const")
        bc = [self.b_const]
        self.iotf = self.sb(st, "iotf", [128, 512], F32)
        self.ident = self.sb(st, "ident", [128, 128], F32)
        self.ones_bf = self.sb(st, "ones_bf", [128, 128], BF16)
        self.ones_f = self.sb(st, "ones_f", [128, 128], F32)
        self.LT = self.sb(st, "LT", [128, 128], BF16)
        self.UT = self.sb(st, "UT", [128, 128], F32)
        self.D0 = self.sb(st, "D0", [128, 512], F32)
        self.D1 = self.sb(st, "D1", [128, 512], F32)
        self.selE = self.sb(st, "selE", [32, 32 * 128], BF16)
        self.Eh = self.sb(st, "Eh", [4, 4 * 65], BF16)
        self.epsc = self.sb(st, "epsc", [128, 1], F32)
        self.onec = self.sb(st, "onec", [128, 1], F32)
        self.thr = self.sb(st, "thr", [128, 32], F32)
        self.sel = self.sb(st, "sel", [128, 2], F32)
        self.sel8 = self.sb(st, "sel8", [128, 2], F32)
        self.inv2pi = self.sb(st, "inv2pi", [128, 1], F32)
        S.op("pool", lambda e: e.iota(self.iotf[:], [[1, 512]], base=0, channel_multiplier=-1,
                                      allow_small_or_imprecise_dtypes=True), [], bc)
        P.ts("dve", self.ident[:], self.iotf[:, 0:128], 0.0, None, ALU.is_equal, None, bc, bc)
        P.ts("dve", self.UT[:], self.iotf[:, 0:128], 0.0, None, ALU.is_ge, None, bc, bc)
        P.ts("dve", self.LT[:], self.iotf[:, 0:128], 0.0, None, ALU.is_le, None, bc, bc)
        P.ts("dve", self.D0[:], self.iotf[:], -1.0, None, ALU.mult, None, bc, bc)
        P.ts("dve", self.D1[:], self.D0[:], 1.0, None, ALU.add, None, bc, bc)
        S.op("pool", lambda e: e.memset(self.ones_bf[:], 1.0), [], bc)
        S.op("pool", lambda e: e.memset(self.ones_f[:], 1.0), [], bc)
        S.op("pool", lambda e: e.memset(self.epsc[:], EPS), [], bc)
        S.op("pool", lambda e: e.memset(self.onec[:], 1.0), [], bc)
        tst = ExitStack()
        tmp = self.sb(tst, "seltmp", [32, 32 * 128], F32)
        S.op("pool", lambda e: e.iota(tmp[:].rearrange("k (e m) -> k e m", m=128), [[1, 32], [0, 128]], base=0,
                                      channel_multiplier=-1, allow_small_or_imprecise_dtypes=True), [], bc)
        P.ts("dve", self.selE[:], tmp[:], 0.0, None, ALU.is_equal, None, bc, bc)
        S.op("pool", lambda e: e.iota(tmp[0:4, 0:260].rearrange("k (h m) -> k h m", m=65), [[1, 4], [0, 65]], base=0,
                                      channel_multiplier=-1, allow_small_or_imprecise_dtypes=True), bc, bc)
        P.ts("dve", self.Eh[:], tmp[0:4, 0:260], 0.0, None, ALU.is_equal, None, bc, bc)
        S.op("dve", lambda e: e.memset(self.Eh[:].rearrange("k (h m) -> k h m", m=65)[:, :, 0:64], 0.0), bc, bc)
        S.barrier()
        tst.close()
        thr_d = self.din("thr", [128, 32])
        sel_d = self.din("sel", [128, 2])
        inv_d = self.din("inv2pi", [128, 1])
        P.dma("sp", "c_thr", self.thr[:], thr_d.ap(), [], bc)
        P.dma("sp", "c_sel", self.sel[:], sel_d.ap(), [], bc)
        P.dma("sp", "c_inv", self.inv2pi[:], inv_d.ap(), [], bc)
        P.ts("dve", self.sel8[:], self.sel[:], -8.0, None, ALU.mult, None, bc, bc)
        self.psb = [st.enter_context(nc.psum_tensor("psb%d" % i, [128, 512], F32)) for i in range(8)]
        self.ps_bufs = [Buf("ps%d" % i) for i in range(8)]
        self.ps_i = 0

    def ps_next(self, lo=0, hi=8):
        key = (lo, hi)
        if not hasattr(self, "_psrot"):
            self._psrot = {}
        i = self._psrot.get(key, 0)
        self._psrot[key] = i + 1
        j = lo + i % (hi - lo)
        return self.psb[j], self.ps_bufs[j]

    def rope_tables(self, tmp, src, ncols, cos_out, sin_out, out_buf):
        P, S = self, self.S
        pi_, pfl, t, ki, kf, bt = tmp
        sl = slice(64, 96)
        n = ncols
        P.dma("sp", "rp_ld", pi_[sl, 0:n], src.partition_broadcast(32), [], [bt])
        P.cp("dve", pfl[sl, 0:n], pi_[sl, 0:n], [bt], [bt])
        for which, tab in ((0.0, sin_out), (0.25, cos_out)):
            P.ts("dve", t[sl, 0:n], pfl[sl, 0:n], self.inv2pi[sl, 0:1], which, ALU.mult, ALU.add, [bt, self.b_const], [bt])
            P.cp("dve", ki[sl, 0:n], t[sl, 0:n], [bt], [bt])
            P.cp("dve", kf[sl, 0:n], ki[sl, 0:n], [bt], [bt])
            P.tt("dve", t[sl, 0:n], t[sl, 0:n], kf[sl, 0:n], ALU.subtract, [bt], [bt])
            P.ts("dve", kf[sl, 0:n], t[sl, 0:n], 0.5, None, ALU.is_gt, None, [bt], [bt])
            P.tt("dve", t[sl, 0:n], t[sl, 0:n], kf[sl, 0:n], ALU.subtract, [bt], [bt])
            P.ts("dve", kf[sl, 0:n], t[sl, 0:n], -0.5, None, ALU.is_lt, None, [bt], [bt])
            P.tt("dve", t[sl, 0:n], t[sl, 0:n], kf[sl, 0:n], ALU.add, [bt], [bt])
            P.act(tab, t[sl, 0:n], AF.Sin, [bt], [bt, out_buf], scale=float(2 * np.pi * (1 - 1e-6)))

    def rope_tmp(self, st, n):
        return (self.sb(st, "rp_i", [128, n], I32), self.sb(st, "rp_f", [128, n], F32), self.sb(st, "rp_t", [128, n], F32),
                self.sb(st, "rp_ki", [128, n], I32), self.sb(st, "rp_kf", [128, n], F32), Buf("rp"))

    def setup_rope(self, st):
        self.pos_full = self.din("pos_full", [1, SEQ], I32)
        pl = self.din("pos_loc", [1, TL], I32)
        self.cosL = self.sb(st, "cosL", [128, TL], BF16)
        self.sinL = self.sb(st, "sinL", [128, TL], BF16)
        self.b_rope = Buf("rope")
        with ExitStack() as tmp:
            T = self.rope_tmp(tmp, 512)
            for c in range(4):
                cs = slice(c * 512, (c + 1) * 512)
                self.rope_tables(T, pl.ap()[:, cs], 512, self.cosL[64:96, cs], self.sinL[64:96, cs], self.b_rope)
            self.S.barrier()

    def load_w(self, st, name, shape, src, cast=True, key=None, skip=False):
        t = self.sb(st, name, shape, BF16 if cast else F32)
        b = Buf(name)
        if src is not None:
            self.dma("pool" if cast else "sp", key or ("w_" + name), t[:], src, [], [b])
        return t, b

    def transpose_tiles(self, st_pools, row_aps, dst, dst_buf, col0):
        xin = st_pools["xin"]
        tiles = []
        for t, ap in enumerate(row_aps):
            j, xt, xb = xin.next()
            self.dma("sp", "xin%d" % j, xt[:], ap, [], [xb])
            tiles.append((xt, xb))
        n = len(tiles)
        for dc in range(8):
            ps, pb = self.ps_next()
            for t, (xt, xb) in enumerate(tiles):
                self.tr(ps[:, t * 128:(t + 1) * 128], xt[:, dc * 128:(dc + 1) * 128], [xb], [pb])
            self.cp(self.evac_eng(), dst[:, dc, col0:col0 + n * 128], ps[:, 0:n * 128], [pb], [dst_buf])

    def rms_scale(self, st_pools, ps_list, pb_list, n_feat, out_tile, out_buf, ncols=512, ps_range=(0, 8)):
        sqp = st_pools["sq"]
        f32p = st_pools["f32"]
        sqs = []
        for ps, pb in zip(ps_list, pb_list):
            j, sq, sqb = sqp.next()
            self.act(sq[:, 0:ncols], ps[:, 0:ncols], AF.Square, [pb], [sqb])
            sqs.append((sq, sqb))
        pss, pbs = self.ps_next(*ps_range)
        for i, (sq, sqb) in enumerate(sqs):
            self.mm(pss[:, 0:ncols], self.ones_bf[:], sq[:, 0:ncols], i == 0, i == len(sqs) - 1,
                    [sqb, self.b_const], [pbs])
        j, sd, sdb = f32p.next()
        self.act(sd[:, 0:ncols], pss[:, 0:ncols], AF.Sqrt, [pbs, self.b_const], [sdb], bias=self.epsc[:, 0:1],
                 scale=1.0 / n_feat)
        self.S.op("dve", lambda e, o=sd[:, 0:ncols]: e.reciprocal(out=o, in_=o), [sdb], [sdb])
        for i, (ps, pb) in enumerate(zip(ps_list, pb_list)):
            self.tt("dve", out_tile[:, i, 0:ncols], ps[:, 0:ncols], sd[:, 0:ncols], ALU.mult, [pb, sdb], [out_buf])

    def emit_layer(self, l, xfull_rows, xloc_rows, out_rows, W):
        nc, S, P = self.nc, self.S, self
        bc = self.b_const
        dr = self.dr
        with ExitStack() as lst:
            negcK = self.sb(lst, "negcK", [128, 32 * 4], F32)
            negcKb = Buf("negcK")
            cqa = self.sb(lst, "cqa", [4, TL], BF16)
            cqab = Buf("cqa")
            WtT = self.sb(lst, "WtT", [32, TL], BF16)
            WtTb = [Buf("WtT%d" % s) for s in range(4)]
            actT = self.sb(lst, "actT", [128, 8, TL], BF16)
            actTb = [Buf("actT%d" % s) for s in range(4)]
            import os
            stop = int(os.environ.get("KSTOP", "9"))
            if stop < 1:
                return
            if not self.kv_exchange:
                self.stage_K(l, xfull_rows, W, negcK, negcKb, cqa, cqab)
            if self.debug:
                for nm, shp in (("KTm", [4, 96, SEQ]), ("Vm", [SEQ, 256]), ("KTs", [4, 64, SEQ]), ("Vs", [SEQ, 256]),
                                ("KTf", [4, 64, SEQ]), ("Vf", [SEQ, 256])):
                    self.dump(nm, self.dr[nm].ap(), shp, BF16, [self.dbuf[nm]])
                self.dump("negcK", negcK[:], [128, 128], F32, [negcKb])
                self.dump("cqa", cqa[:], [4, TL], BF16, [cqab])
            if stop < 2:
                return
            with ExitStack() as yst:
                yT = [self.sb(yst, "yT%d" % n, [128, 2, TL], BF16) for n in range(4)]
                yTb = [[Buf("yT%d_%d" % (n, s)) for s in range(4)] for n in range(4)]
                self.stage_Q(l, xfull_rows, xloc_rows, W, actT, actTb, yT, yTb, negcK, negcKb, cqa, cqab)
                if self.debug:
                    for n in range(4):
                        self.dump("yT%d" % n, yT[n][:], [128, 2, TL], BF16, yTb[n])
                    self.dump("xlT", actT[:], [128, 8, TL], BF16, actTb)
                    self.dump("negcK2", negcK[:], [128, 128], F32, [negcKb])
                if stop < 4:
                    return
                with ExitStack() as mst:
                    mrgT = self.sb(mst, "mrgT", [128, 8, TL], BF16)
                    mrgTb = [Buf("mrgT%d" % s) for s in range(4)]
                    self.stage_G(l, W, actT, actTb, yT, yTb, mrgT, mrgTb)
                    self.dump("mrgT", mrgT[:], [128, 8, TL], BF16, mrgTb)
                    if stop < 5:
                        return
                    self.stage_F1(l, xloc_rows, W, actT, actTb, mrgT, mrgTb, WtT, WtTb)
            self.dump("h", self.dr["h"].ap(), [TL, D], F32, [self.dbuf["h"]])
            self.dump("WtT", WtT[:], [32, TL], BF16, WtTb)
            if stop < 6:
                return
            self.stage_F2(l, out_rows, W, actT, actTb, WtT, WtTb)

    def mk_pools(self, st, xin=True):
        pools = {
            "sq": self.pool(st, "sq", [128, 512], BF16, 2),
            "f32": self.pool(st, "f32", [128, 512], F32, 6),
            "bf": self.pool(st, "bft", [128, 512], BF16, 8),
        }
        if xin:
            pools["xin"] = self.pool(st, "xin", [128, 1024], F32, 4)
        return pools

    def stage_K2(self, l, W, xlT, actTb, cqa, cqab, xloc_rows):
        S, P = self.S, self
        bc = self.b_const
        X = self.xch
        r3 = lambda name, m: W[name].ap().rearrange("p (k m) -> p k m", m=m)
        groups = [[0, 1], [2, 3], [4, 5], [6, 7]]
        with ExitStack() as st:
            pools = self.mk_pools(st, xin=False)
            Wf, bWf = self.load_w(st, "Wf", [128, 8, 4], r3("wK_f", 4))
            Wsbk, bWsbk = self.load_w(st, "Wsbk", [128, 8, 256], r3("wK_sbk", 256))
            Wfxk, bWfxk = self.load_w(st, "Wfxk", [128, 8, 256], r3("wK_fxk", 256))
            Wv2, bWv2 = self.load_w(st, "Wv2", [128, 8, 512], r3("wK_v2", 512))
            Wckv, bWckv = self.load_w(st, "Wckv", [128, 8, 128], r3("wK_ckv", 128))
            Wkr, bWkr = self.load_w(st, "Wkr", [128, 8, 128], r3("wK_kr", 128))
            Wkrr, bWkrr = self.load_w(st, "Wkrr", [128, 8, 128], r3("wK_krr", 128))
            S.op("dve", lambda e: e.tensor_scalar_mul(out=Wkrr[:, :, 64:80], in0=Wkrr[:, :, 64:80], scalar1=-1.0),
                 [bWkrr], [bWkrr])
            ukf, bukf = self.load_w(st, "ukf", [128, 512], None, cast=False, skip=True)
            kvn, bkvn = self.load_w(st, "kvn", [128, 1], W["kvn"].ap(), cast=False)
            P.dma("sp", "w_ukf", ukf[:, 0:256], W["w_ukv_k"].ap(), [], [bukf])
            P.dma("sp", "w_ukf", ukf[:, 256:512], W["w_ukv_v"].ap(), [], [bukf])
            Wukv = self.sb(st, "Wukv", [128, 512], BF16)
            bWukv = Buf("Wukv")
            P.ts("dve", Wukv[:], ukf[:], kvn[:, 0:1], None, ALU.mult, None, [bukf, bkvn], [bWukv])
            bfg, bbfg = self.load_w(st, "bfg", [128, 4], W["bfg"].ap().partition_broadcast(128), cast=False)
            ckvn_p = self.pool(st, "ckvn", [128, 1, 512], BF16, 2)
            kst_p = self.pool(st, "kst", [128, 4, 512], BF16, 2)
            krst_p = self.pool(st, "krst", [128, 512], BF16, 2)
            vst_p = self.pool(st, "vst", [128, 4, 256], BF16, 2)
            vst2_p = self.pool(st, "vst2", [128, 4, 512], BF16, 2)
            sm_p = self.pool(st, "smallK", [128, 16], F32, 4)
            spown = self.sb(st, "spown", [128, 128], F32)
            bspo = Buf("spown")
            S.op("pool", lambda e: e.memset(spown[:], 0.0), [], [bspo])
            S.op("pool", lambda e: e.memset(cqa[:], 0.0), [], [cqab])
            f32p = pools["f32"]
            sl = slice(64, 96)
            xin8 = {"xin": self.pool(st, "xin", [128, 1024], F32, 8)}
            for s in range(4):
                self.transpose_tiles(xin8, [xloc_rows(s * 4 + t) for t in range(4)], xlT, actTb[s], s * 512)
            for s in range(4):
                cols = slice(s * 512, (s + 1) * 512)
                xTb = actTb[s]
                xT = lambda dc, a=0, b=512, s=s: xlT[:, dc, s * 512 + a:s * 512 + b]
                KV, bKV = X["KVx"][s], X["bKVx"][s]
                VX, bVX = X["Vx"][s], X["bVx"][s]
                for t in range(4):
                    lt = s * 4 + t
                    ps, pb = self.ps_next()
                    for dc in range(8):
                        P.mm(ps[:, 0:4], xT(dc, t * 128, (t + 1) * 128), Wf[:, dc, :], dc == 0, dc == 7, [xTb, bWf], [pb])
                    j, sm, smb = sm_p.next()
                    P.tt("dve", sm[:, 0:4], ps[:, 0:4], bfg[:], ALU.add, [pb, bbfg], [smb])
                    P.act(sm[:, 4:8], sm[:, 0:4], AF.Exp, [smb], [smb], scale=-1.0)
                    P.act(spown[:, lt * 4:(lt + 1) * 4], sm[:, 4:8], AF.Ln, [smb, bc], [bspo], bias=self.onec[:, 0:1])
                for (Wk, bWk, rbase) in ((Wsbk, bWsbk, 384), (Wfxk, bWfxk, 640)):
                    j, kst, kstb = kst_p.next()
                    for pr in range(2):
                        ps, pb = self.ps_next()
                        for dc in range(8):
                            P.mm(ps[:], Wk[:, dc, pr * 128:(pr + 1) * 128], xT(dc), dc == 0, dc == 7, [bWk, xTb], [pb])
                        P.cp(self.evac_eng(), kst[:, pr, :], ps[:], [pb], [kstb])
                        P.dma("sp", "kst%d" % j, KV.ap()[rbase + pr * 128:rbase + (pr + 1) * 128, :], kst[:, pr, :],
                              [kstb], [bKV])
                j, vst2, vst2b = vst2_p.next()
                for t in range(4):
                    ps, pb = self.ps_next()
                    for dc in range(8):
                        P.mm(ps[:], xT(dc, t * 128, (t + 1) * 128), Wv2[:, dc, :], dc == 0, dc == 7, [xTb, bWv2], [pb])
                    P.cp(self.evac_eng(), vst2[:, t, :], ps[:], [pb], [vst2b])
                P.dma("sp", "vst2a%d" % j, VX.ap()[:, 256:768].rearrange("(t p) c -> p t c", p=128), vst2[:], [vst2b], [bVX])
                ps, pb = self.ps_next()
                for dc in range(8):
                    P.mm(ps[:], Wckv[:, dc, :], xT(dc), dc == 0, dc == 7, [bWckv, xTb], [pb])
                j, ckvn, ckvnb = ckvn_p.next()
                self.rms_scale(pools, [ps], [pb], 128, ckvn, ckvnb)
                j, kst, kstb = kst_p.next()
                for h in range(4):
                    ps, pb = self.ps_next()
                    P.mm(ps[0:64, :], Wukv[:, h * 64:(h + 1) * 64], ckvn[:, 0, :], True, True, [bWukv, ckvnb], [pb])
                    P.cp(self.evac_eng(), kst[0:64, h, :], ps[0:64, :], [pb], [kstb])
                P.dma("sp", "kst%d" % j, KV.ap()[0:384, :].rearrange("(h p) t -> p h t", p=96)[0:64], kst[0:64, :, :],
                      [kstb], [bKV])
                psa, pba = self.ps_next()
                for dc in range(8):
                    P.mm(psa[:], Wkr[:, dc, :], xT(dc), dc == 0, dc == 7, [bWkr, xTb], [pba])
                psb_, pbb = self.ps_next()
                for dc in range(8):
                    P.mm(psb_[:], Wkrr[:, dc, :], xT(dc), dc == 0, dc == 7, [bWkrr, xTb], [pbb])
                j1, t1, t1b = f32p.next()
                j2, t2, t2b = f32p.next()
                P.tt("dve", t1[sl, :], psa[sl, :], self.cosL[sl, cols], ALU.mult, [pba, self.b_rope], [t1b])
                P.tt("dve", t2[sl, :], psb_[sl, :], self.sinL[sl, cols], ALU.mult, [pbb, self.b_rope], [t2b])
                j, krst, krstb = krst_p.next()
                P.tt("dve", krst[sl, :], t1[sl, :], t2[sl, :], ALU.add, [t1b, t2b], [krstb])
                for h in range(4):
                    P.dma("sp", "krst%d" % j, KV.ap()[h * 96 + 64:h * 96 + 96, :], krst[sl, :], [krstb], [bKV])
                j, vst, vstb = vst_p.next()
                for tp in range(2):
                    ps, pb = self.ps_next()
                    for t2_ in range(2):
                        t = tp * 2 + t2_
                        P.mm(ps[:, t2_ * 256:(t2_ + 1) * 256], ckvn[:, 0, t * 128:(t + 1) * 128], Wukv[:, 256:512],
                             True, True, [ckvnb, bWukv], [pb])
                    P.cp(self.evac_eng(), vst[:, tp * 2:tp * 2 + 2, :], ps[:].rearrange("p (t c) -> p t c", c=256), [pb], [vstb])
                P.dma("sp", "vst%d" % j, VX.ap()[:, 0:256].rearrange("(t p) c -> p t c", p=128), vst[:], [vstb], [bVX])
                for (a_, ba_, o_, bo_) in ((KV, bKV, X["KVg"][s], X["bKVg"][s]), (VX, bVX, X["Vg"][s], X["bVg"][s])):
                    S.dma("pool", "ccKV%d" % s,
                          lambda e, a_=a_, o_=o_: e.collective_compute("AllGather", ALU.bypass, replica_groups=groups,
                                                                       ins=[a_.ap().opt()], outs=[o_.ap().opt()]),
                          [ba_], [X["bKVg"][s], X["bVg"][s]], inc=1)
            P.dma("sp", "spx", X["spx"].ap(), spown[:], [bspo], [X["bspx"]])
            S.dma("pool", "ccS",
                  lambda e: e.collective_compute("AllGather", ALU.bypass, replica_groups=groups,
                                                 ins=[X["spx"].ap().opt()], outs=[X["spg"].ap().opt()]),
                  [X["bspx"]], [X["bspg"]], inc=1)
            S.barrier()

    def stage_C(self, l, negcK, negcKb, cqa, cqab):
        S, P = self.S, self
        bc = self.b_const
        X, dr, db = self.xch, self.dr, self.dbuf
        with ExitStack() as st:
            spall = self.sb(st, "spall", [128, 32 * 4], F32)
            bsp = Buf("spall")
            for s in range(4):
                for r in range(2):
                    g = G_PAR[r][s]
                    P.dma("sp", "spall", spall[:, g * 16:(g + 1) * 16], X["spg"].ap()[r * 128:(r + 1) * 128, s * 16:(s + 1) * 16],
                          [X["bspg"]], [bsp])
            import os
            cumb = os.environ.get("CUMB", "1") == "1"
            if cumb:
                spre = self.sb(st, "spre", [128, 32 * 4], F32)
                bspre = Buf("spre")
                S.op("dve", lambda e: e.memset(spre[:], 0.0), [], [bspre])
                for gt in range(31):
                    P.tt("dve", spre[:, (gt + 1) * 4:(gt + 2) * 4], spre[:, gt * 4:(gt + 1) * 4],
                         spall[:, gt * 4:(gt + 1) * 4], ALU.add, [bspre, bsp], [bspre])
                ps2, pb2 = self.ps_next()
                P.mm(ps2[:, 0:128], self.UT[:], spall[:], True, False, [bc, bsp], [pb2])
                P.mm(ps2[:, 0:128], self.ones_f[:], spre[:], False, True, [bc, bspre], [pb2])
                P.cp("dve", negcK[:], ps2[:, 0:128], [pb2], [negcKb])
            for g in range(8):
                for t in range(4):
                    if cumb:
                        break
                    gt = g * 4 + t
                    ps2, pb2 = self.ps_next()
                    P.mm(ps2[:, 0:4], self.UT[:], spall[:, gt * 4:(gt + 1) * 4], True, gt == 0, [bc, bsp], [pb2])
                    for tp_ in range(gt):
                        P.mm(ps2[:, 0:4], self.ones_f[:], spall[:, tp_ * 4:(tp_ + 1) * 4], False, tp_ == gt - 1,
                             [bc, bsp], [pb2])
                    P.cp("dve", negcK[:, gt * 4:(gt + 1) * 4], ps2[:, 0:4], [pb2], [negcKb])
                par = 0 if g in G_PAR[0] else 1
                s_ = G_PAR[par].index(g)
                ps, pb = self.ps_next()
                for t in range(4):
                    gt = g * 4 + t
                    P.mm(ps[0:4, t * 128:(t + 1) * 128], negcK[:, gt * 4:(gt + 1) * 4], self.ident[:], True, True,
                         [negcKb, bc], [pb])
                P.stt("dve", cqa[:, s_ * 512:(s_ + 1) * 512], ps[0:4, :], self.sel8[0:4, par:par + 1],
                      cqa[:, s_ * 512:(s_ + 1) * 512], ALU.mult, ALU.add, [pb, bc, cqab], [cqab])
            S.barrier()


    def stage_K(self, l, xfull_rows, W, negcK, negcKb, cqa, cqab):
        S, P = self.S, self
        bc = self.b_const
        dr = self.dr
        db = self.dbuf
        r3 = lambda name, m: W[name].ap().rearrange("p (k m) -> p k m", m=m)
        with ExitStack() as st:
            pools = self.mk_pools(st)
            RT = self.rope_tmp(st, 512)
            cosg_p = self.pool(st, "cosg", [128, 512], BF16, 2)
            sing_p = self.pool(st, "sing", [128, 512], BF16, 2)
            Wf, bWf = self.load_w(st, "Wf", [128, 8, 4], r3("wK_f", 4))
            Wsbk, bWsbk = self.load_w(st, "Wsbk", [128, 8, 256], r3("wK_sbk", 256))
            Wfxk, bWfxk = self.load_w(st, "Wfxk", [128, 8, 256], r3("wK_fxk", 256))
            Wv2, bWv2 = self.load_w(st, "Wv2", [128, 8, 512], r3("wK_v2", 512))
            Wckv, bWckv = self.load_w(st, "Wckv", [128, 8, 128], r3("wK_ckv", 128))
            Wkr, bWkr = self.load_w(st, "Wkr", [128, 8, 128], r3("wK_kr", 128))
            Wkrr, bWkrr = self.load_w(st, "Wkrr", [128, 8, 128], r3("wK_krr", 128))
            S.op("dve", lambda e: e.tensor_scalar_mul(out=Wkrr[:, :, 64:80], in0=Wkrr[:, :, 64:80], scalar1=-1.0),
                 [bWkrr], [bWkrr])
            ukf, bukf = self.load_w(st, "ukf", [128, 512], None, cast=False, skip=True)
            kvn, bkvn = self.load_w(st, "kvn", [128, 1], W["kvn"].ap(), cast=False)
            P.dma("sp", "w_ukf", ukf[:, 0:256], W["w_ukv_k"].ap(), [], [bukf])
            P.dma("sp", "w_ukf", ukf[:, 256:512], W["w_ukv_v"].ap(), [], [bukf])
            Wukv = self.sb(st, "Wukv", [128, 512], BF16)
            bWukv = Buf("Wukv")
            P.ts("dve", Wukv[:], ukf[:], kvn[:, 0:1], None, ALU.mult, None, [bukf, bkvn], [bWukv])
            bfg, bbfg = self.load_w(st, "bfg", [128, 4], W["bfg"].ap().partition_broadcast(128), cast=False)
            xTg = self.pool(st, "xTg", [128, 8, 512], BF16, 2)
            ckvn_p = self.pool(st, "ckvn", [128, 1, 512], BF16, 2)
            kst_p = self.pool(st, "kst", [128, 4, 512], BF16, 2)
            krst_p = self.pool(st, "krst", [128, 512], BF16, 2)
            vst_p = self.pool(st, "vst", [128, 4, 256], BF16, 2)
            vst2_p = self.pool(st, "vst2", [128, 4, 512], BF16, 2)
            sm_p = self.pool(st, "smallK", [128, 16], F32, 4)
            spall = self.sb(st, "spall", [128, 32 * 4], F32)
            bsp = [Buf("sp%d" % g_) for g_ in range(8)]
            S.op("pool", lambda e: e.memset(cqa[:], 0.0), [], [cqab])
            f32p = pools["f32"]
            import os
            ksub = int(os.environ.get("KSUB", "9"))
            for g in range(8):
                cols = slice(g * 512, (g + 1) * 512)
                rows = xfull_rows(g)
                j, xT, xTb = xTg.next()
                self.transpose_tiles(pools, [rows[t * 128:(t + 1) * 128, :] for t in range(4)], xT, xTb, 0)
                for t in range(4):
                    gt = g * 4 + t
                    ps, pb = self.ps_next()
                    for dc in range(8):
                        P.mm(ps[:, 0:4], xT[:, dc, t * 128:(t + 1) * 128], Wf[:, dc, :], dc == 0, dc == 7,
                             [xTb, bWf], [pb])
                    j, sm, smb = sm_p.next()
                    P.tt("dve", sm[:, 0:4], ps[:, 0:4], bfg[:], ALU.add, [pb, bbfg], [smb])
                    P.act(sm[:, 4:8], sm[:, 0:4], AF.Exp, [smb], [smb], scale=-1.0)
                    P.act(spall[:, gt * 4:(gt + 1) * 4], sm[:, 4:8], AF.Ln, [smb, bc], [bsp[g]], bias=self.onec[:, 0:1])
                for (Wk, bWk, name) in ((Wsbk, bWsbk, "KTs"), (Wfxk, bWfxk, "KTf")):
                    j, kst, kstb = kst_p.next()
                    for pr in range(2):
                        ps, pb = self.ps_next()
                        for dc in range(8):
                            P.mm(ps[:], Wk[:, dc, pr * 128:(pr + 1) * 128], xT[:, dc, :], dc == 0, dc == 7,
                                 [bWk, xTb], [pb])
                        P.cp(self.evac_eng(), kst[:, pr, :], ps[:], [pb], [kstb])
                        P.dma("sp", "kst%d" % j, dr[name].ap()[2 * pr:2 * pr + 2, :, cols].rearrange("h p t -> (h p) t"),
                              kst[:, pr, :], [kstb], [db[name]])
                j, vst2, vst2b = vst2_p.next()
                for t in range(4):
                    ps, pb = self.ps_next()
                    for dc in range(8):
                        P.mm(ps[:], xT[:, dc, t * 128:(t + 1) * 128], Wv2[:, dc, :], dc == 0, dc == 7, [xTb, bWv2], [pb])
                    P.cp(self.evac_eng(), vst2[:, t, :], ps[:], [pb], [vst2b])
                P.dma("sp", "vst2a%d" % j, dr["Vs"].ap()[cols, :].rearrange("(t p) c -> p t c", p=128),
                      vst2[:, :, 0:256], [vst2b], [db["Vs"]])
                P.dma("sp", "vst2b%d" % j, dr["Vf"].ap()[cols, :].rearrange("(t p) c -> p t c", p=128),
                      vst2[:, :, 256:512], [vst2b], [db["Vf"]])
                ps, pb = self.ps_next()
                for dc in range(8):
                    P.mm(ps[:], Wckv[:, dc, :], xT[:, dc, :], dc == 0, dc == 7, [bWckv, xTb], [pb])
                j, ckvn, ckvnb = ckvn_p.next()
                self.rms_scale(pools, [ps], [pb], 128, ckvn, ckvnb)
                j, kst, kstb = kst_p.next()
                for h in range(4):
                    ps, pb = self.ps_next()
                    P.mm(ps[0:64, :], Wukv[:, h * 64:(h + 1) * 64], ckvn[:, 0, :], True, True, [bWukv, ckvnb], [pb])
                    P.cp(self.evac_eng(), kst[0:64, h, :], ps[0:64, :], [pb], [kstb])
                P.dma("sp", "kst%d" % j, dr["KTm"].ap()[:, 0:64, cols].rearrange("h p t -> p h t"), kst[0:64, :, :],
                      [kstb], [db["KTm"]])
                psa, pba = self.ps_next()
                for dc in range(8):
                    P.mm(psa[:], Wkr[:, dc, :], xT[:, dc, :], dc == 0, dc == 7, [bWkr, xTb], [pba])
                psb_, pbb = self.ps_next()
                for dc in range(8):
                    P.mm(psb_[:], Wkrr[:, dc, :], xT[:, dc, :], dc == 0, dc == 7, [bWkrr, xTb], [pbb])
                j1, t1, t1b = f32p.next()
                j2, t2, t2b = f32p.next()
                sl = slice(64, 96)
                jc, cosg, cosgb = cosg_p.next()
                js, sing, singb = sing_p.next()
                self.rope_tables(RT, self.pos_full.ap()[:, cols], 512, cosg[sl, :], sing[sl, :], cosgb)
                P.tt("dve", t1[sl, :], psa[sl, :], cosg[sl, :], ALU.mult, [pba, cosgb], [t1b])
                P.tt("dve", t2[sl, :], psb_[sl, :], sing[sl, :], ALU.mult, [pbb, cosgb], [t2b])
                j, krst, krstb = krst_p.next()
                P.tt("dve", krst[sl, :], t1[sl, :], t2[sl, :], ALU.add, [t1b, t2b], [krstb])
                for h in range(4):
                    P.dma("sp", "krst%d" % j, dr["KTm"].ap()[h, 64:96, cols], krst[sl, :], [krstb], [db["KTm"]])
                j, vst, vstb = vst_p.next()
                for tp in range(2):
                    ps, pb = self.ps_next()
                    for t2_ in range(2):
                        t = tp * 2 + t2_
                        P.mm(ps[:, t2_ * 256:(t2_ + 1) * 256], ckvn[:, 0, t * 128:(t + 1) * 128], Wukv[:, 256:512],
                             True, True, [ckvnb, bWukv], [pb])
                    P.cp(self.evac_eng(), vst[:, tp * 2:tp * 2 + 2, :],
                         ps[:].rearrange("p (t c) -> p t c", c=256), [pb], [vstb])
                P.dma("sp", "vst%d" % j, dr["Vm"].ap()[cols, :].rearrange("(t p) c -> p t c", p=128), vst[:],
                      [vstb], [db["Vm"]])
                for t in range(4):
                    gt = g * 4 + t
                    ps2, pb2 = self.ps_next()
                    P.mm(ps2[:, 0:4], self.UT[:], spall[:, gt * 4:(gt + 1) * 4], True, gt == 0, [bc, bsp[g]], [pb2])
                    for tp_ in range(gt):
                        P.mm(ps2[:, 0:4], self.ones_f[:], spall[:, tp_ * 4:(tp_ + 1) * 4], False, tp_ == gt - 1,
                             [bc, bsp[tp_ // 4]], [pb2])
                    P.cp("dve", negcK[:, gt * 4:(gt + 1) * 4], ps2[:, 0:4], [pb2], [negcKb])
                par = 0 if g in G_PAR[0] else 1
                s_ = G_PAR[par].index(g)
                ps, pb = self.ps_next()
                for t in range(4):
                    gt = g * 4 + t
                    P.mm(ps[0:4, t * 128:(t + 1) * 128], negcK[:, gt * 4:(gt + 1) * 4], self.ident[:], True, True,
                         [negcKb, bc], [pb])
                P.stt("dve", cqa[:, s_ * 512:(s_ + 1) * 512], ps[0:4, :], self.sel8[0:4, par:par + 1],
                      cqa[:, s_ * 512:(s_ + 1) * 512], ALU.mult, ALU.add, [pb, bc, cqab], [cqab])
            S.barrier()

    def stage_Q(self, l, xfull_rows, xloc_rows, W, actT, actTb, yT, yTb, negcK, negcKb, cqa, cqab):
        S, P = self.S, self
        bc = self.b_const
        dr, db = self.dr, self.dbuf
        r3 = lambda name, m: W[name].ap().rearrange("p (k m) -> p k m", m=m)
        xlT = actT
        if self.kv_exchange:
            self.stage_K2(l, W, xlT, actTb, cqa, cqab, xloc_rows)
        else:
            with ExitStack() as st:
                pools = self.mk_pools(st)
                for s in range(4):
                    self.transpose_tiles(pools, [xloc_rows(s * 4 + t) for t in range(4)], actT, actTb[s], s * 512)
                S.barrier()

        with ExitStack() as st:
            pools = self.mk_pools(st, xin=False)
            f32p, bfp = pools["f32"], pools["bf"]
            Wcv, bWcv = self.load_w(st, "Wcv", [128, 8, 512], r3("wQ_cv", 512))
            cw, bcw = self.load_w(st, "convw", [128, 2, 31], W["convw"].ap().rearrange("p (c j) -> p c j", j=31), cast=False)
            cb_, bcb = self.load_w(st, "convb", [128, 2], W["convb"].ap(), cast=False)
            cg, bcg = self.load_w(st, "clng", [128, 2], W["clng"].ap(), cast=False)
            cbt, bcbt = self.load_w(st, "clnb", [128, 2], W["clnb"].ap(), cast=False)
            dg = self.sb(st, "dg", [128, 2, 31, 128], BF16)
            bdg = Buf("dg")
            for cc in range(2):
                for j in range(31):
                    P.ts("dve", dg[:, cc, j, :], self.ident[:], cw[:, cc, j:j + 1], None, ALU.mult, None,
                         [bc, bcw], [bdg])
            hp = self.sb(st, "hp", [128, 2, 4, 544], BF16)
            bhp = [Buf("hp%d" % s) for s in range(4)]
            bhalo = Buf("halo")
            xa = self.sb(st, "xha", [128, 1024], F32)
            xb = self.sb(st, "xhb", [128, 1024], F32)
            bxa, bxb = Buf("xa"), Buf("xb")
            S.op("pool", lambda e: e.memset(xa[0:32, :], 0.0), [], [bxa])
            for s in range(4):
                ga, gb = G_PAR[0][s] - 1, G_PAR[1][s] - 1
                xfb = getattr(self, "xfull_bufs", None) or (lambda g_: [])
                if ga >= 0:
                    P.dma("sp", "xha", xa[s * 32:(s + 1) * 32, :], xfull_rows(ga)[480:512, :], xfb(ga), [bxa])
                P.dma("sp", "xhb", xb[s * 32:(s + 1) * 32, :], xfull_rows(gb)[480:512, :], xfb(gb), [bxb])
            P.ts("dve", xa[:], xa[:], self.sel[:, 0:1], None, ALU.mult, None, [bxa, bc], [bxa])
            P.stt("dve", xa[:], xb[:], self.sel[:, 1:2], xa[:], ALU.mult, ALU.add, [bxb, bc, bxa], [bxa])
            xhT = self.sb(st, "xhT", [128, 8, 128], BF16)
            bxhT = Buf("xhT")
            for dc in range(8):
                ps, pb = self.ps_next()
                P.tr(ps[:, 0:128], xa[:, dc * 128:(dc + 1) * 128], [bxa], [pb])
                P.cp(self.evac_eng(), xhT[:, dc, :], ps[:, 0:128], [pb], [bxhT])

            def glu(cc, rhs_fn, ncols, rbufs, out_ap, out_buf):
                psa, pba = self.ps_next()
                for dc in range(8):
                    P.mm(psa[:, 0:ncols], Wcv[:, dc, cc * 128:(cc + 1) * 128], rhs_fn(dc), dc == 0, dc == 7,
                         [bWcv] + rbufs, [pba])
                psg, pbg = self.ps_next()
                for dc in range(8):
                    P.mm(psg[:, 0:ncols], Wcv[:, dc, 256 + cc * 128:256 + (cc + 1) * 128], rhs_fn(dc), dc == 0, dc == 7,
                         [bWcv] + rbufs, [pbg])
                j, sg, sgb = f32p.next()
                P.act(sg[:, 0:ncols], psg[:, 0:ncols], AF.Sigmoid, [pbg], [sgb])
                P.tt("dve", out_ap, psa[:, 0:ncols], sg[:, 0:ncols], ALU.mult, [pba, sgb], [out_buf])

            for cc in range(2):
                j, hh, hhb = bfp.next()
                glu(cc, lambda dc: xhT[:, dc, :], 128, [bxhT], hh[:, 0:128], hhb)
                P.cp("dve", hp[:, cc, :, 0:32], hh[:, 0:128].rearrange("p (s t) -> p s t", t=32), [hhb], bhp)
                for s in range(4):
                    glu(cc, lambda dc, s=s: xlT[:, dc, s * 512:(s + 1) * 512], 512, [actTb[s]],
                        hp[:, cc, s, 32:544], bhp[s])
            for s in range(4):
                ys = []
                for cc in range(2):
                    ps, pb = self.ps_next(0, 4)
                    for j in range(31):
                        P.mm(ps[:], dg[:, cc, j, :], hp[:, cc, s, 2 + j:2 + j + 512], j == 0, j == 30, [bdg, bhp[s]], [pb])
                    jj, y, yb = f32p.next()
                    P.ts("dve", y[:], ps[:], cb_[:, cc:cc + 1], None, ALU.add, None, [pb, bcb], [yb])
                    ys.append((y, yb))
                pss, pbs = self.ps_next(4, 8)
                psq, pbq = self.ps_next(4, 8)
                for cc, (y, yb) in enumerate(ys):
                    j1, ybf, ybfb = bfp.next()
                    j2, ysq, ysqb = bfp.next()
                    P.cp("dve", ybf[:], y[:], [yb], [ybfb])
                    P.act(ysq[:], y[:], AF.Square, [yb], [ysqb])
                    P.mm(pss[:], self.ones_bf[:], ybf[:], cc == 0, cc == 1, [bc, ybfb], [pbs])
                    P.mm(psq[:], self.ones_bf[:], ysq[:], cc == 0, cc == 1, [bc, ysqb], [pbq])
                jm, mean, meanb = f32p.next()
                P.act(mean[:], pss[:], AF.Copy, [pbs], [meanb], scale=1.0 / 256)
                jv, var, varb = f32p.next()
                P.act(var[:], mean[:], AF.Square, [meanb], [varb])
                P.stt("dve", var[:], psq[:], 1.0 / 256, var[:], ALU.mult, ALU.subtract, [pbq, varb], [varb])
                P.act(var[:], var[:], AF.Sqrt, [varb, bc], [varb], bias=self.epsc[:, 0:1])
                S.op("dve", lambda e, o=var[:]: e.reciprocal(out=o, in_=o), [varb], [varb])
                for cc, (y, yb) in enumerate(ys):
                    P.tt("dve", y[:], y[:], mean[:], ALU.subtract, [yb, meanb], [yb])
                    P.tt("dve", y[:], y[:], var[:], ALU.mult, [yb, varb], [yb])
                    P.act(yT[2][:, cc, s * 512:(s + 1) * 512], y[:], AF.Silu, [yb, bcg, bcbt], [yTb[2][s]],
                          bias=cbt[:, cc:cc + 1], scale=cg[:, cc:cc + 1])
            S.barrier()

        if self.kv_exchange:
            self.stage_C(l, negcK, negcKb, cqa, cqab)
        import os
        if int(os.environ.get("KSTOP", "9")) < 3:
            return
        self.attn_mla(l, W, xlT, actTb, yT[0], yTb[0])
        self.attn_sbfox(l, W, xlT, actTb, yT[1], yTb[1], "sb", None, None, None, None)
        self.attn_sbfox(l, W, xlT, actTb, yT[3], yTb[3], "fox", negcK, negcKb, cqa, cqab)

    def stage_G(self, l, W, xlT, actTb, yT, yTb, mrgT, mrgTb):
        S, P = self.S, self
        with ExitStack() as st:
            pools = self.mk_pools(st, xin=False)
            f32p = pools["f32"]
            Wb, bWb = self.load_w(st, "Wb", [128, 4, 2, 1024], W["wB"].ap().rearrange("p (n c d) -> p n c d", n=4, c=2))
            bg, bbg = self.load_w(st, "bg", [128, 32], W["bg"].ap(), cast=False)
            wg_p = self.pool(st, "wg", [128, 8, 128], BF16, 3)
            acc = self.sb(st, "gacc", [128, 4, 512], F32)
            baccs = [Buf("gacc%d" % s) for s in range(4)]
            for dc in range(8):
                for n in range(4):
                    ci = dc * 4 + n
                    j, wg, wgb = wg_p.next()
                    P.dma("pool", "wg%d" % j, wg[:], W["wG"].ap()[ci].rearrange("p (k m) -> p k m", m=128), [], [wgb])
                    for s in range(4):
                        cols = slice(s * 512, (s + 1) * 512)
                        psg, pbg = self.ps_next()
                        for kc in range(8):
                            P.mm(psg[:], wg[:, kc, :], xlT[:, kc, cols], kc == 0, kc == 7, [wgb, actTb[s]], [pbg])
                        jg, gs_, gsb = f32p.next()
                        P.act(gs_[:], psg[:], AF.Sigmoid, [pbg, bbg], [gsb], bias=bg[:, ci:ci + 1])
                        psp, pbp = self.ps_next()
                        for cc in range(2):
                            P.mm(psp[:], Wb[:, n, cc, dc * 128:(dc + 1) * 128], yT[n][:, cc, cols], cc == 0, cc == 1,
                                 [bWb, yTb[n][s]], [pbp])
                        if n == 0:
                            P.tt("dve", acc[:, s, :], gs_[:], psp[:], ALU.mult, [gsb, pbp], [baccs[s]])
                        else:
                            P.tt("dve", gs_[:], gs_[:], psp[:], ALU.mult, [gsb, pbp], [gsb])
                            if n < 3:
                                P.tt("dve", acc[:, s, :], acc[:, s, :], gs_[:], ALU.add, [baccs[s], gsb], [baccs[s]])
                            else:
                                P.tt("dve", mrgT[:, dc, cols], acc[:, s, :], gs_[:], ALU.add, [baccs[s], gsb], [mrgTb[s]])
            S.barrier()

    def make_pen(self, st, pools, strict):
        S, P = self.S, self
        bc = self.b_const
        Dm = self.D1 if strict else self.D0
        penb = self.sb(st, "penb", [128, 16, 512], BF16)
        penbb = Buf("penb")
        f32p = pools["f32"]
        for i in range(16):
            jt, tmp, tmpb = f32p.next()
            P.ts("dve", tmp[:], Dm[:], self.thr[:, i:i + 1], 0.0, ALU.subtract, ALU.max, [bc], [tmpb])
            P.ts("dve", penb[:, i, :], tmp[:], NEGBIG, None, ALU.mult, None, [tmpb], [penbb])
        return penb, penbb

    def attn_core(self, kind, pools, KT, KTb, kdim, QT, QTb, qtn_p, Vt, Vtb, h, s, scale, yTn, yTnb, penb, penbb,
                  negcK=None, negcKb=None, rs_p=None):
        S, P = self.S, self
        bc = self.b_const
        f32p, bfp = pools["f32"], pools["bf"]
        NB = 8 * (s + 1)
        qc = slice(s * 512, (s + 1) * 512)
        cls = s % 2
        hp = h // 2
        acc_o, bo = self.ps_next(4, 8)
        order = list(range(NB))
        QTn = QTnb = None
        if kind == "sb":
            order = order[::-1]
            jq, QTn, QTnb = qtn_p.next()
            P.ts("dve", QTn[0:kdim, :], QT[0:kdim, h, qc], -0.125, None, ALU.mult, None, [QTb[s]], [QTnb])
        rs_state = [None]
        pv_pend = []

        def stage1(i):
            kb = order[i]
            kc = slice(kb * 128, (kb + 1) * 128)
            masked = kb >= 8 * s
            ps, pb = self.ps_next(0, 4)
            P.mm(ps[:], KT[0:kdim, kc], QT[0:kdim, h, qc], True, True, [KTb, QTb[s]], [pb])
            src, srcb = ps, pb
            pj = None
            if masked:
                pj = cls * 8 + (kb - 8 * s)
                jz, zm, zmb = f32p.next()
                P.tt("dve", zm[:], ps[:], penb[:, pj, :], ALU.add, [pb, penbb], [zmb])
                src, srcb = zm, zmb
            if kind != "sb":
                jP, Pt, Ptb = bfp.next()
                if kind == "fox":
                    P.act(Pt[:], src[:], AF.Exp, [srcb, negcKb], [Ptb], scale=scale,
                          bias=negcK[:, kb * 4 + h:kb * 4 + h + 1])
                else:
                    P.act(Pt[:], src[:], AF.Exp, [srcb], [Ptb], scale=scale)
                return (kb, kc, pj, Pt, Ptb)
            je, ee, eeb = f32p.next()
            P.act(ee[:], src[:], AF.Exp, [srcb], [eeb], scale=scale)
            jsp, sp, spb = bfp.next()
            P.act(sp[:], ee[:], AF.Ln, [eeb, bc], [spb], bias=self.onec[:, 0:1])
            return (kb, kc, pj, sp, spb)

        def stage2(i, st1):
            kb, kc, pj, t, tb = st1
            first, last = (i == 0), (i == NB - 1)
            if kind != "sb":
                P.mm(acc_o[:], Vt[:, kb, :], t[:], first, last, [Vtb, tb], [bo])
                return
            sp, spb = t, tb
            prev_rs = rs_state[0]
            psG, pbG = self.ps_next(0, 4)
            P.mm(psG[:], self.LT[:], sp[:], True, False, [bc, spb], [pbG])
            if prev_rs is not None:
                P.mm(psG[:], self.ones_bf[:], prev_rs[0][:], False, False, [bc, prev_rs[1]], [pbG])
            P.mm(psG[:], KT[0:kdim, kc], QTn[0:kdim, :], False, True, [KTb, QTnb], [pbG])
            srcG, srcGb = psG, pbG
            if pj is not None:
                jg, gm, gmb = f32p.next()
                P.tt("dve", gm[:], psG[:], penb[:, pj, :], ALU.subtract, [pbG, penbb], [gmb])
                srcG, srcGb = gm, gmb
            jA, At, Atb = bfp.next()
            P.act(At[:], srcG[:], AF.Exp, [srcGb], [Atb], scale=-1.0)
            pv_pend.append((kb, At, Atb, first, last))
            if not last:
                jr, rs, rsb = rs_p.next()
                if prev_rs is None:
                    P.cp("dve", rs[:], sp[:], [spb], [rsb])
                else:
                    P.tt("dve", rs[:], prev_rs[0][:], sp[:], ALU.add, [prev_rs[1], spb], [rsb])
                rs_state[0] = (rs, rsb)

        def stage3():
            kb, At, Atb, first, last = pv_pend.pop(0)
            P.mm(acc_o[:], Vt[:, kb, hp * 128:(hp + 1) * 128], At[:], first, last, [Vtb, Atb], [bo])

        DEPTH_PIPE = 2 if kind != "sb" else 1
        pend = []
        for i in range(NB):
            pend.append((i, stage1(i)))
            if len(pend) > DEPTH_PIPE:
                i0, st0 = pend.pop(0)
                stage2(i0, st0)
            if len(pv_pend) > 1:
                stage3()
        for i0, st0 in pend:
            stage2(i0, st0)
        while pv_pend:
            stage3()
        po = (h % 2) * 64
        dst = yTn[po:po + 64, h // 2, qc]
        if kind == "sb":
            P.cp("dve", dst, acc_o[po:po + 64, :], [bo], [yTnb[s]])
        else:
            jr, rec, recb = f32p.next()
            S.op("dve", lambda e, o=rec[0:64, :], i=acc_o[64:128, :]: e.reciprocal(out=o, in_=i), [bo], [recb])
            P.tt("dve", dst, acc_o[0:64, :], rec[0:64, :], ALU.mult, [bo, recb], [yTnb[s]])

    def kv_src(self, kind, h, g):
        X = self.xch
        r = 0 if g in G_PAR[0] else 1
        s_ = G_PAR[r].index(g)
        base, kd = {"mla": (0, 96), "sb": (384, 64), "fox": (640, 64)}[kind]
        r0 = r * 896 + base + h * kd
        return X["KVg"][s_].ap()[r0:r0 + kd, :], X["bKVg"][s_]

    def v_src(self, kind, g, c0, c1):
        X = self.xch
        r = 0 if g in G_PAR[0] else 1
        s_ = G_PAR[r].index(g)
        cb = {"mla": 0, "sb": 256, "fox": 512}[kind]
        return (X["Vg"][s_].ap()[r * 512:(r + 1) * 512, cb + c0:cb + c1].rearrange("(t p) c -> p t c", p=128),
                X["bVg"][s_])

    def attn_mla(self, l, W, xlT, actTb, yTn, yTnb):
        S, P = self.S, self
        bc = self.b_const
        dr, db = self.dr, self.dbuf
        with ExitStack() as st:
            pools = self.mk_pools(st, xin=False)
            f32p = pools["f32"]
            Wcq, bWcq = self.load_w(st, "Wcq", [128, 8, 256], W["wQ_cq"].ap().rearrange("p (k m) -> p k m", m=256))
            qn, bqn = self.load_w(st, "qn", [128, 2], W["qn"].ap(), cast=False)
            Wuq = self.sb(st, "Wuq", [128, 2, 384], BF16)
            Wuqr = self.sb(st, "Wuqr", [128, 2, 384], BF16)
            bWuq, bWuqr = Buf("Wuq"), Buf("Wuqr")
            with ExitStack() as st2:
                uf, buf_ = self.load_w(st2, "uqf", [128, 2, 384], W["w_uq"].ap().rearrange("p (k m) -> p k m", m=384), cast=False)
                ur, bur = self.load_w(st2, "uqrf", [128, 2, 384], W["w_uqr"].ap().rearrange("p (k m) -> p k m", m=384), cast=False)
                for rc in range(2):
                    P.ts("dve", Wuq[:, rc, :], uf[:, rc, :], qn[:, rc:rc + 1], None, ALU.mult, None, [buf_, bqn], [bWuq])
                    P.ts("dve", Wuqr[:, rc, :], ur[:, rc, :], qn[:, rc:rc + 1], None, ALU.mult, None, [bur, bqn], [bWuqr])
                    for h in range(4):
                        o = h * 96 + 64
                        P.ts("dve", Wuqr[:, rc, o:o + 16], Wuqr[:, rc, o:o + 16], -1.0, None, ALU.mult, None,
                             [bWuqr], [bWuqr])
                S.barrier()
            QT = self.sb(st, "QTm", [128, 4, TL], BF16)
            QTb = [Buf("QTm%d" % s) for s in range(4)]
            cqn_p = self.pool(st, "cqn", [128, 2, 512], BF16, 2)
            sl = slice(64, 96)
            for s in range(4):
                qc = slice(s * 512, (s + 1) * 512)
                tc_ = slice(SEQ + s * 512, SEQ + (s + 1) * 512)
                pss, pbs = [], []
                for rc in range(2):
                    ps, pb = self.ps_next(0, 4)
                    for dc in range(8):
                        P.mm(ps[:], Wcq[:, dc, rc * 128:(rc + 1) * 128], xlT[:, dc, qc], dc == 0, dc == 7,
                             [bWcq, actTb[s]], [pb])
                    pss.append(ps)
                    pbs.append(pb)
                j, cqn, cqnb = cqn_p.next()
                self.rms_scale(pools, pss, pbs, 256, cqn, cqnb, ps_range=(4, 8))
                for h in range(4):
                    psa, pba = self.ps_next(4, 8)
                    psb_, pbb = self.ps_next(4, 8)
                    for rc in range(2):
                        P.mm(psa[0:96, :], Wuq[:, rc, h * 96:(h + 1) * 96], cqn[:, rc, :], rc == 0, rc == 1,
                             [bWuq, cqnb], [pba])
                    for rc in range(2):
                        P.mm(psb_[0:96, :], Wuqr[:, rc, h * 96:(h + 1) * 96], cqn[:, rc, :], rc == 0, rc == 1,
                             [bWuqr, cqnb], [pbb])
                    P.cp("act", QT[0:64, h, qc], psa[0:64, :], [pba], [QTb[s]])
                    j1, t1, t1b = f32p.next()
                    j2, t2, t2b = f32p.next()
                    P.tt("dve", t1[sl, :], psa[sl, :], self.cosL[sl, qc], ALU.mult, [pba, self.b_rope], [t1b])
                    P.tt("dve", t2[sl, :], psb_[sl, :], self.sinL[sl, qc], ALU.mult, [pbb, self.b_rope], [t2b])
                    P.tt("dve", QT[sl, h, qc], t1[sl, :], t2[sl, :], ALU.add, [t1b, t2b], [QTb[s]])
            vh_p = self.pool(st, "Vh", [128, 32, 128], BF16, 2)
            for (tile_, b_) in zip(vh_p.tiles, vh_p.bufs):
                S.op("pool", lambda e, t=tile_: e.memset(t[:, :, 64:128], 1.0), [], [b_])
            kt_p = self.pool(st, "KT", [128, SEQ], BF16, 2)
            penb, penbb = self.make_pen(st, pools, False)
            for h in range(4):
                j, KT, KTb = kt_p.next()
                jv, Vt, Vtb = vh_p.next()
                if self.kv_exchange:
                    for g in range(8):
                        ka, kb_ = self.kv_src("mla", h, g)
                        P.dma("sp", "KT%d" % j, KT[0:96, g * 512:(g + 1) * 512], ka, [kb_], [KTb])
                        va, vb_ = self.v_src("mla", g, h * 64, (h + 1) * 64)
                        P.dma("sp", "Vh%d" % jv, Vt[:, g * 4:(g + 1) * 4, 0:64], va, [vb_], [Vtb])
                else:
                    P.dma("sp", "KT%d" % j, KT[0:96, :], dr["KTm"].ap()[h], [db["KTm"]], [KTb])
                    for q8 in range(8):
                        P.dma("sp", "Vh%d" % jv, Vt[:, q8 * 4:(q8 + 1) * 4, 0:64],
                              dr["Vm"].ap()[q8 * 512:(q8 + 1) * 512, h * 64:(h + 1) * 64].rearrange("(t p) c -> p t c", p=128),
                              [db["Vm"]], [Vtb])
                for s in range(4):
                    self.attn_core("mla", pools, KT, KTb, 96, QT, QTb, None, Vt, Vtb, h, s, float(96 ** -0.5), yTn, yTnb,
                                   penb, penbb)
            S.barrier()

    def attn_sbfox(self, l, W, xlT, actTb, yTn, yTnb, kind, negcK, negcKb, cqa, cqab):
        S, P = self.S, self
        bc = self.b_const
        dr, db = self.dr, self.dbuf
        fox = kind == "fox"
        kdim = 65
        with ExitStack() as st:
            pools = self.mk_pools(st, xin=False)
            Wq = self.sb(st, "Wq" + kind, [128, 8, 4, 65], BF16)
            bWq = Buf("Wq")
            S.op("pool", lambda e: e.memset(Wq[:], 0.0), [], [bWq])
            P.dma("pool", "w_Wq", Wq[:, :, :, 0:64],
                  W["wQ_fxq" if fox else "wQ_sbq"].ap().rearrange("p (k h m) -> p k h m", h=4, m=64), [], [bWq])
            QT = self.sb(st, "QT" + kind, [128, 4, TL], BF16)
            QTb = [Buf("QT%d" % s) for s in range(4)]
            qtn_p = None if fox else self.pool(st, "QTn", [128, 512], BF16, 2)
            for s in range(4):
                qc = slice(s * 512, (s + 1) * 512)
                for h in range(4):
                    ps, pb = self.ps_next(0, 4)
                    for dc in range(8):
                        P.mm(ps[0:kdim, :], Wq[:, dc, h, 0:kdim], xlT[:, dc, qc], dc == 0, (dc == 7) and not fox,
                             [bWq, actTb[s]], [pb])
                    if fox:
                        P.mm(ps[0:kdim, :], self.Eh[0:4, h * 65:(h + 1) * 65], cqa[0:4, qc], False, True,
                             [bc, cqab], [pb])
                    P.cp("act", QT[0:kdim, h, qc], ps[0:kdim, :], [pb], [QTb[s]])
            vname, kname = ("Vf", "KTf") if fox else ("Vs", "KTs")
            if fox:
                vh_p = self.pool(st, "Vhf", [128, 32, 128], BF16, 2)
                for (tile_, b_) in zip(vh_p.tiles, vh_p.bufs):
                    S.op("pool", lambda e, t=tile_: e.memset(t[:, :, 64:128], 1.0), [], [b_])
            else:
                Vt = self.sb(st, "Vt" + kind, [128, 32, 256], BF16)
                Vtb = Buf("Vt")
                for q8 in range(8):
                    if self.kv_exchange:
                        va, vb_ = self.v_src("sb", q8, 0, 256)
                        P.dma("sp", "Vt", Vt[:, q8 * 4:(q8 + 1) * 4, :], va, [vb_], [Vtb])
                    else:
                        P.dma("sp", "Vt", Vt[:, q8 * 4:(q8 + 1) * 4, :],
                              dr[vname].ap()[q8 * 512:(q8 + 1) * 512, :].rearrange("(t p) c -> p t c", p=128),
                              [db[vname]], [Vtb])
            kt_p = self.pool(st, "KT" + kind, [128, SEQ], BF16, 2)
            for (tile_, b_) in zip(kt_p.tiles, kt_p.bufs):
                S.op("pool", lambda e, t=tile_: e.memset(t[64:65, :], 1.0 if fox else 0.0), [], [b_])
            rs_p = None if fox else self.pool(st, "rsum", [128, 512], BF16, 2)
            penb, penbb = self.make_pen(st, pools, not fox)
            for h in range(4):
                j, KT, KTb = kt_p.next()
                if self.kv_exchange:
                    for g in range(8):
                        ka, kb_ = self.kv_src(kind, h, g)
                        P.dma("sp", "KT%d" % j, KT[0:64, g * 512:(g + 1) * 512], ka, [kb_], [KTb])
                else:
                    P.dma("sp", "KT%d" % j, KT[0:64, :], dr[kname].ap()[h, 0:64, :], [db[kname]], [KTb])
                if fox:
                    jv, Vt, Vtb = vh_p.next()
                    for q8 in range(8):
                        if self.kv_exchange:
                            va, vb_ = self.v_src("fox", q8, h * 64, (h + 1) * 64)
                            P.dma("sp", "Vh%d" % jv, Vt[:, q8 * 4:(q8 + 1) * 4, 0:64], va, [vb_], [Vtb])
                        else:
                            P.dma("sp", "Vh%d" % jv, Vt[:, q8 * 4:(q8 + 1) * 4, 0:64],
                                  dr[vname].ap()[q8 * 512:(q8 + 1) * 512, h * 64:(h + 1) * 64].rearrange("(t p) c -> p t c", p=128),
                                  [db[vname]], [Vtb])
                for s in range(4):
                    self.attn_core(kind, pools, KT, KTb, kdim, QT, QTb, qtn_p, Vt, Vtb, h, s, 0.125, yTn, yTnb,
                                   penb, penbb, negcK=negcK, negcKb=negcKb, rs_p=rs_p)
            S.barrier()

    def ln_a(self, r, rb, smp, junk_p):
        S, P = self.S, self
        j, sm, smb = smp.next()
        jj, junk, junkb = junk_p.next()
        S.op("dve", lambda e, o=sm[:, 0:1], i=r[:]: e.reduce_sum(out=o, in_=i, axis=AX.X), [rb], [smb])
        P.ts("dve", sm[:, 1:2], sm[:, 0:1], -1.0 / D, None, ALU.mult, None, [smb], [smb])
        P.act(r[:], r[:], AF.Identity, [rb, smb], [rb], bias=sm[:, 1:2])
        P.act(junk[:], r[:], AF.Square, [rb], [junkb])
        return (sm, smb, junk, junkb)

    def ln_b(self, r, rb, gbc, bbc, gb_bufs, st_):
        S, P = self.S, self
        sm, smb, junk, junkb = st_
        S.op("dve", lambda e, o=sm[:, 2:3], i=junk[:]: e.reduce_sum(out=o, in_=i, axis=AX.X), [junkb], [smb])
        P.act(sm[:, 3:4], sm[:, 2:3], AF.Sqrt, [smb, self.b_const], [smb], bias=self.epsc[:, 0:1], scale=1.0 / D)
        S.op("dve", lambda e, o=sm[:, 4:5], i=sm[:, 3:4]: e.reciprocal(out=o, in_=i), [smb], [smb])
        P.stt("dve", r[:], r[:], sm[:, 4:5], gbc[:], ALU.mult, ALU.mult, [rb, smb] + gb_bufs, [rb])
        P.tt("pool", r[:], r[:], bbc[:], ALU.add, [rb] + gb_bufs, [rb])

    def stage_F1(self, l, xloc_rows, W, actT, actTb, mrgT, mrgTb, WtT, WtTb):
        S, P = self.S, self
        bc = self.b_const
        dr, db = self.dr, self.dbuf
        with ExitStack() as st:
            Wo, bWo = self.load_w(st, "Wo", [128, 8, 1024], W["wO"].ap().rearrange("p (k m) -> p k m", m=1024))
            g1, bg1 = self.load_w(st, "ln1g", [128, D], W["ln1_g"].ap().partition_broadcast(128), cast=False)
            b1, bb1 = self.load_w(st, "ln1b", [128, D], W["ln1_b"].ap().partition_broadcast(128), cast=False)
            wR, bwR = self.load_w(st, "wR", [128, 8, 36], W["wR"].ap().rearrange("p (k m) -> p k m", m=36), cast=False)
            bR, bbR = self.load_w(st, "bR", [128, 36], W["bR"].ap().partition_broadcast(128), cast=False)
            xr_p = self.pool(st, "xr", [128, D], F32, 3)
            r_p = self.pool(st, "r", [128, D], F32, 4)
            hTf_p = self.pool(st, "hTf", [128, 8, 128], F32, 3)
            sm_p = self.pool(st, "smF", [128, 128], F32, 4)
            sm2_p = self.pool(st, "smF2", [128, 64], F32, 4)
            smln = self.pool(st, "smln", [128, 8], F32, 4)
            junk_p = self.pool(st, "junk", [128, D], F32, 2)
            gbb = Buf("gb")
            def phaseA(tile):
                s = tile // 4
                tcs = slice(tile * 128, (tile + 1) * 128)
                j, xr, xrb = xr_p.next()
                P.dma("sp", "xr%d" % j, xr[:], xloc_rows(tile), [], [xrb])
                jr, r, rb = r_p.next()
                for half in range(2):
                    ps, pb = self.ps_next()
                    for kc in range(8):
                        P.mm(ps[:], mrgT[:, kc, tcs], Wo[:, kc, half * 512:(half + 1) * 512], kc == 0, kc == 7,
                             [mrgTb[s], bWo], [pb])
                    P.stt("dve", r[:, half * 512:(half + 1) * 512], xr[:, half * 512:(half + 1) * 512], ALPHA, ps[:],
                          ALU.mult, ALU.add, [xrb, pb], [rb])
                lnst = self.ln_a(r, rb, smln, junk_p)
                return dict(tile=tile, s=s, tcs=tcs, r=r, rb=rb, jr=jr, lnst=lnst)

            def phaseB(st_):
                tile, s, tcs, r, rb, jr = st_["tile"], st_["s"], st_["tcs"], st_["r"], st_["rb"], st_["jr"]
                sm_, smb_, junk_, junkb_ = st_["lnst"]
                S.op("dve", lambda e, o=sm_[:, 2:3], i=junk_[:]: e.reduce_sum(out=o, in_=i, axis=AX.X), [junkb_], [smb_])
                P.act(sm_[:, 3:4], sm_[:, 2:3], AF.Sqrt, [smb_, self.b_const], [smb_], bias=self.epsc[:, 0:1], scale=1.0 / D)
                yield
                S.op("dve", lambda e, o=sm_[:, 4:5], i=sm_[:, 3:4]: e.reciprocal(out=o, in_=i), [smb_], [smb_])
                P.stt("dve", r[:], r[:], sm_[:, 4:5], g1[:], ALU.mult, ALU.mult, [rb, smb_, bg1, bb1], [rb])
                P.tt("pool", r[:], r[:], b1[:], ALU.add, [rb, bg1, bb1], [rb])
                yield
                P.dma("sp", "r%d" % jr, dr["h"].ap()[tile * 128:(tile + 1) * 128, :], r[:], [rb], [db["h"]])
                jh, hTf, hTfb = hTf_p.next()
                for q in range(2):
                    ps, pb = self.ps_next()
                    for i in range(4):
                        dc = q * 4 + i
                        P.tr(ps[:, i * 128:(i + 1) * 128], r[:, dc * 128:(dc + 1) * 128], [rb], [pb])
                    P.cp("act", hTf[:, q * 4:(q + 1) * 4, :], ps[:].rearrange("p (i t) -> p i t", t=128), [pb], [hTfb])
                st_["hTf"], st_["hTfb"] = hTf, hTfb
                yield
                for q in range(2):
                    P.cp("dve", actT[:, q * 4:(q + 1) * 4, tcs], hTf[:, q * 4:(q + 1) * 4, :], [hTfb],
                         [actTb[s]])

            def phaseC(st_):
                s, tcs, hTf, hTfb = st_["s"], st_["tcs"], st_["hTf"], st_["hTfb"]
                ps, pb = self.ps_next()
                for kc in range(8):
                    P.mm(ps[:, 0:36], hTf[:, kc, :], wR[:, kc, :], kc == 0, kc == 7, [hTfb, bwR], [pb])
                j, sm, smb = sm_p.next()
                j2, s2, s2b = sm2_p.next()
                B = [smb]
                P.tt("dve", sm[:, 0:36], ps[:, 0:36], bR[:], ALU.add, [pb, bbR], B)
                c = lambda i: sm[:, i:i + 1]
                S.op("dve", lambda e, o=c(112), i=sm[:, 0:4]: e.reduce_max(out=o, in_=i, axis=AX.X), B, B)
                P.ts("dve", c(113), c(112), -1.0, None, ALU.mult, None, B, B)
                P.act(sm[:, 36:40], sm[:, 0:4], AF.Exp, B, B, bias=c(113))
                yield
                S.op("dve", lambda e, o=c(114), i=sm[:, 36:40]: e.reduce_sum(out=o, in_=i, axis=AX.X), B, B)
                S.op("dve", lambda e, o=c(115), i=c(114): e.reciprocal(out=o, in_=i), B, B)
                P.ts("dve", sm[:, 40:44], sm[:, 0:4], c(112), None, ALU.is_equal, None, B, B)
                P.ts("dve", sm[:, 44:48], sm[:, 40:44], 1.0, 1e9, ALU.subtract, ALU.mult, B, B)
                for g in range(4):
                    P.ts("dve", sm[:, 48 + g * 8:56 + g * 8], sm[:, 4 + g * 8:12 + g * 8], sm[:, 44 + g:45 + g], None,
                         ALU.add, None, B, B)
                S.op("dve", lambda e, o=c(116), i=sm[:, 48:80]: e.reduce_max(out=o, in_=i, axis=AX.X), B, B)
                P.ts("dve", s2[:, 0:32], sm[:, 48:80], c(116), None, ALU.is_equal, None, B + [s2b], [s2b])
                P.stt("dve", sm[:, 80:112], s2[:, 0:32], -1e9, sm[:, 48:80], ALU.mult, ALU.add, B + [s2b], B)
                S.op("dve", lambda e, o=c(118), i=sm[:, 80:112]: e.reduce_max(out=o, in_=i, axis=AX.X), B, B)
                P.ts("dve", s2[:, 32:64], sm[:, 80:112], c(118), None, ALU.is_equal, None, B + [s2b], [s2b])
                P.ts("dve", c(117), c(116), -1.0, None, ALU.mult, None, B, B)
                P.act(c(119), c(118), AF.Exp, B, B, bias=c(117))
                yield
                P.ts("dve", c(120), c(119), 1.0, None, ALU.add, None, B, B)
                S.op("dve", lambda e, o=c(120): e.reciprocal(out=o, in_=o), B, B)
                P.tt("dve", c(121), c(115), c(120), ALU.mult, B, B)
                P.tt("dve", c(122), c(121), c(119), ALU.mult, B, B)
                P.ts("dve", s2[:, 0:32], s2[:, 0:32], c(121), None, ALU.mult, None, B + [s2b], [s2b])
                P.stt("dve", s2[:, 0:32], s2[:, 32:64], c(122), s2[:, 0:32], ALU.mult, ALU.add, B + [s2b], [s2b])
                ps, pb = self.ps_next()
                P.mm(ps[0:32, 0:128], s2[:, 0:32], self.ident[:], True, True, [s2b, bc], [pb])
                P.cp("act", WtT[0:32, tcs], ps[0:32, 0:128], [pb], [WtTb[s]])

            states = {}
            for step in range(16 + 2):
                if step < 16:
                    states[step] = phaseA(step)
                gens = []
                if 0 <= step - 1 < 16:
                    gens.append(phaseB(states[step - 1]))
                if 0 <= step - 2 < 16:
                    gens.append(phaseC(states.pop(step - 2)))
                while gens:
                    for g_ in list(gens):
                        try:
                            next(g_)
                        except StopIteration:
                            gens.remove(g_)
            S.barrier()

    def stage_F2(self, l, out_rows, W, actT, actTb, WtT, WtTb):
        S, P = self.S, self
        bc = self.b_const
        dr, db = self.dr, self.dbuf
        hT = actT
        with ExitStack() as st:
            acc = self.sb(st, "acc", [128, 16, D], F32)
            accb = [Buf("acc%d" % t) for t in range(16)]
            with ExitStack() as st2:
                hid_p = self.pool(st2, "hid", [128, 2, TL], BF16, 3)
                wgu_p = self.pool(st2, "wgu", [128, 8, 512], BF16, 2)
                wd_p = self.pool(st2, "wd", [128, 2, D], BF16, 3)
                wbc_p = self.pool(st2, "wbc", [128, TL], BF16, 1)
                sg_p = self.pool(st2, "sg", [128, 512], F32, 3)
                t_p = self.pool(st2, "tF", [128, 512], F32, 3)
                pend_down = [None]

                def down(eg, hids, wds):
                    for tile in range(16):
                        tcs = slice(tile * 128, (tile + 1) * 128)
                        for half in range(2):
                            ps, pb = self.ps_next()
                            k = 0
                            for ei in range(2):
                                for fc in range(2):
                                    P.mm(ps[:], hids[ei][0][:, fc, tcs], wds[ei][0][:, fc, half * 512:(half + 1) * 512],
                                         k == 0, k == 3, [hids[ei][1], wds[ei][1]], [pb])
                                    k += 1
                            dst = acc[:, tile, half * 512:(half + 1) * 512]
                            if eg == 0:
                                P.cp("dve", dst, ps[:], [pb], [accb[tile]])
                            else:
                                P.tt("dve", dst, dst, ps[:], ALU.add, [pb, accb[tile]], [accb[tile]])

                for eg in range(16):
                    hids, wds = [], []
                    for ei in range(2):
                        e_ = eg * 2 + ei
                        j, wgu, wgub = wgu_p.next()
                        P.dma("pool", "wgu%d" % j, wgu[:, :, 0:256], W["wEg"].ap()[e_].rearrange("p (k m) -> p k m", m=256),
                              [], [wgub])
                        P.dma("pool", "wgu%d" % j, wgu[:, :, 256:512], W["wEu"].ap()[e_].rearrange("p (k m) -> p k m", m=256),
                              [], [wgub])
                        j, wd, wdb = wd_p.next()
                        P.dma("pool", "wd%d" % j, wd[:], W["wEd"].ap()[e_].rearrange("p (k m) -> p k m", m=D), [], [wdb])
                        jw, wbc, wbcb = wbc_p.next()
                        for s in range(4):
                            cs = slice(s * 512, (s + 1) * 512)
                            ps, pb = self.ps_next()
                            P.mm(ps[:], self.selE[0:32, e_ * 128:(e_ + 1) * 128], WtT[0:32, cs], True, True,
                                 [bc, WtTb[s]], [pb])
                            P.cp("act", wbc[:, cs], ps[:], [pb], [wbcb])
                        jh, hid, hidb = hid_p.next()
                        for fc in range(2):
                            for s in range(4):
                                cs = slice(s * 512, (s + 1) * 512)
                                psg, pbg = self.ps_next()
                                for kc in range(8):
                                    P.mm(psg[:], wgu[:, kc, fc * 128:(fc + 1) * 128], hT[:, kc, cs], kc == 0, kc == 7,
                                         [wgub, actTb[s]], [pbg])
                                psu, pbu = self.ps_next()
                                for kc in range(8):
                                    P.mm(psu[:], wgu[:, kc, 256 + fc * 128:256 + (fc + 1) * 128], hT[:, kc, cs], kc == 0,
                                         kc == 7, [wgub, actTb[s]], [pbu])
                                j1, sg, sgb = sg_p.next()
                                P.act(sg[:], psg[:], AF.Silu, [pbg], [sgb])
                                j2, tt_, ttb = t_p.next()
                                P.tt("dve", tt_[:], sg[:], psu[:], ALU.mult, [sgb, pbu], [ttb])
                                P.tt("pool", hid[:, fc, cs], tt_[:], wbc[:, cs], ALU.mult, [ttb, wbcb], [hidb])
                        hids.append((hid, hidb))
                        wds.append((wd, wdb))
                        if ei == 0 and pend_down[0] is not None:
                            down(*pend_down[0])
                            pend_down[0] = None
                    pend_down[0] = (eg, hids, wds)
                down(*pend_down[0])
                S.barrier()
            with ExitStack() as st3:
                g2, bg2 = self.load_w(st3, "ln2g", [128, D], W["ln2_g"].ap().partition_broadcast(128), cast=False)
                b2, bb2 = self.load_w(st3, "ln2b", [128, D], W["ln2_b"].ap().partition_broadcast(128), cast=False)
                xr_p = self.pool(st3, "hr", [128, D], F32, 4)
                smln = self.pool(st3, "smln2", [128, 8], F32, 4)
                junk_p = self.pool(st3, "junk2", [128, D], F32, 2)

                hr_q = {}

                def hload(tile):
                    j, xr, xrb = xr_p.next()
                    P.dma("sp", "hr%d" % j, xr[:], dr["h"].ap()[tile * 128:(tile + 1) * 128, :], [db["h"]], [xrb])
                    hr_q[tile] = (xr, xrb)

                hload(0)
                hload(1)

                def l2a(tile):
                    if tile + 2 < 16:
                        hload(tile + 2)
                    xr, xrb = hr_q.pop(tile)
                    r = acc[:, tile, :]
                    P.stt("dve", r, xr[:], ALPHA, r, ALU.mult, ALU.add, [xrb, accb[tile]], [accb[tile]])
                    return self.ln_a(acc[:, tile, :], accb[tile], smln, junk_p)

                def l2b(tile, st_):
                    self.ln_b(acc[:, tile, :], accb[tile], g2, b2, [bg2, bb2], st_)
                    ob = self.out_bufs[tile // 4] if getattr(self, "out_bufs", None) else db["out"]
                    P.dma("sp", "oacc%d" % (tile % 4), out_rows(tile), acc[:, tile, :], [accb[tile]], [ob])
                    if tile % 4 == 3 and getattr(self, "after_slot", None):
                        self.after_slot(tile // 4)

                prev = None
                for tile in range(16):
                    cur = (tile, l2a(tile))
                    if prev is not None:
                        l2b(*prev)
                    prev = cur
                l2b(*prev)
                S.barrier()

    def build(self):
        nc, S = self.nc, self.S
        x_full = self.din("x_full", [SEQ, D])
        x_loc = self.din("x_loc", [TL, D])
        out = self.dout("out", [TL, D])
        Ws = {}
        import os
        stop = int(os.environ.get("KSTOP", "9"))
        for l in self.layers:
            Ws[l] = {k: self.din("%s_l%d" % (k, l), shp) for k, shp in LAYER_SHAPES.items()
                     if stop >= 6 or k not in ("wEg", "wEu", "wEd")}
        self.dr = {
            "KTm": self.dint("KTm", [4, 96, SEQ], BF16), "Vm": self.dint("Vm", [SEQ, 256], BF16),
            "KTs": self.dint("KTs", [4, 64, SEQ], BF16), "Vs": self.dint("Vs", [SEQ, 256], BF16),
            "KTf": self.dint("KTf", [4, 64, SEQ], BF16), "Vf": self.dint("Vf", [SEQ, 256], BF16),
            "h": self.dint("hbuf", [TL, D], F32),
        }
        self.dbuf = {k: Buf(k) for k in list(self.dr.keys()) + ["out"]}
        import os
        self.kv_exchange = os.environ.get("KVX", "1") == "1"
        if self.kv_exchange:
            self.xch = {
                "KVx": [self.dint("KVx%d" % s_, [896, GS], BF16) for s_ in range(4)],
                "KVg": [self.dint("KVg%d" % s_, [2 * 896, GS], BF16) for s_ in range(4)],
                "Vx": [self.dint("Vx%d" % s_, [GS, 768], BF16) for s_ in range(4)],
                "Vg": [self.dint("Vg%d" % s_, [2 * GS, 768], BF16) for s_ in range(4)],
                "spx": self.dint("spx", [128, 128], F32), "spg": self.dint("spg", [256, 128], F32),
                "bKVx": [Buf() for _ in range(4)], "bKVg": [Buf() for _ in range(4)],
                "bVx": [Buf() for _ in range(4)], "bVg": [Buf() for _ in range(4)],
                "bspx": Buf(), "bspg": Buf(),
            }
        with ExitStack() as st:
            self.setup_consts(st)
            self.setup_rope(st)
            if len(self.layers) == 1:
                l = self.layers[0]
                self.emit_layer(l, lambda g: x_full.ap()[g * GS:(g + 1) * GS, :],
                                lambda t: x_loc.ap()[t * 128:(t + 1) * 128, :],
                                lambda t: out.ap()[t * 128:(t + 1) * 128, :], Ws[l])
            else:
                xl1 = [self.dint("xl1_%d" % s_, [GS, D], F32) for s_ in range(4)]
                xg = [self.dint("xg_%d" % s_, [2 * GS, D], F32) for s_ in range(4)]
                bxl1 = [Buf("xl1_%d" % s_) for s_ in range(4)]
                bxg = [Buf("xg_%d" % s_) for s_ in range(4)]
                self.dbuf["out"] = None
                self.out_bufs = bxl1
                groups = [[0, 1], [2, 3], [4, 5], [6, 7]]

                def gather(s_):
                    S.dma("pool", "cc%d" % s_,
                          lambda e, s_=s_: e.collective_compute("AllGather", ALU.bypass, replica_groups=groups,
                                                                ins=[xl1[s_].ap().opt()], outs=[xg[s_].ap().opt()]),
                          [bxl1[s_]], [bxg[s_]], inc=1)
                self.after_slot = gather
                self.emit_layer(0, lambda g: x_full.ap()[g * GS:(g + 1) * GS, :],
                                lambda t: x_loc.ap()[t * 128:(t + 1) * 128, :],
                                lambda t: xl1[t // 4].ap()[(t % 4) * 128:(t % 4 + 1) * 128, :], Ws[0])
                self.after_slot = None
                if self.kv_exchange and os.environ.get("BSKIP", "1") == "1":
                    S.barrier(skip=("cc0", "cc1", "cc2", "cc3"))
                    self.xfull_bufs = lambda g_: [bxg[G_PAR[0].index(g_)] if g_ in G_PAR[0] else bxg[G_PAR[1].index(g_)]]
                else:
                    S.barrier()
                S.new_epoch()
                self.out_bufs = None
                self.dbuf["out"] = Buf("out")

                def xfull1(g):
                    if g in G_PAR[0]:
                        return xg[G_PAR[0].index(g)].ap()[0:GS, :]
                    return xg[G_PAR[1].index(g)].ap()[GS:2 * GS, :]
                self.emit_layer(1, xfull1, lambda t: xl1[t // 4].ap()[(t % 4) * 128:(t % 4 + 1) * 128, :],
                                lambda t: out.ap()[t * 128:(t + 1) * 128, :], Ws[1])
            S.barrier()
            S.emit()
        S.close()
        return nc


_PROG_CACHE = {}


def _get_prog(layers):
    key = tuple(layers)
    if key not in _PROG_CACHE:
        _PROG_CACHE[key] = Prog(list(layers)).build()
    return _PROG_CACHE[key]


def _idx(par):
    return np.concatenate([np.arange(g * GS, (g + 1) * GS) for g in G_PAR[par]])


def kernel(**inputs):
    inputs = {k: np.asarray(v) for k, v in inputs.items()}
    x = np.ascontiguousarray(inputs["x"], dtype=np.float32)
    positions = inputs["positions"]
    nc = _get_prog((0, 1))
    la = {}
    for l in range(DEPTH):
        for k, v in layer_arrays(inputs, l).items():
            la["%s_l%d" % (k, l)] = v
    in_maps = []
    for core in range(N_CORES):
        b, par = core // 2, core % 2
        thr, sel, inv2pi = core_meta(core)
        idx = _idx(par)
        m = {"x_full": np.ascontiguousarray(x[b]), "x_loc": np.ascontiguousarray(x[b][idx]),
             "pos_full": np.ascontiguousarray(positions[b].reshape(1, SEQ).astype(np.int32)),
             "pos_loc": np.ascontiguousarray(positions[b][idx].reshape(1, TL).astype(np.int32)),
             "thr": thr, "sel": sel, "inv2pi": inv2pi}
        m.update(la)
        in_maps.append(m)
    res = run_bass_kernel_spmd(nc, in_maps, core_ids=list(range(N_CORES)))
    out = np.empty_like(x)
    for core in range(N_CORES):
        b, par = core // 2, core % 2
        out[b][_idx(par)] = res.results[core]["out"]
    return out
```
